# Optimizing a Trainium2 kernel written in Bass

```python
import math
import jax, jax.numpy as jnp
from jax import lax
import numpy as np

D_MODEL = 1024
BATCH = 16
SEQ = 2048
DEPTH = 2

N_A_LAYERS = DEPTH // 2
N_B_LAYERS = DEPTH - N_A_LAYERS
N_HEADS = 16
HEAD_DIM = D_MODEL // N_HEADS
Q_BLOCK = 128
CONV_WIDTH = 31
N_EXPERTS = 32
TOP_K = 4
D_FF = D_MODEL
SWIGLU_LIMIT = 7.0
SWIGLU_ALPHA = 1.702
MOE_BLOCK = 128
ALPHA_DN = (2.0 * DEPTH) ** 0.25
BETA_DN = (8.0 * DEPTH) ** -0.25
LN_EPS = 1e-5

kernel_name = "yoco_conformer_stickbreaking_moe_adaln"


def layer_norm(x, g, b):
    xf = x.astype(jnp.float32)
    mu = jnp.mean(xf, axis=-1, keepdims=True)
    var = jnp.mean(jnp.square(xf - mu), axis=-1, keepdims=True)
    y = (xf - mu) * lax.rsqrt(var + LN_EPS) * g.astype(jnp.float32) + b.astype(jnp.float32)
    return y.astype(x.dtype)


def modulate(x, shift, scale):
    return x * (1.0 + scale[:, None, :]) + shift[:, None, :]


def conformer_conv(h, w1, b1, dw, db, lg, lb, w2, b2):
    u = h @ w1 + b1
    a, g = jnp.split(u, 2, axis=-1)
    u = a * jax.nn.sigmoid(g)
    u = lax.conv_general_dilated(
        u, dw[:, None, :], window_strides=(1,), padding=[(CONV_WIDTH - 1, 0)],
        dimension_numbers=('NWC', 'WIO', 'NWC'), feature_group_count=D_MODEL) + db
    u = jax.nn.silu(layer_norm(u, lg, lb))
    return u @ w2 + b2


def stick_breaking_attention(h, q_w, k, v, o_w):
    B, L, _ = h.shape
    q = (h @ q_w).reshape(B, L, N_HEADS, HEAD_DIM).transpose(0, 2, 1, 3)
    scale = HEAD_DIM ** -0.5
    outs = []
    for i in range(L // Q_BLOCK):
        t0, end = i * Q_BLOCK, (i + 1) * Q_BLOCK
        qb = q[:, :, t0:end]
        kb, vb = k[:, :, :end], v[:, :, :end]
        z = jnp.einsum('bhtd,bhsd->bhts', qb, kb).astype(jnp.float32) * scale
        t_pos = t0 + jnp.arange(Q_BLOCK)[:, None]
        s_pos = jnp.arange(end)[None, :]
        causal = s_pos < t_pos
        log_om = jnp.where(causal, jax.nn.log_sigmoid(-z), 0.0)
        suffix = lax.cumsum(log_om, axis=3, reverse=True) - log_om
        log_a = jax.nn.log_sigmoid(z) + suffix
        a = jnp.where(causal, jnp.exp(log_a), 0.0)
        outs.append(jnp.einsum('bhts,bhsd->bhtd', a.astype(vb.dtype), vb))
    o = jnp.concatenate(outs, axis=2)
    o = o.transpose(0, 2, 1, 3).reshape(B, L, D_MODEL)
    return o @ o_w


def clamped_swiglu(u):
    x_glu, x_lin = jnp.split(u, 2, axis=-1)
    x_glu = jnp.minimum(x_glu, SWIGLU_LIMIT)
    x_lin = jnp.clip(x_lin, -SWIGLU_LIMIT, SWIGLU_LIMIT)
    return (x_lin + 1.0) * (x_glu * jax.nn.sigmoid(SWIGLU_ALPHA * x_glu))


def moe(h, router_w, router_b, w_in, b_in, w_out, b_out):
    B, L, D = h.shape
    T = B * L
    A = T * TOP_K
    xt = h.reshape(T, D)
    logits = (xt @ router_w + router_b).astype(jnp.float32)
    top_val, top_idx = lax.top_k(logits, TOP_K)
    gates = jax.nn.softmax(top_val, axis=-1).astype(h.dtype)
    flat_e = top_idx.reshape(A).astype(jnp.int32)
    flat_tok = jnp.arange(A, dtype=jnp.int32) // TOP_K
    flat_g = gates.reshape(A)
    order = jnp.argsort(flat_e, stable=True)
    s_e, s_tok, s_g = flat_e[order], flat_tok[order], flat_g[order]
    counts = jnp.bincount(flat_e, length=N_EXPERTS).astype(jnp.int32)
    starts = jnp.cumsum(counts) - counts
    padded = ((counts + MOE_BLOCK - 1) // MOE_BLOCK) * MOE_BLOCK
    pend = jnp.cumsum(padded)
    pstart = pend - padded
    dest = pstart[s_e] + (jnp.arange(A, dtype=jnp.int32) - starts[s_e])
    n_blocks = -(-A // MOE_BLOCK) + N_EXPERTS
    R = n_blocks * MOE_BLOCK
    row_tok = jnp.zeros((R,), jnp.int32).at[dest].set(s_tok)
    row_g = jnp.zeros((R,), h.dtype).at[dest].set(s_g)
    block_start = jnp.arange(n_blocks, dtype=jnp.int32) * MOE_BLOCK
    block_e = jnp.minimum(jnp.searchsorted(pend, block_start, side='right'),
                          N_EXPERTS - 1).astype(jnp.int32)

    def expert_block(args):
        e, tok, g = args
        xe = xt[tok]
        u = xe @ w_in[e] + b_in[e]
        y = clamped_swiglu(u) @ w_out[e] + b_out[e]
        return y * g[:, None]

    yb = lax.map(expert_block, (block_e, row_tok.reshape(n_blocks, MOE_BLOCK),
                                row_g.reshape(n_blocks, MOE_BLOCK)))
    out = jnp.zeros((T, D), h.dtype).at[row_tok].add(yb.reshape(R, D))
    return out.reshape(B, L, D)


def setup_inputs(seed: int = 0) -> dict:
    key = jax.random.key(seed)
    ks = jax.random.split(key, 26)
    D, E, F = D_MODEL, N_EXPERTS, D_FF
    nrm = lambda k, s: jax.random.normal(k, s, jnp.float32)
    inv = D ** -0.5
    k_part = nrm(ks[13], (D, D)) * inv
    v_part = nrm(ks[14], (D, D)) * inv * BETA_DN
    return {
        "x": nrm(ks[0], (BATCH, SEQ, D)),
        "c": nrm(ks[1], (BATCH, D)),
        "ada_w": nrm(ks[2], (DEPTH, D, 6 * D)) * (0.1 * inv),
        "ada_b": 0.01 * nrm(ks[3], (DEPTH, 6 * D)),
        "ln_g": 1.0 + 0.01 * nrm(ks[4], (DEPTH, 2, D)),
        "ln_b": 0.01 * nrm(ks[5], (DEPTH, 2, D)),
        "cv_w1": nrm(ks[6], (N_A_LAYERS, D, 2 * D)) * inv,
        "cv_b1": 0.01 * nrm(ks[7], (N_A_LAYERS, 2 * D)),
        "cv_dw": nrm(ks[8], (N_A_LAYERS, CONV_WIDTH, D)) * CONV_WIDTH ** -0.5,
        "cv_db": 0.01 * nrm(ks[9], (N_A_LAYERS, D)),
        "cv_ln_g": 1.0 + 0.01 * nrm(ks[10], (N_A_LAYERS, D)),
        "cv_ln_b": 0.01 * nrm(ks[11], (N_A_LAYERS, D)),
        "cv_w2": nrm(ks[12], (N_A_LAYERS, D, D)) * inv * BETA_DN,
        "cv_b2": 0.01 * nrm(ks[15], (N_A_LAYERS, D)),
        "kv_w": jnp.concatenate([k_part, v_part], axis=1),
        "q_w": nrm(ks[16], (N_B_LAYERS, D, D)) * inv,
        "o_w": nrm(ks[17], (N_B_LAYERS, D, D)) * inv * BETA_DN,
        "router_w": nrm(ks[18], (DEPTH, D, E)) * inv,
        "router_b": 0.01 * nrm(ks[19], (DEPTH, E)),
        "moe_w_in": nrm(ks[20], (DEPTH, E, D, 2 * F)) * inv,
        "moe_b_in": 0.01 * nrm(ks[21], (DEPTH, E, 2 * F)),
        "moe_w_out": nrm(ks[22], (DEPTH, E, F, D)) * (F ** -0.5) * BETA_DN,
        "moe_b_out": 0.01 * nrm(ks[23], (DEPTH, E, D)),
    }


def reference(x, c, ada_w, ada_b, ln_g, ln_b, cv_w1, cv_b1, cv_dw, cv_db,
              cv_ln_g, cv_ln_b, cv_w2, cv_b2, kv_w, q_w, o_w, router_w,
              router_b, moe_w_in, moe_b_in, moe_w_out, moe_b_out):
    B, L, D = x.shape
    c_act = jax.nn.silu(c)
    k = v = None
    for l in range(DEPTH):
        mod = c_act @ ada_w[l] + ada_b[l]
        sh1, sc1, g1, sh2, sc2, g2 = jnp.split(mod, 6, axis=-1)
        h = modulate(x, sh1, sc1)
        if l < N_A_LAYERS:
            y = conformer_conv(h, cv_w1[l], cv_b1[l], cv_dw[l], cv_db[l],
                               cv_ln_g[l], cv_ln_b[l], cv_w2[l], cv_b2[l])
        else:
            j = l - N_A_LAYERS
            y = stick_breaking_attention(h, q_w[j], k, v, o_w[j])
        x = layer_norm(ALPHA_DN * x + (1.0 + g1)[:, None, :] * y, ln_g[l, 0], ln_b[l, 0])
        h = modulate(x, sh2, sc2)
        y = moe(h, router_w[l], router_b[l], moe_w_in[l], moe_b_in[l],
                moe_w_out[l], moe_b_out[l])
        x = layer_norm(ALPHA_DN * x + (1.0 + g2)[:, None, :] * y, ln_g[l, 1], ln_b[l, 1])
        if l == N_A_LAYERS - 1:
            kv = x @ kv_w
            k, v = jnp.split(kv, 2, axis=-1)
            k = k.reshape(B, L, N_HEADS, HEAD_DIM).transpose(0, 2, 1, 3)
            v = v.reshape(B, L, N_HEADS, HEAD_DIM).transpose(0, 2, 1, 3)
    return x
```

```python
import numpy as np
from contextlib import ExitStack
import concourse.bass as bass
import concourse.mybir as mybir
from concourse.bass_utils import run_bass_kernel_spmd

F32 = mybir.dt.float32
BF16 = mybir.dt.bfloat16
I32 = mybir.dt.int32
U32 = mybir.dt.uint32
AF = mybir.ActivationFunctionType
ALU = mybir.AluOpType
AX = mybir.AxisListType


class Tk:
    __slots__ = ("name", "w", "r")

    def __init__(self, name=""):
        self.name = name
        self.w = None
        self.r = {}


class Sched:
    SEM_ROLL = 30000
    NDMA = 8

    def __init__(self, nc, stack):
        self.nc = nc
        self.stack = stack
        self.engs = {"pe": nc.tensor, "dve": nc.vector, "act": nc.scalar,
                     "pool": nc.gpsimd, "sp": nc.sync}
        self.sems = []
        self.owner = []
        self.esem = {}
        self.ecnt = {}
        self.seen = {e: {} for e in self.engs}
        for e in self.engs:
            self._new_esem(e)
        self.dq = {}
        for q in ("sp", "pool", "act"):
            ids = [self._alloc_sem(f"dma_{q}_{i}", None) for i in range(self.NDMA)]
            self.dq[q] = {"ids": ids, "uses": [0] * self.NDMA, "next": 0}
        self.n_inst = 0
        self.n_wait = 0

    def _alloc_sem(self, name, owner):
        h = self.stack.enter_context(self.nc.semaphore(name))
        self.sems.append(h)
        self.owner.append(owner)
        return len(self.sems) - 1

    def _new_esem(self, e):
        sid = self._alloc_sem(f"e_{e}_{len(self.sems)}", e)
        self.esem[e] = sid
        self.ecnt[e] = 0

    def _wait(self, eng, deps):
        e = self.engs[eng]
        seen = self.seen[eng]
        for sid, val in deps.items():
            if seen.get(sid, 0) >= val:
                continue
            e.wait_ge(self.sems[sid], val)
            self.n_wait += 1
            seen[sid] = val

    def _deps(self, eng, reads, writes):
        deps = {}

        def add(tok, raw):
            sid, val = tok
            if self.owner[sid] == eng and not raw:
                return
            if deps.get(sid, 0) < val:
                deps[sid] = val
        for t in reads:
            if t.w is not None:
                add(t.w, True)
        for t in writes:
            if t.w is not None:
                add(t.w, False)
            for sid, val in t.r.items():
                add((sid, val), False)
        return deps

    def _commit(self, tok, reads, writes):
        sid, val = tok
        for t in reads:
            if t.r.get(sid, 0) < val:
                t.r[sid] = val
        for t in writes:
            t.w = tok
            t.r = {}

    def op(self, eng, fn, reads=(), writes=()):
        if self.ecnt[eng] >= self.SEM_ROLL:
            self._new_esem(eng)
        self._wait(eng, self._deps(eng, reads, writes))
        ins = fn(self.engs[eng])
        self.ecnt[eng] += 1
        ins.then_inc(self.sems[self.esem[eng]], 1)
        tok = (self.esem[eng], self.ecnt[eng])
        self._commit(tok, reads, writes)
        self.n_inst += 1
        return tok

    def dma(self, q, fn, reads=(), writes=()):
        d = self.dq[q]
        i = d["next"]
        d["next"] = (i + 1) % self.NDMA
        sid = d["ids"][i]
        deps = self._deps(q, reads, writes)
        if d["uses"][i] > 0:
            v = 16 * d["uses"][i]
            if deps.get(sid, 0) < v:
                deps[sid] = v
        self._wait(q, deps)
        ins = fn(self.engs[q])
        d["uses"][i] += 1
        ins.then_inc(self.sems[sid], 16)
        tok = (sid, 16 * d["uses"][i])
        self._commit(tok, reads, writes)
        self.n_inst += 1
        return tok

    def barrier(self):
        deps = {}
        for e in self.engs:
            if self.ecnt[e] > 0:
                deps[self.esem[e]] = self.ecnt[e]
        for q, d in self.dq.items():
            for i, sid in enumerate(d["ids"]):
                if d["uses"][i] > 0:
                    deps[sid] = 16 * d["uses"][i]
        for e in self.engs:
            dd = {s: v for s, v in deps.items() if self.owner[s] != e}
            self._wait(e, dd)

    def final_wait(self, toks, eng="sp"):
        deps = {}
        for sid, val in toks:
            if deps.get(sid, 0) < val:
                deps[sid] = val
        self._wait(eng, deps)


D = 1024
NTOK = 4096
SEQ = 2048
NT = NTOK // 128
NE = 32
CAP = 1024
NBLK = CAP // 128
ALPHA_DN = 4.0 ** 0.25
LN_EPS = 1e-5
SW_LIM = 7.0
SW_ALPHA = 1.702
BIG = 1.0e6


class B:
    def __init__(self, t):
        self.t = t
        self.k = Tk()

    def __getitem__(self, i):
        return self.t[i]


class K:
    def __init__(self, nc, S, dbg):
        self.nc = nc
        self.S = S
        self.dbg = dbg


_UID = [0]


def _sb(st, nc, name, shape, dt):
    _UID[0] += 1
    return B(st.enter_context(nc.sbuf_tensor(f"s{_UID[0]}_{name}", list(shape), dt)))


def _ps(st, nc, name, shape, dt=F32):
    _UID[0] += 1
    return B(st.enter_context(nc.psum_tensor(f"p{_UID[0]}_{name}", list(shape), dt)))


def build_program(upto=99, dbg=False, start=0, small_moe=False):
    nc = bass.Bass("TRN2", target_bir_lowering=False)
    dt_in = lambda n, s, d=F32: nc.dram_tensor(n, list(s), d, kind="ExternalInput").ap()
    I = {}
    I["x"] = dt_in("x", [NTOK, D])
    I["cT"] = dt_in("cT", [128, 8, 2])
    I["ada_w"] = dt_in("ada_w", [2, D, 6 * D] if start == 0 else [2, 8, 8])
    I["ada_b"] = dt_in("ada_b", [2, 6 * D])
    I["ln_g"] = dt_in("ln_g", [4, D])
    I["ln_b"] = dt_in("ln_b", [4, D])
    I["cv_w1"] = dt_in("cv_w1", [D, 2 * D])
    I["cv_b1T"] = dt_in("cv_b1T", [128, 16])
    I["cv_dwT"] = dt_in("cv_dwT", [128, 8, 31])
    I["cv_vecT"] = dt_in("cv_vecT", [128, 3, 8])
    I["cv_w2"] = dt_in("cv_w2", [D, D])
    I["cv_b2"] = dt_in("cv_b2", [1, D])
    I["kv_w"] = dt_in("kv_w", [D, 2 * D])
    I["q_w"] = dt_in("q_w", [D, D])
    I["o_w"] = dt_in("o_w", [D, D])
    I["router_w"] = dt_in("router_w", [2, D, NE])
    I["router_b"] = dt_in("router_b", [2, NE])
    I["moe_w_in"] = dt_in("moe_w_in", [2, NE, D, 2 * D] if not small_moe else [2, NE, 8, 16])
    I["moe_b_in"] = dt_in("moe_b_in", [2, NE, 2 * D])
    I["moe_w_out"] = dt_in("moe_w_out", [2, NE, D, D] if not small_moe else [2, NE, 8, 8])
    I["moe_b_out"] = dt_in("moe_b_out", [2, NE, D])
    out = nc.dram_tensor("out", [NTOK, D], F32, kind="ExternalOutput").ap()
    scr = lambda n, s, d: nc.dram_tensor(n, list(s), d, kind="Internal").ap()
    Dr = {}
    Dr["modrow"] = scr("modrow", [2, 2, 6 * D], F32)
    Dr["gluT"] = scr("gluT", [8, 128, NTOK], BF16)
    Dr["xa"] = scr("xa", [NTOK, D], F32)
    Dr["xb"] = scr("xb", [NTOK, D], F32)
    Dr["Xe"] = scr("Xe", [NE * CAP, D], BF16)
    Dr["Y"] = scr("Y", [NE * CAP, D], BF16)
    Dr["kT"] = scr("kT", [8, 128, NTOK], BF16)
    Dr["qT"] = scr("qT", [8, 128, NTOK], BF16)
    Dr["v"] = scr("v", [NTOK, D], BF16)
    Dr["oT"] = scr("oT", [8, 128, NTOK], BF16)
    dbg_out = {}
    if dbg:
        dbg_out["d_mod"] = nc.dram_tensor("d_mod", [2, 2, 6 * D], F32, kind="ExternalOutput").ap()
        dbg_out["d_xa"] = nc.dram_tensor("d_xa", [NTOK, D], F32, kind="ExternalOutput").ap()
        dbg_out["d_lg"] = nc.dram_tensor("d_lg", [NTOK, NE], F32, kind="ExternalOutput").ap()
        dbg_out["d_sl"] = nc.dram_tensor("d_sl", [NTOK, 4], I32, kind="ExternalOutput").ap()
        dbg_out["d_gt"] = nc.dram_tensor("d_gt", [NTOK, 4], F32, kind="ExternalOutput").ap()
        if start == 6:
            dbg_out["d_kT"] = nc.dram_tensor("d_kT", [8, 128, NTOK], BF16, kind="ExternalOutput").ap()
            dbg_out["d_v"] = nc.dram_tensor("d_v", [NTOK, D], BF16, kind="ExternalOutput").ap()

    with ExitStack() as gst:
        S = Sched(nc, gst)
        G = {}
        G["ident_f"] = _sb(gst, nc, "ident_f", [128, 128], F32)
        G["ident_b"] = _sb(gst, nc, "ident_b", [128, 128], BF16)
        G["ones_f"] = _sb(gst, nc, "ones_f", [128, 128], F32)
        G["ones_b"] = _sb(gst, nc, "ones_b", [128, 128], BF16)
        G["triU"] = _sb(gst, nc, "triU", [128, 128], F32)
        G["eps"] = _sb(gst, nc, "eps", [128, 1], F32)
        G["one"] = _sb(gst, nc, "one", [128, 1], F32)
        G["slots"] = _sb(gst, nc, "slots", [128, NT, 4], I32)
        G["gates"] = _sb(gst, nc, "gates", [128, NT, 4], F32)
        G["gmat"] = _sb(gst, nc, "gmat", [128, NT, NE], F32)
        G["modT"] = _sb(gst, nc, "modT", [128, 4, 6, 8], F32)
        G["ecap"] = _sb(gst, nc, "ecap", [128, NE], F32)
        tmp = _sb(gst, nc, "tmp_iota", [128, 128], F32)
        G["iota"] = tmp
        S.op("pool", lambda e: e.iota(tmp[:], pattern=[[1, 128]], base=0, channel_multiplier=-1,
                                      allow_small_or_imprecise_dtypes=True), writes=[tmp.k])
        S.op("dve", lambda e: e.tensor_single_scalar(G["ident_f"][:], tmp[:], 0.0, ALU.is_equal),
             reads=[tmp.k], writes=[G["ident_f"].k])
        S.op("dve", lambda e: e.tensor_single_scalar(G["ident_b"][:], tmp[:], 0.0, ALU.is_equal),
             reads=[tmp.k], writes=[G["ident_b"].k])
        S.op("dve", lambda e: e.tensor_single_scalar(G["triU"][:], tmp[:], 0.0, ALU.is_gt),
             reads=[tmp.k], writes=[G["triU"].k])
        S.op("dve", lambda e: e.memset(G["ones_f"][:], 1.0), writes=[G["ones_f"].k])
        S.op("dve", lambda e: e.memset(G["ones_b"][:], 1.0), writes=[G["ones_b"].k])
        S.op("dve", lambda e: e.memset(G["eps"][:], LN_EPS), writes=[G["eps"].k])
        S.op("dve", lambda e: e.memset(G["one"][:], 1.0), writes=[G["one"].k])
        S.op("pool", lambda e: e.iota(G["ecap"][:], pattern=[[CAP, NE]], base=0, channel_multiplier=0,
                                      allow_small_or_imprecise_dtypes=True), writes=[G["ecap"].k])
        ctx = K(nc, S, dbg)
        ctx.I, ctx.Dr, ctx.G, ctx.out, ctx.dbg_out = I, Dr, G, out, dbg_out
        ctx.upto = upto
        ctx.bc_reg = nc.gpsimd.to_reg(NE * CAP - 1)
        for nm in ('slots', 'gates', 'gmat'):
            G[nm].kt = [Tk() for _ in range(NT)]

        phases = [
            ("mod", lambda: phase_mod(ctx)),
            ("glu", lambda: phase_glu(ctx)),
            ("conv", lambda: phase_conv(ctx)),
            ("front0", lambda: phase_front(ctx, 0)),
            ("experts0", lambda: phase_experts(ctx, 0)),
            ("combine0", lambda: phase_combine(ctx, 0)),
            ("kvq", lambda: phase_kvq(ctx)),
            ("attn", lambda: phase_attn(ctx)),
            ("oproj", lambda: phase_oproj(ctx)),
            ("front1", lambda: phase_front(ctx, 1)),
            ("experts1", lambda: phase_experts(ctx, 1)),
            ("combine1", lambda: phase_combine(ctx, 1)),
        ]
        if start > 0:
            din = nc.dram_tensor("d_in_x", [NTOK, D], F32, kind="ExternalInput").ap()
            dstn = "xb" if start in (6, 7, 8) else "xa"
            S.dma("sp", lambda e: e.dma_start(out=Dr[dstn], in_=din))
            dmod = nc.dram_tensor("d_in_mod", [2, 2, 6 * D], F32, kind="ExternalInput").ap()
            S.dma("sp", lambda e: e.dma_start(out=Dr["modrow"], in_=dmod))
            S.barrier()
            load_modT(ctx)
            S.barrier()
        for i, (name, fn) in enumerate(phases):
            if i > upto:
                break
            if i < start:
                continue
            fn()
            S.barrier()
        if dbg:
            dump_dbg(ctx)
            S.barrier()
    return nc


def dump_dbg(ctx):
    nc, S = ctx.nc, ctx.S
    if "d_kT" in ctx.dbg_out:
        S.dma("sp", lambda e: e.dma_start(out=ctx.dbg_out["d_kT"], in_=ctx.Dr["kT"]))
        S.dma("sp", lambda e: e.dma_start(out=ctx.dbg_out["d_v"], in_=ctx.Dr["v"]))
    S.dma("sp", lambda e: e.dma_start(out=ctx.dbg_out["d_mod"], in_=ctx.Dr["modrow"]))
    S.dma("sp", lambda e: e.dma_start(out=ctx.dbg_out["d_xa"], in_=ctx.Dr["xa"]))
    S.dma("sp", lambda e: e.dma_start(out=ctx.dbg_out["d_sl"].rearrange("(t p) k -> p t k", p=128),
                                      in_=ctx.G["slots"][:]), reads=ctx.G["slots"].kt)
    S.dma("sp", lambda e: e.dma_start(out=ctx.dbg_out["d_gt"].rearrange("(t p) k -> p t k", p=128),
                                      in_=ctx.G["gates"][:]), reads=ctx.G["gates"].kt)


def phase_mod(ctx):
    nc, S, I, Dr, G = ctx.nc, ctx.S, ctx.I, ctx.Dr, ctx.G
    with ExitStack() as st:
        cT = _sb(st, nc, "cT", [128, 8, 2], F32)
        caT = _sb(st, nc, "caT", [128, 8, 2], BF16)
        wb = [_sb(st, nc, f"adaw{i}", [128, 8, 3 * D], BF16) for i in range(2)]
        bb = _sb(st, nc, "adab", [1, 2, 6 * D], BF16)
        msb = _sb(st, nc, "modsb", [2, 2, 6 * D], F32)
        pp = [_ps(st, nc, f"pmod{i}", [128, 512]) for i in range(2)]
        S.dma("sp", lambda e: e.dma_start(out=cT[:], in_=I["cT"]), writes=[cT.k])
        for l in range(2):
            cast_load(S, lambda c0, c1, l=l: bb[0:1, l, c0:c1], lambda c0, c1, l=l: I["ada_b"][l:l + 1, c0:c1], 6 * D, bb.k)
        S.op("act", lambda e: e.activation(caT[:], cT[:], AF.Silu), reads=[cT.k], writes=[caT.k])
        it = 0
        for l in range(2):
            for hf in range(2):
                w = wb[it % 2]
                src = I["ada_w"][l, :, hf * 3 * D:(hf + 1) * 3 * D].rearrange("(k p) n -> p k n", p=128)
                for k in range(8):
                    cast_load(S, lambda c0, c1, k=k: w[:, k, c0:c1], lambda c0, c1, k=k: src[:, k, c0:c1], 3 * D, w.k, step=1536)
                for n in range(6):
                    p = pp[n % 2]
                    col = hf * 3 * D + n * 512
                    for k in range(8):
                        S.op("pe", lambda e, k=k: e.matmul(p[0:2, :], caT[:, k, :], w[:, k, n * 512:(n + 1) * 512],
                                                          start=(k == 0), stop=False),
                             reads=[caT.k, w.k], writes=[p.k])
                    S.op("pe", lambda e: e.matmul(p[0:2, :], G["ones_b"][0:1, 0:2], bb[0:1, l, col:col + 512],
                                                  start=False, stop=True),
                         reads=[G["ones_b"].k, bb.k], writes=[p.k])
                    S.op("act", lambda e: e.activation(msb[0:2, l, col:col + 512], p[0:2, :], AF.Copy),
                         reads=[p.k], writes=[msb.k])
                it += 1
        for l in range(2):
            S.dma("sp", lambda e, l=l: e.dma_start(out=Dr["modrow"][l, :, :], in_=msb[0:2, l, :]), reads=[msb.k])
    S.barrier()
    load_modT(ctx)


def load_modT(ctx):
    nc, S, I, Dr, G = ctx.nc, ctx.S, ctx.I, ctx.Dr, ctx.G
    for l in range(2):
        for b in range(2):
            for wch in range(6):
                S.dma("sp", lambda e, l=l, b=b, wch=wch: e.dma_start(
                    out=G["modT"][:, l * 2 + b, wch, :],
                    in_=Dr["modrow"][l, b, wch * D:(wch + 1) * D].rearrange("(k p) -> p k", p=128),
                    allow_slow_non_contiguous=True), writes=[G["modT"].k])


def load_bc(ctx, st, name, src_row):
    t = _sb(st, ctx.nc, name, [128, D], F32)
    ctx.S.dma("sp", lambda e: e.dma_start(out=t[:], in_=src_row.partition_broadcast(128)), writes=[t.k])
    return t


def add_one(ctx, t):
    ctx.S.op("pool", lambda e: e.tensor_scalar_add(t[:], t[:], 1.0), reads=[t.k], writes=[t.k])


def mod_scale_bias(ctx, st, l, w_sh, w_sc, name):
    nc, S, G = ctx.nc, ctx.S, ctx.G
    sc = _sb(st, nc, name + "_sc", [128, 2, 8], F32)
    sh = _sb(st, nc, name + "_sh", [128, 2, 8], F32)
    for b in range(2):
        S.op("dve", lambda e, b=b: e.tensor_scalar_add(sc[:, b, :], G["modT"][:, l * 2 + b, w_sc, :], 1.0),
             reads=[G["modT"].k], writes=[sc.k])
        S.op("dve", lambda e, b=b: e.tensor_copy(sh[:, b, :], G["modT"][:, l * 2 + b, w_sh, :]),
             reads=[G["modT"].k], writes=[sh.k])
    return sc, sh


def run_interleaved(gens, width):
    active = []
    it = iter(gens)
    more = True
    while True:
        while more and len(active) < width:
            try:
                active.append(next(it))
            except StopIteration:
                more = False
        if not active:
            break
        for g in list(active):
            try:
                next(g)
            except StopIteration:
                active.remove(g)


class Epi:
    def __init__(self, ctx, st, l, sub, gate_which):
        nc, S, I, Dr = ctx.nc, ctx.S, ctx.I, ctx.Dr
        self.ctx = ctx
        self.lng = load_bc(ctx, st, f"lng{l}{sub}", I["ln_g"][l * 2 + sub, :])
        self.lnb = load_bc(ctx, st, f"lnb{l}{sub}", I["ln_b"][l * 2 + sub, :])
        self.gate = []
        for b in range(2):
            g = load_bc(ctx, st, f"gate{l}{sub}{b}", Dr["modrow"][l, b, gate_which * D:(gate_which + 1) * D])
            add_one(ctx, g)
            self.gate.append(g)
        self.t1 = [_sb(st, nc, f"ep_t1_{i}", [128, D], F32) for i in range(2)]
        self.r = [_sb(st, nc, f"ep_r_{i}", [128, D], F32) for i in range(2)]
        self.xo = [_sb(st, nc, f"ep_xo_{i}", [128, D], F32) for i in range(2)]
        self.st6 = [_sb(st, nc, f"ep_st_{i}", [128, 2, 6], F32) for i in range(2)]
        self.mv = [_sb(st, nc, f"ep_mv_{i}", [128, 4], F32) for i in range(2)]
        self.n = 0

    def run_g(self, ys, x_t, b, dst):
        S = self.ctx.S
        i = self.n % 2
        self.n += 1
        t1, r, xo, st6, mv = self.t1[i], self.r[i], self.xo[i], self.st6[i], self.mv[i]
        g = self.gate[b]
        for h, (yb, yap) in enumerate(ys):
            S.op("dve", lambda e, h=h, yap=yap: e.tensor_tensor(t1[:, h * 512:(h + 1) * 512], yap,
                                                              g[:, h * 512:(h + 1) * 512], ALU.mult),
                 reads=[yb.k, g.k], writes=[t1.k])
        yield
        S.op("dve", lambda e: e.scalar_tensor_tensor(r[:], x_t[:], ALPHA_DN, t1[:], ALU.mult, ALU.add),
             reads=[x_t.k, t1.k], writes=[r.k])
        yield
        for h in range(2):
            S.op("dve", lambda e, h=h: e.bn_stats(st6[:, h, :], r[:, h * 512:(h + 1) * 512]),
                 reads=[r.k], writes=[st6.k])
        S.op("dve", lambda e: e.bn_aggr(mv[:, 0:2], st6[:].rearrange("p a b -> p (a b)")), reads=[st6.k], writes=[mv.k])
        yield
        S.op("act", lambda e: e.activation(mv[:, 2:3], mv[:, 1:2], AF.Sqrt, bias=self.ctx.G["eps"][:], scale=1.0),
             reads=[mv.k, self.ctx.G["eps"].k], writes=[mv.k])
        yield
        S.op("dve", lambda e: e.reciprocal(mv[:, 2:3], mv[:, 2:3]), reads=[mv.k], writes=[mv.k])
        S.op("dve", lambda e: e.tensor_scalar(mv[:, 3:4], mv[:, 0:1], -1.0, mv[:, 2:3], ALU.mult, ALU.mult),
             reads=[mv.k], writes=[mv.k])
        S.op("act", lambda e: e.activation(t1[:], r[:], AF.Identity, bias=mv[:, 3:4], scale=mv[:, 2:3]),
             reads=[r.k, mv.k], writes=[t1.k])
        yield
        S.op("pool", lambda e: e.tensor_tensor(xo[:], t1[:], self.lng[:], ALU.mult),
             reads=[t1.k, self.lng.k], writes=[xo.k])
        S.op("pool", lambda e: e.tensor_tensor(xo[:], xo[:], self.lnb[:], ALU.add),
             reads=[xo.k, self.lnb.k], writes=[xo.k])
        S.dma("sp", lambda e: e.dma_start(out=dst, in_=xo[:]), reads=[xo.k])
        yield

    def run(self, ys, x_t, b, dst):
        for _ in self.run_g(ys, x_t, b, dst):
            pass


def transpose_tile(ctx, src, psT, evac):
    S, G = ctx.S, ctx.G
    for k in range(8):
        p = psT[k // 4]
        S.op("pe", lambda e, k=k, p=p: e.transpose(p[:, (k % 4) * 128:(k % 4 + 1) * 128], src[:, k * 128:(k + 1) * 128],
                                                 G["ident_f"][:]),
             reads=[src.k, G["ident_f"].k], writes=[p.k])
    for k in range(8):
        p = psT[k // 4]
        evac(k, p, p[:, (k % 4) * 128:(k % 4 + 1) * 128])


def phase_glu(ctx):
    nc, S, I, Dr, G = ctx.nc, ctx.S, ctx.I, ctx.Dr, ctx.G
    with ExitStack() as st:
        w1 = _sb(st, nc, "w1", [128, 8, 2 * D], BF16)
        b1T = _sb(st, nc, "b1T", [128, 16], F32)
        sc, sh = mod_scale_bias(ctx, st, 0, 0, 1, "m1")
        xt = [_sb(st, nc, f"xt{i}", [128, D], F32) for i in range(3)]
        hT = [_sb(st, nc, f"hT{i}", [128, 8, 512], BF16) for i in range(2)]
        sig = [_sb(st, nc, f"sig{i}", [128, 512], F32) for i in range(2)]
        gl = [_sb(st, nc, f"gl{i}", [128, 8, 512], BF16) for i in range(2)]
        psT = [_ps(st, nc, f"psT{i}", [128, 512]) for i in range(2)]
        pa = [_ps(st, nc, f"pa{i}", [128, 512]) for i in range(2)]
        pg = [_ps(st, nc, f"pg{i}", [128, 512]) for i in range(2)]
        src = I["cv_w1"].rearrange("(k p) n -> p k n", p=128)
        for k in range(8):
            S.dma("pool", lambda e, k=k: e.dma_start(out=w1[:, k, :], in_=src[:, k, :]), writes=[w1.k])
        S.dma("sp", lambda e: e.dma_start(out=b1T[:], in_=I["cv_b1T"]), writes=[b1T.k])
        ti = 0
        for j in range(NTOK // 512):
            b = (j * 512) // SEQ
            h = hT[j % 2]
            for i in range(4):
                x_t = xt[ti % 3]
                ti += 1
                r0 = j * 512 + i * 128
                S.dma("sp", lambda e, x_t=x_t, r0=r0: e.dma_start(out=x_t[:], in_=I["x"][r0:r0 + 128, :]), writes=[x_t.k])
                transpose_tile(ctx, x_t, psT, lambda k, p, ap, i=i: S.op(
                    "act", lambda e: e.activation(h[:, k, i * 128:(i + 1) * 128], ap, AF.Identity,
                                                  bias=sh[:, b, k:k + 1], scale=sc[:, b, k:k + 1]),
                    reads=[p.k, sh.k, sc.k], writes=[h.k]))
            g_o = gl[j % 2]
            for m in range(8):
                a_p, g_p, sg = pa[m % 2], pg[m % 2], sig[m % 2]
                for k in range(8):
                    S.op("pe", lambda e, k=k: e.matmul(a_p[:], w1[:, k, m * 128:(m + 1) * 128], h[:, k, :],
                                                      start=(k == 0), stop=(k == 7)), reads=[w1.k, h.k], writes=[a_p.k])
                for k in range(8):
                    S.op("pe", lambda e, k=k: e.matmul(g_p[:], w1[:, k, D + m * 128:D + (m + 1) * 128], h[:, k, :],
                                                      start=(k == 0), stop=(k == 7)), reads=[w1.k, h.k], writes=[g_p.k])
                S.op("act", lambda e: e.activation(sg[:], g_p[:], AF.Sigmoid, bias=b1T[:, 8 + m:9 + m], scale=1.0),
                     reads=[g_p.k, b1T.k], writes=[sg.k])
                S.op("dve", lambda e: e.scalar_tensor_tensor(g_o[:, m, :], a_p[:], b1T[:, m:m + 1], sg[:], ALU.add, ALU.mult),
                     reads=[a_p.k, b1T.k, sg.k], writes=[g_o.k])
            S.dma("sp", lambda e, j=j: e.dma_start(out=Dr["gluT"][:, :, j * 512:(j + 1) * 512].rearrange("k p t -> p k t"),
                                                  in_=g_o[:]), reads=[g_o.k])


def phase_conv(ctx):
    nc, S, I, Dr, G = ctx.nc, ctx.S, ctx.I, ctx.Dr, ctx.G
    with ExitStack() as st:
        dwT = _sb(st, nc, "dwT", [128, 8, 31], F32)
        vecT = _sb(st, nc, "vecT", [128, 3, 8], F32)
        dg = _sb(st, nc, "dg", [128, 8 * 31, 128], BF16)
        w2 = _sb(st, nc, "w2", [128, 8, D], BF16)
        b2 = _sb(st, nc, "b2", [1, D], BF16)
        epi = Epi(ctx, st, 0, 0, 2)
        glb = [_sb(st, nc, f"glb{i}", [128, 8, 544], BF16) for i in range(2)]
        vb = _sb(st, nc, "vb", [128, 8, 512], BF16)
        vsq = [_sb(st, nc, f"vsq{i}", [128, 512], BF16) for i in range(2)]
        sT = _sb(st, nc, "sT", [128, 8, 512], BF16)
        mean = _sb(st, nc, "cmean", [128, 512], F32)
        msq = _sb(st, nc, "cmsq", [128, 512], F32)
        rstd = _sb(st, nc, "crstd", [128, 512], F32)
        nmr = _sb(st, nc, "cnmr", [128, 512], F32)
        zt = [_sb(st, nc, f"czt{i}", [128, 512], F32) for i in range(2)]
        xt = [_sb(st, nc, f"cxt{i}", [128, D], F32) for i in range(2)]
        pc = [_ps(st, nc, f"pc{i}", [128, 512]) for i in range(2)]
        ps1 = _ps(st, nc, "ps1", [128, 512])
        ps2 = _ps(st, nc, "ps2", [128, 512])
        py = [_ps(st, nc, f"py{i}", [128, 512]) for i in range(4)]
        S.dma("sp", lambda e: e.dma_start(out=dwT[:], in_=I["cv_dwT"]), writes=[dwT.k])
        S.dma("sp", lambda e: e.dma_start(out=vecT[:], in_=I["cv_vecT"]), writes=[vecT.k])
        S.dma("pool", lambda e: e.dma_start(out=b2[:], in_=I["cv_b2"]), writes=[b2.k])
        src = I["cv_w2"].rearrange("(k p) n -> p k n", p=128)
        for k in range(8):
            S.dma("pool", lambda e, k=k: e.dma_start(out=w2[:, k, :], in_=src[:, k, :]), writes=[w2.k])
        for c in range(8):
            for k in range(31):
                S.op("dve", lambda e, c=c, k=k: e.tensor_scalar(dg[:, c * 31 + k, :], G["ident_b"][:], dwT[:, c, k:k + 1], None,
                                                              ALU.mult),
                     reads=[G["ident_b"].k, dwT.k], writes=[dg.k])
        ti = 0
        for j in range(NTOK // 512):
            b = (j * 512) // SEQ
            t0 = j * 512
            g_in = glb[j % 2]
            if t0 % SEQ == 0:
                S.op("pool", lambda e: e.memset(g_in[:, :, 0:30], 0.0), writes=[g_in.k])
                S.dma("sp", lambda e: e.dma_start(out=g_in[:, :, 30:542],
                                                  in_=Dr["gluT"][:, :, t0:t0 + 512].rearrange("k p t -> p k t")),
                      writes=[g_in.k])
            else:
                S.dma("sp", lambda e: e.dma_start(out=g_in[:, :, 0:542],
                                                  in_=Dr["gluT"][:, :, t0 - 30:t0 + 512].rearrange("k p t -> p k t")),
                      writes=[g_in.k])
            for c in range(8):
                p = pc[c % 2]
                vq = vsq[c % 2]
                for k in range(31):
                    S.op("pe", lambda e, k=k: e.matmul(p[:], dg[:, c * 31 + k, :], g_in[:, c, k:k + 512],
                                                      start=(k == 0), stop=(k == 30)),
                         reads=[dg.k, g_in.k], writes=[p.k])
                S.op("act", lambda e: e.activation(vb[:, c, :], p[:], AF.Identity, bias=vecT[:, 0, c:c + 1], scale=1.0),
                     reads=[p.k, vecT.k], writes=[vb.k])
                S.op("act", lambda e: e.activation(vq[:], p[:], AF.Square, bias=vecT[:, 0, c:c + 1], scale=1.0),
                     reads=[p.k, vecT.k], writes=[vq.k])
                S.op("pe", lambda e: e.matmul(ps1[:], G["ones_b"][:], vb[:, c, :], start=(c == 0), stop=(c == 7)),
                     reads=[G["ones_b"].k, vb.k], writes=[ps1.k])
                S.op("pe", lambda e: e.matmul(ps2[:], G["ones_b"][:], vq[:], start=(c == 0), stop=(c == 7)),
                     reads=[G["ones_b"].k, vq.k], writes=[ps2.k])
            S.op("act", lambda e: e.activation(mean[:], ps1[:], AF.Copy, scale=1.0 / D), reads=[ps1.k], writes=[mean.k])
            S.op("act", lambda e: e.activation(msq[:], ps1[:], AF.Square, scale=1.0 / D), reads=[ps1.k], writes=[msq.k])
            S.op("dve", lambda e: e.scalar_tensor_tensor(rstd[:], ps2[:], 1.0 / D, msq[:], ALU.mult, ALU.subtract),
                 reads=[ps2.k, msq.k], writes=[rstd.k])
            S.op("act", lambda e: e.activation(rstd[:], rstd[:], AF.Sqrt, bias=G["eps"][:], scale=1.0),
                 reads=[rstd.k, G["eps"].k], writes=[rstd.k])
            S.op("dve", lambda e: e.reciprocal(rstd[:], rstd[:]), reads=[rstd.k], writes=[rstd.k])
            S.op("dve", lambda e: e.scalar_tensor_tensor(nmr[:], mean[:], -1.0, rstd[:], ALU.mult, ALU.mult),
                 reads=[mean.k, rstd.k], writes=[nmr.k])
            for c in range(8):
                z = zt[c % 2]
                S.op("dve", lambda e: e.tensor_tensor(z[:], vb[:, c, :], rstd[:], ALU.mult), reads=[vb.k, rstd.k], writes=[z.k])
                S.op("pool", lambda e: e.tensor_tensor(z[:], z[:], nmr[:], ALU.add), reads=[z.k, nmr.k], writes=[z.k])
                S.op("act", lambda e: e.activation(sT[:, c, :], z[:], AF.Silu, bias=vecT[:, 2, c:c + 1],
                                                   scale=vecT[:, 1, c:c + 1]),
                     reads=[z.k, vecT.k], writes=[sT.k])
            for i in range(4):
                r0 = t0 + i * 128
                x_t = xt[ti % 2]
                S.dma("sp", lambda e: e.dma_start(out=x_t[:], in_=I["x"][r0:r0 + 128, :]), writes=[x_t.k])
                ys = []
                for n in range(2):
                    p = py[(ti % 2) * 2 + n]
                    for c in range(8):
                        S.op("pe", lambda e, c=c: e.matmul(p[:], sT[:, c, i * 128:(i + 1) * 128], w2[:, c, n * 512:(n + 1) * 512],
                                                          start=(c == 0), stop=False), reads=[sT.k, w2.k], writes=[p.k])
                    S.op("pe", lambda e: e.matmul(p[:], G["ones_b"][0:1, :], b2[0:1, n * 512:(n + 1) * 512], start=False, stop=True),
                         reads=[G["ones_b"].k, b2.k], writes=[p.k])
                    ys.append((p, p[:]))
                ti += 1
                epi.run(ys, x_t, b, Dr["xa"][r0:r0 + 128, :])


def phase_front(ctx, l):
    nc, S, I, Dr, G = ctx.nc, ctx.S, ctx.I, ctx.Dr, ctx.G
    with ExitStack() as st:
        S2, H2 = [], []
        for b in range(2):
            s2 = load_bc(ctx, st, f"S2_{b}", Dr["modrow"][l, b, 4 * D:5 * D])
            add_one(ctx, s2)
            S2.append(s2)
            H2.append(load_bc(ctx, st, f"H2_{b}", Dr["modrow"][l, b, 3 * D:4 * D]))
        rw = _sb(st, nc, "rw", [128, 8, NE], F32)
        rb = _sb(st, nc, "rb", [1, NE], F32)
        srun = _sb(st, nc, "srun", [128, NE], F32)
        xt = [_sb(st, nc, f"fx{i}", [128, D], F32) for i in range(2)]
        h2 = [_sb(st, nc, f"fh{i}", [128, D], F32) for i in range(2)]
        h2b = [_sb(st, nc, f"fhb{i}", [128, D], BF16) for i in range(3)]
        h2T = [_sb(st, nc, f"fhT{i}", [128, 8, 128], F32) for i in range(2)]
        sm = lambda nm, w: [_sb(st, nc, f"{nm}{i}", [128, w], F32) for i in range(2)]
        lg, top8, nv0, ex, ssum, mask, slotv, bad, oh, junk, slotf, okk, g4, gtmp = (
            sm("lg", NE), sm("top8", 8), sm("nv0", 1), sm("ex", 4), sm("ssum", 1), sm("mask", NE), sm("slotv", NE),
            sm("bad", NE), sm("oh", NE), sm("junk", NE), sm("slotf", 4), sm("okk", 4), sm("g4", 4), sm("gtmp", NE))
        cur = sm("cur", NE)
        psT = [_ps(st, nc, f"fpT{i}", [128, 512]) for i in range(2)]
        plg = [_ps(st, nc, f"fplg{i}", [128, 512]) for i in range(2)]
        ppos = [_ps(st, nc, f"fpps{i}", [128, 512]) for i in range(2)]
        S.dma("sp", lambda e: e.dma_start(out=rw[:], in_=I["router_w"][l].rearrange("(k p) e -> p k e", p=128)), writes=[rw.k])
        S.dma("sp", lambda e: e.dma_start(out=rb[:], in_=I["router_b"][l:l + 1, :]), writes=[rb.k])
        S.op("dve", lambda e: e.memset(srun[:], 0.0), writes=[srun.k])
        def tile_g(t):
            b = t // 16
            i = t % 2
            x_t, h, hb, hT = xt[i], h2[i], h2b[t % 3], h2T[i]
            S.dma("sp", lambda e: e.dma_start(out=x_t[:], in_=Dr["xa"][t * 128:(t + 1) * 128, :]), writes=[x_t.k])
            S.op("pool", lambda e: e.tensor_tensor(h[:], x_t[:], S2[b][:], ALU.mult), reads=[x_t.k, S2[b].k], writes=[h.k])
            S.op("pool", lambda e: e.tensor_tensor(h[:], h[:], H2[b][:], ALU.add), reads=[h.k, H2[b].k], writes=[h.k])
            S.op("act", lambda e: e.activation(hb[:], h[:], AF.Copy), reads=[h.k], writes=[hb.k])
            yield
            transpose_tile(ctx, h, psT, lambda k, p, ap: S.op(
                "act", lambda e: e.activation(hT[:, k, :], ap, AF.Copy), reads=[p.k], writes=[hT.k]))
            yield
            pl = plg[i]
            for k in range(8):
                S.op("pe", lambda e, k=k: e.matmul(pl[:, 0:NE], hT[:, k, :], rw[:, k, :], start=(k == 0), stop=False),
                     reads=[hT.k, rw.k], writes=[pl.k])
            S.op("pe", lambda e: e.matmul(pl[:, 0:NE], G["ones_f"][0:1, :], rb[0:1, :], start=False, stop=True),
                 reads=[G["ones_f"].k, rb.k], writes=[pl.k])
            S.op("dve", lambda e: e.tensor_copy(lg[i][:], pl[:, 0:NE]), reads=[pl.k], writes=[lg[i].k])
            yield
            S.op("dve", lambda e: e.tensor_copy(cur[i][:], lg[i][:]), reads=[lg[i].k], writes=[cur[i].k])
            yield
            for k in range(4):
                S.op("dve", lambda e, k=k: e.tensor_reduce(top8[i][:, k:k + 1], cur[i][:], AX.X, ALU.max),
                     reads=[cur[i].k], writes=[top8[i].k])
                if k < 3:
                    S.op("dve", lambda e, k=k: e.tensor_scalar(oh[i][:], cur[i][:], top8[i][:, k:k + 1], None, ALU.is_equal),
                         reads=[cur[i].k, top8[i].k], writes=[oh[i].k])
                    S.op("dve", lambda e: e.scalar_tensor_tensor(cur[i][:], oh[i][:], -BIG, cur[i][:], ALU.mult, ALU.add),
                         reads=[oh[i].k, cur[i].k], writes=[cur[i].k])
                yield
            S.op("dve", lambda e: e.tensor_scalar_mul(nv0[i][:], top8[i][:, 0:1], -1.0), reads=[top8[i].k], writes=[nv0[i].k])
            S.op("act", lambda e: e.activation(ex[i][:], top8[i][:, 0:4], AF.Exp, bias=nv0[i][:], scale=1.0),
                 reads=[top8[i].k, nv0[i].k], writes=[ex[i].k])
            S.op("dve", lambda e: e.tensor_reduce(ssum[i][:], ex[i][:], AX.X, ALU.add), reads=[ex[i].k], writes=[ssum[i].k])
            S.op("dve", lambda e: e.reciprocal(ssum[i][:], ssum[i][:]), reads=[ssum[i].k], writes=[ssum[i].k])
            S.op("dve", lambda e: e.tensor_scalar(g4[i][:], ex[i][:], ssum[i][:], None, ALU.mult),
                 reads=[ex[i].k, ssum[i].k], writes=[g4[i].k])
            yield
            S.op("dve", lambda e: e.tensor_scalar(mask[i][:], lg[i][:], top8[i][:, 3:4], None, ALU.is_ge),
                 reads=[lg[i].k, top8[i].k], writes=[mask[i].k])
            pp = ppos[i]
            S.op("pe", lambda e: e.matmul(pp[:, 0:NE], G["triU"][:], mask[i][:], start=True, stop=False),
                 reads=[G["triU"].k, mask[i].k], writes=[pp.k])
            S.op("pe", lambda e: e.matmul(pp[:, 0:NE], G["ones_f"][:], srun[:], start=False, stop=True),
                 reads=[G["ones_f"].k, srun.k], writes=[pp.k])
            S.op("dve", lambda e: e.tensor_tensor(srun[:], srun[:], mask[i][:], ALU.add), reads=[srun.k, mask[i].k], writes=[srun.k])
            yield
            S.op("dve", lambda e: e.tensor_single_scalar(bad[i][:], pp[:, 0:NE], float(CAP) - 0.5, ALU.is_ge),
                 reads=[pp.k], writes=[bad[i].k])
            S.op("dve", lambda e: e.tensor_tensor(slotv[i][:], pp[:, 0:NE], G["ecap"][:], ALU.add),
                 reads=[pp.k, G["ecap"].k], writes=[slotv[i].k])
            S.op("dve", lambda e: e.scalar_tensor_tensor(slotv[i][:], bad[i][:], BIG, slotv[i][:], ALU.mult, ALU.add),
                 reads=[bad[i].k, slotv[i].k], writes=[slotv[i].k])
            yield
            for k in range(4):
                S.op("dve", lambda e, k=k: e.tensor_scalar(oh[i][:], lg[i][:], top8[i][:, k:k + 1], None, ALU.is_equal),
                     reads=[lg[i].k, top8[i].k], writes=[oh[i].k])
                S.op("dve", lambda e: e.tensor_tensor(junk[i][:], oh[i][:], slotv[i][:], ALU.mult),
                     reads=[oh[i].k, slotv[i].k], writes=[junk[i].k])
                S.op("dve", lambda e, k=k: e.tensor_reduce(slotf[i][:, k:k + 1], junk[i][:], AX.X, ALU.add),
                     reads=[junk[i].k], writes=[slotf[i].k])
                yield
            S.op("dve", lambda e: e.tensor_single_scalar(okk[i][:], slotf[i][:], 1.0e5, ALU.is_lt), reads=[slotf[i].k], writes=[okk[i].k])
            yield
            gk, sk, mk = G["gates"].kt[t], G["slots"].kt[t], G["gmat"].kt[t]
            S.op("dve", lambda e: e.tensor_tensor(G["gates"][:, t, :], g4[i][:], okk[i][:], ALU.mult),
                 reads=[g4[i].k, okk[i].k], writes=[gk])
            S.op("dve", lambda e: e.tensor_copy(G["slots"][:, t, :], slotf[i][:]), reads=[slotf[i].k], writes=[sk])
            yield
            for k in range(4):
                dstm = G["gmat"][:, t, :] if k == 0 else gtmp[i][:]
                S.op("dve", lambda e, k=k, dstm=dstm: e.tensor_scalar(dstm, lg[i][:], top8[i][:, k:k + 1], G["gates"][:, t, k:k + 1],
                                                                    ALU.is_equal, ALU.mult),
                     reads=[lg[i].k, top8[i].k, gk], writes=[mk if k == 0 else gtmp[i].k])
                if k > 0:
                    S.op("dve", lambda e: e.tensor_tensor(G["gmat"][:, t, :], G["gmat"][:, t, :], gtmp[i][:], ALU.add),
                         reads=[mk, gtmp[i].k], writes=[mk])
            for k in range(4):
                S.dma("pool", lambda e, k=k: e.indirect_dma_start(
                    out=Dr["Xe"], out_offset=bass.IndirectOffsetOnAxis(ap=G["slots"][:, t, k:k + 1], axis=0),
                    in_=hb[:], in_offset=None, bounds_check=ctx.bc_reg, oob_is_err=False), reads=[hb.k, sk])
            yield

        run_interleaved((tile_g(t) for t in range(NT)), 2)


def phase_experts(ctx, l):
    nc, S, I, Dr, G = ctx.nc, ctx.S, ctx.I, ctx.Dr, ctx.G
    with ExitStack() as st:
        win = [_sb(st, nc, f"win{i}", [128, 8, 2 * D], BF16) for i in range(2)]
        wout = [_sb(st, nc, f"wout{i}", [128, 8, D], BF16) for i in range(2)]
        bin_ = [_sb(st, nc, f"bin{i}", [1, 2 * D], BF16) for i in range(2)]
        xe = [_sb(st, nc, f"xe{i}", [128, D], BF16) for i in range(2)]
        xT = [_sb(st, nc, f"xT{i}", [128, 8, 128], BF16) for i in range(2)]
        xg = [_sb(st, nc, f"xg{i}", [128, 512], F32) for i in range(2)]
        sg = [_sb(st, nc, f"sg{i}", [128, 512], F32) for i in range(2)]
        xl = [_sb(st, nc, f"xl{i}", [128, 512], F32) for i in range(2)]
        tt = [_sb(st, nc, f"tt{i}", [128, 512], F32) for i in range(2)]
        act = [_sb(st, nc, f"act{i}", [128, D], BF16) for i in range(2)]
        actT = [_sb(st, nc, f"actT{i}", [128, 8, 128], BF16) for i in range(2)]
        yb = [_sb(st, nc, f"yb{i}", [128, D], BF16) for i in range(2)]
        psT = [_ps(st, nc, f"epT{i}", [128, 1024], BF16) for i in range(2)]
        pu = [_ps(st, nc, f"epu{i}", [128, 512]) for i in range(4)]
        py = [_ps(st, nc, f"epy{i}", [128, 512]) for i in range(2)]

        def load_w(e):
            w, wo, bi = win[e % 2], wout[e % 2], bin_[e % 2]
            s1 = I["moe_w_in"][l, e].rearrange("(k p) n -> p k n", p=128)
            s2 = I["moe_w_out"][l, e].rearrange("(k p) n -> p k n", p=128)
            for k in range(8):
                S.dma("pool", lambda q, k=k: q.dma_start(out=w[:, k, :], in_=s1[:, k, :]), writes=[w.k])
            for k in range(8):
                S.dma("pool", lambda q, k=k: q.dma_start(out=wo[:, k, :], in_=s2[:, k, :]), writes=[wo.k])
            S.dma("pool", lambda q: q.dma_start(out=bi[:], in_=I["moe_b_in"][l, e:e + 1, :]), writes=[bi.k])

        load_w(0)
        load_w(1)
        blocks = [(e_, j) for e_ in range(NE) for j in range(NBLK)]
        NB = len(blocks)
        xe4 = xe + [_sb(st, nc, f"xe{i}", [128, D], BF16) for i in range(2, 4)]

        def load_x(n):
            e_, j = blocks[n]
            r0 = e_ * CAP + j * 128
            x_e = xe4[n % 4]
            S.dma("sp", lambda q: q.dma_start(out=x_e[:], in_=Dr["Xe"][r0:r0 + 128, :]), writes=[x_e.k])

        def t_x(n):
            x_e, x_T = xe4[n % 4], xT[n % 2]
            pt = psT[0]
            for k in range(8):
                S.op("pe", lambda q, k=k: q.transpose(pt[:, k * 128:(k + 1) * 128], x_e[:, k * 128:(k + 1) * 128], G["ident_b"][:]),
                     reads=[x_e.k, G["ident_b"].k], writes=[pt.k])
            S.op("act", lambda q: q.activation(x_T[:].rearrange("p k t -> p (k t)"), pt[:], AF.Copy), reads=[pt.k], writes=[x_T.k])

        def mm1(n):
            e_, j = blocks[n]
            i = n % 2
            w, bi = win[e_ % 2], bin_[e_ % 2]
            x_T, a_ = xT[i], act[i]
            for hf in range(2):
                pg, pl = pu[hf * 2], pu[hf * 2 + 1]
                for (p, c0) in ((pg, hf * 512), (pl, D + hf * 512)):
                    for k in range(8):
                        S.op("pe", lambda q, k=k: q.matmul(p[:], x_T[:, k, :], w[:, k, c0:c0 + 512], start=(k == 0), stop=False),
                             reads=[x_T.k, w.k], writes=[p.k])
                    S.op("pe", lambda q: q.matmul(p[:], G["ones_b"][0:1, :], bi[0:1, c0:c0 + 512], start=False, stop=True),
                         reads=[G["ones_b"].k, bi.k], writes=[p.k])
                S.op("dve", lambda q: q.tensor_scalar_min(xg[hf][:], pg[:], SW_LIM), reads=[pg.k], writes=[xg[hf].k])
                S.op("act", lambda q: q.activation(sg[hf][:], xg[hf][:], AF.Sigmoid, scale=SW_ALPHA), reads=[xg[hf].k], writes=[sg[hf].k])
                S.op("dve", lambda q: q.tensor_scalar(xl[hf][:], pl[:], SW_LIM, -SW_LIM, ALU.min, ALU.max), reads=[pl.k], writes=[xl[hf].k])
                S.op("dve", lambda q: q.scalar_tensor_tensor(tt[hf][:], xl[hf][:], 1.0, xg[hf][:], ALU.add, ALU.mult),
                     reads=[xl[hf].k, xg[hf].k], writes=[tt[hf].k])
                S.op("dve", lambda q: q.tensor_tensor(a_[:, hf * 512:(hf + 1) * 512], tt[hf][:], sg[hf][:], ALU.mult),
                     reads=[tt[hf].k, sg[hf].k], writes=[a_.k])

        def t_a(n):
            i = n % 2
            a_, a_T = act[i], actT[i]
            pt2 = psT[1]
            for k in range(8):
                S.op("pe", lambda q, k=k: q.transpose(pt2[:, k * 128:(k + 1) * 128], a_[:, k * 128:(k + 1) * 128], G["ident_b"][:]),
                     reads=[a_.k, G["ident_b"].k], writes=[pt2.k])
            S.op("act", lambda q: q.activation(a_T[:].rearrange("p k t -> p (k t)"), pt2[:], AF.Copy), reads=[pt2.k], writes=[a_T.k])

        def mm2(n):
            e_, j = blocks[n]
            i = n % 2
            wo = wout[e_ % 2]
            r0 = e_ * CAP + j * 128
            a_T, y_ = actT[i], yb[i]
            for n2 in range(2):
                for k in range(8):
                    S.op("pe", lambda q, k=k: q.matmul(py[n2][:], a_T[:, k, :], wo[:, k, n2 * 512:(n2 + 1) * 512], start=(k == 0), stop=(k == 7)),
                         reads=[a_T.k, wo.k], writes=[py[n2].k])
                S.op("act", lambda q: q.activation(y_[:, n2 * 512:(n2 + 1) * 512], py[n2][:], AF.Copy), reads=[py[n2].k], writes=[y_.k])
            S.dma("act", lambda q: q.dma_start(out=Dr["Y"][r0:r0 + 128, :], in_=y_[:]), reads=[y_.k])

        for n in range(min(4, NB)):
            load_x(n)
        t_x(0)
        mm1(0)
        t_x(1)
        mm1(1)
        for m in range(NB):
            t_a(m)
            if m + 2 < NB:
                t_x(m + 2)
            mm2(m)
            if m + 4 < NB:
                load_x(m + 4)
            e_, j = blocks[m]
            if j == NBLK - 1 and e_ + 2 < NE:
                load_w(e_ + 2)
            if m + 2 < NB:
                mm1(m + 2)


def phase_combine(ctx, l):
    nc, S, I, Dr, G = ctx.nc, ctx.S, ctx.I, ctx.Dr, ctx.G
    final = (l == 1) or (ctx.upto == 5)
    with ExitStack() as st:
        epi = Epi(ctx, st, l, 1, 5)
        bo = _sb(st, nc, "bo", [NE, D], F32)
        yk = [[_sb(st, nc, f"yk{k}_{i}", [128, D], BF16) for i in range(2)] for k in range(4)]
        gmT = [_sb(st, nc, f"gmT{i}", [NE, 128], F32) for i in range(2)]
        acc = [_sb(st, nc, f"acc{i}", [128, D], F32) for i in range(2)]
        xt = [_sb(st, nc, f"cx{i}", [128, D], F32) for i in range(2)]
        pT = [_ps(st, nc, f"cpT{i}", [128, 512]) for i in range(2)]
        pb = [_ps(st, nc, f"cpb{i}", [128, 512]) for i in range(4)]
        S.dma("sp", lambda e: e.dma_start(out=bo[:], in_=I["moe_b_out"][l]), writes=[bo.k])
        for k in range(4):
            for i in range(2):
                S.op("pool", lambda e: e.memset(yk[k][i][:], 0.0), writes=[yk[k][i].k])
        def tile_g(t):
            b = t // 16
            i = t % 2
            x_t = xt[i]
            S.dma("sp", lambda e: e.dma_start(out=x_t[:], in_=Dr["xa"][t * 128:(t + 1) * 128, :]), writes=[x_t.k])
            for k in range(4):
                S.dma("pool", lambda e, k=k: e.indirect_dma_start(
                    out=yk[k][i][:], out_offset=None, in_=Dr["Y"],
                    in_offset=bass.IndirectOffsetOnAxis(ap=G["slots"][:, t, k:k + 1], axis=0),
                    bounds_check=ctx.bc_reg, oob_is_err=False), reads=[G["slots"].kt[t]], writes=[yk[k][i].k])
            S.op("pe", lambda e: e.transpose(pT[i][0:NE, 0:128], G["gmat"][:, t, :], G["ident_f"][:]),
                 reads=[G["gmat"].kt[t], G["ident_f"].k], writes=[pT[i].k])
            S.op("act", lambda e: e.activation(gmT[i][:], pT[i][0:NE, 0:128], AF.Copy), reads=[pT[i].k], writes=[gmT[i].k])
            yield
            a = acc[i]
            for n in range(2):
                p = pb[i * 2 + n]
                S.op("pe", lambda e: e.matmul(p[:], gmT[i][:], bo[:, n * 512:(n + 1) * 512], start=True, stop=True),
                     reads=[gmT[i].k, bo.k], writes=[p.k])
                S.op("dve", lambda e: e.scalar_tensor_tensor(a[:, n * 512:(n + 1) * 512], yk[0][i][:, n * 512:(n + 1) * 512],
                                                             G["gates"][:, t, 0:1], p[:], ALU.mult, ALU.add),
                     reads=[yk[0][i].k, G["gates"].kt[t], p.k], writes=[a.k])
                yield
            for k in range(1, 4):
                S.op("dve", lambda e, k=k: e.scalar_tensor_tensor(a[:], yk[k][i][:], G["gates"][:, t, k:k + 1], a[:], ALU.mult, ALU.add),
                     reads=[yk[k][i].k, G["gates"].kt[t], a.k], writes=[a.k])
                yield
            dst = (ctx.out if final else Dr["xb"])[t * 128:(t + 1) * 128, :]
            yield from epi.run_g([(a, a[:, 0:512]), (a, a[:, 512:1024])], x_t, b, dst)

        run_interleaved((tile_g(t) for t in range(NT)), 2)


def phase_kvq(ctx):
    nc, S, I, Dr, G = ctx.nc, ctx.S, ctx.I, ctx.Dr, ctx.G
    with ExitStack() as st:
        kvw = _sb(st, nc, "kvw", [128, 8, 2 * D], BF16)
        qw = _sb(st, nc, "qw", [128, 8, D], BF16)
        sc, sh = mod_scale_bias(ctx, st, 1, 0, 1, "m1b")
        xt = [_sb(st, nc, f"kx{i}", [128, D], F32) for i in range(3)]
        x2T = [_sb(st, nc, f"kxT{i}", [128, 8, 512], BF16) for i in range(2)]
        hT = [_sb(st, nc, f"khT{i}", [128, 8, 512], BF16) for i in range(2)]
        ko = [_sb(st, nc, f"ko{i}", [128, 8, 512], BF16) for i in range(2)]
        qo = [_sb(st, nc, f"qo{i}", [128, 8, 512], BF16) for i in range(2)]
        vo = [_sb(st, nc, f"vo{i}", [128, D], BF16) for i in range(2)]
        psT = [_ps(st, nc, f"kpT{i}", [128, 512]) for i in range(2)]
        pk = [_ps(st, nc, f"kpk{i}", [128, 512]) for i in range(2)]
        pq = [_ps(st, nc, f"kpq{i}", [128, 512]) for i in range(2)]
        pv = [_ps(st, nc, f"kpv{i}", [128, 512]) for i in range(2)]
        s1 = I["kv_w"].rearrange("(k p) n -> p k n", p=128)
        s2 = I["q_w"].rearrange("(k p) n -> p k n", p=128)
        for k in range(8):
            S.dma("pool", lambda e, k=k: e.dma_start(out=kvw[:, k, :], in_=s1[:, k, :]), writes=[kvw.k])
            S.dma("pool", lambda e, k=k: e.dma_start(out=qw[:, k, :], in_=s2[:, k, :]), writes=[qw.k])
        ti = 0
        vi = 0
        for j in range(NTOK // 512):
            b = (j * 512) // SEQ
            xT_, h = x2T[j % 2], hT[j % 2]
            for i in range(4):
                x_t = xt[ti % 3]
                ti += 1
                r0 = j * 512 + i * 128
                S.dma("sp", lambda e: e.dma_start(out=x_t[:], in_=Dr["xb"][r0:r0 + 128, :]), writes=[x_t.k])

                def ev(k, p, ap, i=i):
                    S.op("act", lambda e: e.activation(xT_[:, k, i * 128:(i + 1) * 128], ap, AF.Copy), reads=[p.k], writes=[xT_.k])
                    S.op("act", lambda e: e.activation(h[:, k, i * 128:(i + 1) * 128], ap, AF.Identity,
                                                       bias=sh[:, b, k:k + 1], scale=sc[:, b, k:k + 1]),
                         reads=[p.k, sh.k, sc.k], writes=[h.k])
                transpose_tile(ctx, x_t, psT, ev)
            import os
            part = int(os.environ.get("KVQ_PART", "9"))
            if part < 2:
                continue
            k_o, q_o = ko[j % 2], qo[j % 2]
            for m in range(8):
                p1, p2 = pk[m % 2], pq[m % 2]
                for k in range(8):
                    S.op("pe", lambda e, k=k: e.matmul(p1[:], kvw[:, k, m * 128:(m + 1) * 128], xT_[:, k, :], start=(k == 0), stop=(k == 7)),
                         reads=[kvw.k, xT_.k], writes=[p1.k])
                S.op("act", lambda e: e.activation(k_o[:, m, :], p1[:], AF.Copy), reads=[p1.k], writes=[k_o.k])
                for k in range(8):
                    S.op("pe", lambda e, k=k: e.matmul(p2[:], qw[:, k, m * 128:(m + 1) * 128], h[:, k, :], start=(k == 0), stop=(k == 7)),
                         reads=[qw.k, h.k], writes=[p2.k])
                S.op("act", lambda e: e.activation(q_o[:, m, :], p2[:], AF.Copy, scale=0.125), reads=[p2.k], writes=[q_o.k])
            S.dma("sp", lambda e: e.dma_start(out=Dr["kT"][:, :, j * 512:(j + 1) * 512].rearrange("k p t -> p k t"), in_=k_o[:]), reads=[k_o.k])
            S.dma("sp", lambda e: e.dma_start(out=Dr["qT"][:, :, j * 512:(j + 1) * 512].rearrange("k p t -> p k t"), in_=q_o[:]), reads=[q_o.k])
            if part < 3:
                continue
            for i in range(4):
                r0 = j * 512 + i * 128
                v_o = vo[vi % 2]
                vi += 1
                for n in range(2):
                    p = pv[n]
                    for k in range(8):
                        S.op("pe", lambda e, k=k: e.matmul(p[:], xT_[:, k, i * 128:(i + 1) * 128], kvw[:, k, D + n * 512:D + (n + 1) * 512],
                                                          start=(k == 0), stop=(k == 7)), reads=[xT_.k, kvw.k], writes=[p.k])
                    S.op("act", lambda e: e.activation(v_o[:, n * 512:(n + 1) * 512], p[:], AF.Copy), reads=[p.k], writes=[v_o.k])
                S.dma("sp", lambda e: e.dma_start(out=Dr["v"][r0:r0 + 128, :], in_=v_o[:]), reads=[v_o.k])


def phase_attn(ctx):
    nc, S, I, Dr, G = ctx.nc, ctx.S, ctx.I, ctx.Dr, ctx.G
    with ExitStack() as st:
        kT = _sb(st, nc, "akT", [128, 8, SEQ], BF16)
        qT = _sb(st, nc, "aqT", [128, 8, SEQ], BF16)
        v = _sb(st, nc, "av", [128, 16, D], BF16)
        oT = _sb(st, nc, "aoT", [128, 8, SEQ], BF16)
        ntri = _sb(st, nc, "ntri", [128, 128], BF16)
        nones = _sb(st, nc, "nones", [128, 128], BF16)
        mtmp = _sb(st, nc, "mtmp", [128, 512], F32)
        masks = [_sb(st, nc, f"amask{r}", [128, 512], BF16) for r in range(4)]
        e_sb = [_sb(st, nc, f"ae{i}", [128, 512], F32) for i in range(2)]
        sp = [[_sb(st, nc, f"asp{s_}{i}", [128, 512], BF16) for i in range(2)] for s_ in range(2)]
        a_sb = [[_sb(st, nc, f"aa{s_}{i}", [128, 512], BF16) for i in range(2)] for s_ in range(2)]
        acc = [[_sb(st, nc, f"aacc{s_}{i}", [128, 512], BF16) for i in range(2)] for s_ in range(2)]
        pz = [[_ps(st, nc, f"apz{s_}{i}", [128, 512]) for i in range(2)] for s_ in range(2)]
        pw = [_ps(st, nc, f"apw{i}", [128, 512]) for i in range(2)]
        po = [_ps(st, nc, f"apo{i}", [128, 512]) for i in range(2)]
        S.op("dve", lambda e: e.tensor_scalar(ntri[:], G["iota"][:], 0.0, -1.0, ALU.is_le, ALU.mult), reads=[G["iota"].k], writes=[ntri.k])
        S.op("dve", lambda e: e.memset(nones[:], -1.0), writes=[nones.k])
        for r in range(4):
            S.op("pool", lambda e, r=r: e.iota(mtmp[:], pattern=[[1, 512]], base=-128 * r, channel_multiplier=-1,
                                              allow_small_or_imprecise_dtypes=True), writes=[mtmp.k])
            S.op("dve", lambda e, r=r: e.tensor_single_scalar(masks[r][:], mtmp[:], 0.0, ALU.is_gt), reads=[mtmp.k], writes=[masks[r].k])
        it = 0
        for b in range(2):
            t0 = b * SEQ
            S.dma("sp", lambda e: e.dma_start(out=kT[:], in_=Dr["kT"][:, :, t0:t0 + SEQ].rearrange("k p t -> p k t")), writes=[kT.k])
            S.dma("sp", lambda e: e.dma_start(out=qT[:], in_=Dr["qT"][:, :, t0:t0 + SEQ].rearrange("k p t -> p k t")), writes=[qT.k])
            S.dma("sp", lambda e: e.dma_start(out=v[:], in_=Dr["v"][t0:t0 + SEQ, :].rearrange("(s p) d -> p s d", p=128)), writes=[v.k])
            for ch in range(8):
                pbs = [0, 64]
                for c in range(4):
                    nkb = 4 * c + 4
                    kbs = list(range(nkb - 1, -1, -1))
                    q_ap = [qT[pb:pb + 64, ch, c * 512:(c + 1) * 512] for pb in pbs]

                    def k_ap(kb, s_):
                        return kT[pbs[s_]:pbs[s_] + 64, ch, kb * 128:(kb + 1) * 128]

                    def stP(idx):
                        kb = kbs[idx]
                        r = kb - 4 * c
                        i2 = idx % 2
                        for s_ in (0, 1):
                            z = pz[s_][i2]
                            S.op("pe", lambda q: q.matmul(z[:], k_ap(kb, s_), q_ap[s_], start=True, stop=True), reads=[kT.k, qT.k], writes=[z.k])
                        for s_ in (0, 1):
                            z = pz[s_][i2]
                            S.op("act", lambda q: q.activation(e_sb[s_][:], z[:], AF.Exp), reads=[z.k], writes=[e_sb[s_].k])
                            S.op("act", lambda q: q.activation(sp[s_][i2][:], e_sb[s_][:], AF.Ln, bias=G["one"][:], scale=1.0),
                                 reads=[e_sb[s_].k, G["one"].k], writes=[sp[s_][i2].k])
                        if r >= 0:
                            for s_ in (0, 1):
                                S.op("dve", lambda q: q.tensor_tensor(sp[s_][i2][:], sp[s_][i2][:], masks[r][:], ALU.mult),
                                     reads=[sp[s_][i2].k, masks[r].k], writes=[sp[s_][i2].k])

                    def stQ(idx):
                        kb = kbs[idx]
                        r = kb - 4 * c
                        i2 = idx % 2
                        for s_ in (0, 1):
                            w = pw[s_]
                            S.op("pe", lambda q: q.matmul(w[:], k_ap(kb, s_), q_ap[s_], start=True, stop=False), reads=[kT.k, qT.k], writes=[w.k])
                            S.op("pe", lambda q: q.matmul(w[:], ntri[:], sp[s_][i2][:], start=False, stop=(idx == 0)),
                                 reads=[ntri.k, sp[s_][i2].k], writes=[w.k])
                            if idx > 0:
                                ac = acc[s_][(idx - 1) % 2]
                                S.op("pe", lambda q: q.matmul(w[:], nones[:], ac[:], start=False, stop=True), reads=[nones.k, ac.k], writes=[w.k])
                        for s_ in (0, 1):
                            S.op("act", lambda q: q.activation(a_sb[s_][i2][:], pw[s_][:], AF.Exp), reads=[pw[s_].k], writes=[a_sb[s_][i2].k])
                        if r >= 0:
                            for s_ in (0, 1):
                                S.op("dve", lambda q: q.tensor_tensor(a_sb[s_][i2][:], a_sb[s_][i2][:], masks[r][:], ALU.mult),
                                     reads=[a_sb[s_][i2].k, masks[r].k], writes=[a_sb[s_][i2].k])
                        for s_ in (0, 1):
                            h = 2 * ch + s_
                            S.op("pe", lambda q: q.matmul(po[s_][pbs[s_]:pbs[s_] + 64, :], v[:, kb, h * 64:(h + 1) * 64], a_sb[s_][i2][:],
                                                          start=(idx == 0), stop=(idx == nkb - 1)),
                                 reads=[v.k, a_sb[s_][i2].k], writes=[po[s_].k])
                        if idx < nkb - 1:
                            for s_ in (0, 1):
                                an = acc[s_][idx % 2]
                                if idx == 0:
                                    S.op("pool", lambda q: q.tensor_copy(an[:], sp[s_][i2][:]), reads=[sp[s_][i2].k], writes=[an.k])
                                else:
                                    S.op("pool", lambda q: q.tensor_tensor(an[:], acc[s_][(idx - 1) % 2][:], sp[s_][i2][:], ALU.add),
                                         reads=[acc[s_][(idx - 1) % 2].k, sp[s_][i2].k], writes=[an.k])

                    stP(0)
                    for idx in range(nkb):
                        if idx + 1 < nkb:
                            stP(idx + 1)
                        stQ(idx)
                    for s_ in (0, 1):
                        pb = 64 * s_
                        S.op("act", lambda q: q.activation(oT[pb:pb + 64, ch, c * 512:(c + 1) * 512], po[s_][pb:pb + 64, :], AF.Copy),
                             reads=[po[s_].k], writes=[oT.k])
            S.dma("sp", lambda e: e.dma_start(out=Dr["oT"][:, :, t0:t0 + SEQ].rearrange("k p t -> p k t"), in_=oT[:]), reads=[oT.k])


def phase_oproj(ctx):
    nc, S, I, Dr, G = ctx.nc, ctx.S, ctx.I, ctx.Dr, ctx.G
    with ExitStack() as st:
        ow = _sb(st, nc, "ow", [128, 8, D], BF16)
        epi = Epi(ctx, st, 1, 0, 2)
        oTb = [_sb(st, nc, f"ooT{i}", [128, 8, 512], BF16) for i in range(2)]
        xt = [_sb(st, nc, f"ox{i}", [128, D], F32) for i in range(2)]
        py = [_ps(st, nc, f"opy{i}", [128, 512]) for i in range(4)]
        s1 = I["o_w"].rearrange("(k p) n -> p k n", p=128)
        for k in range(8):
            S.dma("pool", lambda e, k=k: e.dma_start(out=ow[:, k, :], in_=s1[:, k, :]), writes=[ow.k])
        ti = 0
        for j in range(NTOK // 512):
            b = (j * 512) // SEQ
            o_b = oTb[j % 2]
            S.dma("sp", lambda e: e.dma_start(out=o_b[:], in_=Dr["oT"][:, :, j * 512:(j + 1) * 512].rearrange("k p t -> p k t")), writes=[o_b.k])
            for i in range(4):
                r0 = j * 512 + i * 128
                x_t = xt[ti % 2]
                S.dma("sp", lambda e: e.dma_start(out=x_t[:], in_=Dr["xb"][r0:r0 + 128, :]), writes=[x_t.k])
                ys = []
                for n in range(2):
                    p = py[(ti % 2) * 2 + n]
                    for c in range(8):
                        S.op("pe", lambda e, c=c: e.matmul(p[:], o_b[:, c, i * 128:(i + 1) * 128], ow[:, c, n * 512:(n + 1) * 512],
                                                          start=(c == 0), stop=(c == 7)), reads=[o_b.k, ow.k], writes=[p.k])
                    ys.append((p, p[:]))
                ti += 1
                epi.run(ys, x_t, b, Dr["xa"][r0:r0 + 128, :])


def cast_load(S, dst_ap_fn, src_ap_fn, ncols, wkey, q="pool", step=2048):
    for c0 in range(0, ncols, step):
        c1 = min(ncols, c0 + step)
        S.dma(q, lambda e, c0=c0, c1=c1: e.dma_start(out=dst_ap_fn(c0, c1), in_=src_ap_fn(c0, c1)), writes=[wkey])


def make_in_maps(inp, cores):
    f = lambda a: np.ascontiguousarray(a, dtype=np.float32)
    x, c = inp["x"], inp["c"]
    shared = {
        "ada_w": f(inp["ada_w"]), "ada_b": f(inp["ada_b"]),
        "ln_g": f(inp["ln_g"].reshape(4, D)), "ln_b": f(inp["ln_b"].reshape(4, D)),
        "cv_w1": f(inp["cv_w1"][0]),
        "cv_b1T": f(inp["cv_b1"][0].reshape(16, 128).T),
        "cv_dwT": f(inp["cv_dw"][0].reshape(31, 8, 128).transpose(2, 1, 0)),
        "cv_vecT": f(np.stack([inp["cv_db"][0].reshape(8, 128).T, inp["cv_ln_g"][0].reshape(8, 128).T,
                               inp["cv_ln_b"][0].reshape(8, 128).T], axis=1)),
        "cv_w2": f(inp["cv_w2"][0]), "cv_b2": f(inp["cv_b2"][0].reshape(1, D)),
        "kv_w": f(inp["kv_w"]), "q_w": f(inp["q_w"][0]), "o_w": f(inp["o_w"][0]),
        "router_w": f(inp["router_w"]), "router_b": f(inp["router_b"]),
        "moe_w_in": f(inp["moe_w_in"]), "moe_b_in": f(inp["moe_b_in"]),
        "moe_w_out": f(inp["moe_w_out"]), "moe_b_out": f(inp["moe_b_out"]),
    }
    maps = []
    for ci in cores:
        m = dict(shared)
        m["x"] = f(x[2 * ci:2 * ci + 2].reshape(NTOK, D))
        m["cT"] = f(c[2 * ci:2 * ci + 2].reshape(2, 8, 128).transpose(2, 1, 0))
        maps.append(m)
    return maps


def kernel(**inputs):
    inp = {k: np.asarray(v) for k, v in inputs.items()}
    nc = build_program()
    maps = make_in_maps(inp, list(range(8)))
    res = run_bass_kernel_spmd(nc, maps, core_ids=list(range(8)))
    outs = [r["out"].reshape(2, SEQ, D) for r in res.results]
    return np.concatenate(outs, axis=0).astype(np.float32)
```

```python
import numpy as np
from contextlib import ExitStack
import concourse.bass as bass
import concourse.mybir as mybir
from concourse.bass_utils import run_bass_kernel_spmd

F32 = mybir.dt.float32
BF16 = mybir.dt.bfloat16
I32 = mybir.dt.int32
U32 = mybir.dt.uint32
AF = mybir.ActivationFunctionType
ALU = mybir.AluOpType
AX = mybir.AxisListType


class Tk:
    __slots__ = ("name", "w", "r")

    def __init__(self, name=""):
        self.name = name
        self.w = None
        self.r = {}


class Sched:
    SEM_ROLL = 30000
    NDMA = 8

    def __init__(self, nc, stack):
        self.nc = nc
        self.stack = stack
        self.engs = {"pe": nc.tensor, "dve": nc.vector, "act": nc.scalar,
                     "pool": nc.gpsimd, "sp": nc.sync}
        self.sems = []
        self.owner = []
        self.esem = {}
        self.ecnt = {}
        self.seen = {e: {} for e in self.engs}
        for e in self.engs:
            self._new_esem(e)
        self.dq = {}
        for q in ("sp", "pool", "act"):
            ids = [self._alloc_sem(f"dma_{q}_{i}", None) for i in range(self.NDMA)]
            self.dq[q] = {"ids": ids, "uses": [0] * self.NDMA, "next": 0}
        self.n_inst = 0
        self.n_wait = 0

    def _alloc_sem(self, name, owner):
        h = self.stack.enter_context(self.nc.semaphore(name))
        self.sems.append(h)
        self.owner.append(owner)
        return len(self.sems) - 1

    def _new_esem(self, e):
        sid = self._alloc_sem(f"e_{e}_{len(self.sems)}", e)
        self.esem[e] = sid
        self.ecnt[e] = 0

    def _wait(self, eng, deps):
        e = self.engs[eng]
        seen = self.seen[eng]
        for sid, val in deps.items():
            if seen.get(sid, 0) >= val:
                continue
            e.wait_ge(self.sems[sid], val)
            self.n_wait += 1
            seen[sid] = val

    def _deps(self, eng, reads, writes):
        deps = {}

        def add(tok, raw):
            sid, val = tok
            if self.owner[sid] == eng and not raw:
                return
            if deps.get(sid, 0) < val:
                deps[sid] = val
        for t in reads:
            if t.w is not None:
                add(t.w, True)
        for t in writes:
            if t.w is not None:
                add(t.w, False)
            for sid, val in t.r.items():
                add((sid, val), False)
        return deps

    def _commit(self, tok, reads, writes):
        sid, val = tok
        for t in reads:
            if t.r.get(sid, 0) < val:
                t.r[sid] = val
        for t in writes:
            t.w = tok
            t.r = {}

    def op(self, eng, fn, reads=(), writes=()):
        if self.ecnt[eng] >= self.SEM_ROLL:
            self._new_esem(eng)
        self._wait(eng, self._deps(eng, reads, writes))
        ins = fn(self.engs[eng])
        self.ecnt[eng] += 1
        ins.then_inc(self.sems[self.esem[eng]], 1)
        tok = (self.esem[eng], self.ecnt[eng])
        self._commit(tok, reads, writes)
        self.n_inst += 1
        return tok

    def dma(self, q, fn, reads=(), writes=()):
        d = self.dq[q]
        i = d["next"]
        d["next"] = (i + 1) % self.NDMA
        sid = d["ids"][i]
        deps = self._deps(q, reads, writes)
        if d["uses"][i] > 0:
            v = 16 * d["uses"][i]
            if deps.get(sid, 0) < v:
                deps[sid] = v
        self._wait(q, deps)
        ins = fn(self.engs[q])
        d["uses"][i] += 1
        ins.then_inc(self.sems[sid], 16)
        tok = (sid, 16 * d["uses"][i])
        self._commit(tok, reads, writes)
        self.n_inst += 1
        return tok

    def barrier(self):
        deps = {}
        for e in self.engs:
            if self.ecnt[e] > 0:
                deps[self.esem[e]] = self.ecnt[e]
        for q, d in self.dq.items():
            for i, sid in enumerate(d["ids"]):
                if d["uses"][i] > 0:
                    deps[sid] = 16 * d["uses"][i]
        for e in self.engs:
            dd = {s: v for s, v in deps.items() if self.owner[s] != e}
            self._wait(e, dd)

    def final_wait(self, toks, eng="sp"):
        deps = {}
        for sid, val in toks:
            if deps.get(sid, 0) < val:
                deps[sid] = val
        self._wait(eng, deps)


D = 1024
NTOK = 4096
SEQ = 2048
NT = NTOK // 128
NE = 32
CAP = 1024
NBLK = CAP // 128
ALPHA_DN = 4.0 ** 0.25
LN_EPS = 1e-5
SW_LIM = 7.0
SW_ALPHA = 1.702
BIG = 1.0e6


class B:
    def __init__(self, t):
        self.t = t
        self.k = Tk()

    def __getitem__(self, i):
        return self.t[i]


class K:
    def __init__(self, nc, S, dbg):
        self.nc = nc
        self.S = S
        self.dbg = dbg


_UID = [0]


def _sb(st, nc, name, shape, dt):
    _UID[0] += 1
    return B(st.enter_context(nc.sbuf_tensor(f"s{_UID[0]}_{name}", list(shape), dt)))


def _ps(st, nc, name, shape, dt=F32):
    _UID[0] += 1
    return B(st.enter_context(nc.psum_tensor(f"p{_UID[0]}_{name}", list(shape), dt)))


def build_program(upto=99, dbg=False, start=0, small_moe=False):
    nc = bass.Bass("TRN2", target_bir_lowering=False)
    dt_in = lambda n, s, d=F32: nc.dram_tensor(n, list(s), d, kind="ExternalInput").ap()
    I = {}
    I["x"] = dt_in("x", [NTOK, D])
    I["cT"] = dt_in("cT", [128, 8, 2])
    I["ada_w"] = dt_in("ada_w", [2, D, 6 * D] if start == 0 else [2, 8, 8])
    I["ada_b"] = dt_in("ada_b", [2, 6 * D])
    I["ln_g"] = dt_in("ln_g", [4, D])
    I["ln_b"] = dt_in("ln_b", [4, D])
    I["cv_w1"] = dt_in("cv_w1", [D, 2 * D])
    I["cv_b1T"] = dt_in("cv_b1T", [128, 16])
    I["cv_dwT"] = dt_in("cv_dwT", [128, 8, 31])
    I["cv_vecT"] = dt_in("cv_vecT", [128, 3, 8])
    I["cv_w2"] = dt_in("cv_w2", [D, D])
    I["cv_b2"] = dt_in("cv_b2", [1, D])
    I["kv_w"] = dt_in("kv_w", [D, 2 * D])
    I["q_w"] = dt_in("q_w", [D, D])
    I["o_w"] = dt_in("o_w", [D, D])
    I["router_w"] = dt_in("router_w", [2, D, NE])
    I["router_b"] = dt_in("router_b", [2, NE])
    I["moe_w_in"] = dt_in("moe_w_in", [2, NE, D, 2 * D] if not small_moe else [2, NE, 8, 16])
    I["moe_b_in"] = dt_in("moe_b_in", [2, NE, 2 * D])
    I["moe_w_out"] = dt_in("moe_w_out", [2, NE, D, D] if not small_moe else [2, NE, 8, 8])
    I["moe_b_out"] = dt_in("moe_b_out", [2, NE, D])
    out = nc.dram_tensor("out", [NTOK, D], F32, kind="ExternalOutput").ap()
    scr = lambda n, s, d: nc.dram_tensor(n, list(s), d, kind="Internal").ap()
    Dr = {}
    Dr["modrow"] = scr("modrow", [2, 2, 6 * D], F32)
    Dr["gluT"] = scr("gluT", [8, 128, NTOK], BF16)
    Dr["xa"] = scr("xa", [NTOK, D], F32)
    Dr["xb"] = scr("xb", [NTOK, D], F32)
    Dr["Xe"] = scr("Xe", [NE * CAP, D], BF16)
    Dr["Y"] = scr("Y", [NE * CAP, D], BF16)
    Dr["kT"] = scr("kT", [8, 128, NTOK], BF16)
    Dr["qT"] = scr("qT", [8, 128, NTOK], BF16)
    Dr["v"] = scr("v", [NTOK, D], BF16)
    Dr["oT"] = scr("oT", [8, 128, NTOK], BF16)
    dbg_out = {}
    if dbg:
        dbg_out["d_mod"] = nc.dram_tensor("d_mod", [2, 2, 6 * D], F32, kind="ExternalOutput").ap()
        dbg_out["d_xa"] = nc.dram_tensor("d_xa", [NTOK, D], F32, kind="ExternalOutput").ap()
        dbg_out["d_lg"] = nc.dram_tensor("d_lg", [NTOK, NE], F32, kind="ExternalOutput").ap()
        dbg_out["d_sl"] = nc.dram_tensor("d_sl", [NTOK, 4], I32, kind="ExternalOutput").ap()
        dbg_out["d_gt"] = nc.dram_tensor("d_gt", [NTOK, 4], F32, kind="ExternalOutput").ap()
        if start == 6:
            dbg_out["d_kT"] = nc.dram_tensor("d_kT", [8, 128, NTOK], BF16, kind="ExternalOutput").ap()
            dbg_out["d_v"] = nc.dram_tensor("d_v", [NTOK, D], BF16, kind="ExternalOutput").ap()

    with ExitStack() as gst:
        S = Sched(nc, gst)
        G = {}
        G["ident_f"] = _sb(gst, nc, "ident_f", [128, 128], F32)
        G["ident_b"] = _sb(gst, nc, "ident_b", [128, 128], BF16)
        G["ones_f"] = _sb(gst, nc, "ones_f", [128, 128], F32)
        G["ones_b"] = _sb(gst, nc, "ones_b", [128, 128], BF16)
        G["triU"] = _sb(gst, nc, "triU", [128, 128], F32)
        G["eps"] = _sb(gst, nc, "eps", [128, 1], F32)
        G["one"] = _sb(gst, nc, "one", [128, 1], F32)
        G["slots"] = _sb(gst, nc, "slots", [128, NT, 4], I32)
        G["gates"] = _sb(gst, nc, "gates", [128, NT, 4], F32)
        G["gmat"] = _sb(gst, nc, "gmat", [128, NT, NE], F32)
        G["modT"] = _sb(gst, nc, "modT", [128, 4, 6, 8], F32)
        G["ecap"] = _sb(gst, nc, "ecap", [128, NE], F32)
        tmp = _sb(gst, nc, "tmp_iota", [128, 128], F32)
        G["iota"] = tmp
        S.op("pool", lambda e: e.iota(tmp[:], pattern=[[1, 128]], base=0, channel_multiplier=-1,
                                      allow_small_or_imprecise_dtypes=True), writes=[tmp.k])
        S.op("dve", lambda e: e.tensor_single_scalar(G["ident_f"][:], tmp[:], 0.0, ALU.is_equal),
             reads=[tmp.k], writes=[G["ident_f"].k])
        S.op("dve", lambda e: e.tensor_single_scalar(G["ident_b"][:], tmp[:], 0.0, ALU.is_equal),
             reads=[tmp.k], writes=[G["ident_b"].k])
        S.op("dve", lambda e: e.tensor_single_scalar(G["triU"][:], tmp[:], 0.0, ALU.is_gt),
             reads=[tmp.k], writes=[G["triU"].k])
        S.op("dve", lambda e: e.memset(G["ones_f"][:], 1.0), writes=[G["ones_f"].k])
        S.op("dve", lambda e: e.memset(G["ones_b"][:], 1.0), writes=[G["ones_b"].k])
        S.op("dve", lambda e: e.memset(G["eps"][:], LN_EPS), writes=[G["eps"].k])
        S.op("dve", lambda e: e.memset(G["one"][:], 1.0), writes=[G["one"].k])
        S.op("pool", lambda e: e.iota(G["ecap"][:], pattern=[[CAP, NE]], base=0, channel_multiplier=0,
                                      allow_small_or_imprecise_dtypes=True), writes=[G["ecap"].k])
        ctx = K(nc, S, dbg)
        ctx.I, ctx.Dr, ctx.G, ctx.out, ctx.dbg_out = I, Dr, G, out, dbg_out
        ctx.upto = upto
        ctx.bc_reg = nc.gpsimd.to_reg(NE * CAP - 1)
        for nm in ('slots', 'gates', 'gmat'):
            G[nm].kt = [Tk() for _ in range(NT)]

        phases = [
            ("mod", lambda: phase_mod(ctx)),
            ("glu", lambda: phase_glu(ctx)),
            ("conv", lambda: phase_conv(ctx)),
            ("front0", lambda: phase_front(ctx, 0)),
            ("experts0", lambda: phase_experts(ctx, 0)),
            ("combine0", lambda: phase_combine(ctx, 0)),
            ("kvq", lambda: phase_kvq(ctx)),
            ("attn", lambda: phase_attn(ctx)),
            ("oproj", lambda: phase_oproj(ctx)),
            ("front1", lambda: phase_front(ctx, 1)),
            ("experts1", lambda: phase_experts(ctx, 1)),
            ("combine1", lambda: phase_combine(ctx, 1)),
        ]
        if start > 0:
            din = nc.dram_tensor("d_in_x", [NTOK, D], F32, kind="ExternalInput").ap()
            dstn = "xb" if start in (6, 7, 8) else "xa"
            S.dma("sp", lambda e: e.dma_start(out=Dr[dstn], in_=din))
            dmod = nc.dram_tensor("d_in_mod", [2, 2, 6 * D], F32, kind="ExternalInput").ap()
            S.dma("sp", lambda e: e.dma_start(out=Dr["modrow"], in_=dmod))
            S.barrier()
            load_modT(ctx)
            S.barrier()
        for i, (name, fn) in enumerate(phases):
            if i > upto:
                break
            if i < start:
                continue
            fn()
            S.barrier()
        if dbg:
            dump_dbg(ctx)
            S.barrier()
    return nc


def dump_dbg(ctx):
    nc, S = ctx.nc, ctx.S
    if "d_kT" in ctx.dbg_out:
        S.dma("sp", lambda e: e.dma_start(out=ctx.dbg_out["d_kT"], in_=ctx.Dr["kT"]))
        S.dma("sp", lambda e: e.dma_start(out=ctx.dbg_out["d_v"], in_=ctx.Dr["v"]))
    S.dma("sp", lambda e: e.dma_start(out=ctx.dbg_out["d_mod"], in_=ctx.Dr["modrow"]))
    S.dma("sp", lambda e: e.dma_start(out=ctx.dbg_out["d_xa"], in_=ctx.Dr["xa"]))
    S.dma("sp", lambda e: e.dma_start(out=ctx.dbg_out["d_sl"].rearrange("(t p) k -> p t k", p=128),
                                      in_=ctx.G["slots"][:]), reads=ctx.G["slots"].kt)
    S.dma("sp", lambda e: e.dma_start(out=ctx.dbg_out["d_gt"].rearrange("(t p) k -> p t k", p=128),
                                      in_=ctx.G["gates"][:]), reads=ctx.G["gates"].kt)


def phase_mod(ctx):
    nc, S, I, Dr, G = ctx.nc, ctx.S, ctx.I, ctx.Dr, ctx.G
    with ExitStack() as st:
        cT = _sb(st, nc, "cT", [128, 8, 2], F32)
        caT = _sb(st, nc, "caT", [128, 8, 2], BF16)
        wb = [_sb(st, nc, f"adaw{i}", [128, 8, 3 * D], BF16) for i in range(2)]
        bb = _sb(st, nc, "adab", [1, 2, 6 * D], BF16)
        msb = _sb(st, nc, "modsb", [2, 2, 6 * D], F32)
        pp = [_ps(st, nc, f"pmod{i}", [128, 512]) for i in range(2)]
        S.dma("sp", lambda e: e.dma_start(out=cT[:], in_=I["cT"]), writes=[cT.k])
        for l in range(2):
            cast_load(S, lambda c0, c1, l=l: bb[0:1, l, c0:c1], lambda c0, c1, l=l: I["ada_b"][l:l + 1, c0:c1], 6 * D, bb.k)
        S.op("act", lambda e: e.activation(caT[:], cT[:], AF.Silu), reads=[cT.k], writes=[caT.k])
        it = 0
        for l in range(2):
            for hf in range(2):
                w = wb[it % 2]
                src = I["ada_w"][l, :, hf * 3 * D:(hf + 1) * 3 * D].rearrange("(k p) n -> p k n", p=128)
                for k in range(8):
                    cast_load(S, lambda c0, c1, k=k: w[:, k, c0:c1], lambda c0, c1, k=k: src[:, k, c0:c1], 3 * D, w.k, step=1536)
                for n in range(6):
                    p = pp[n % 2]
                    col = hf * 3 * D + n * 512
                    for k in range(8):
                        S.op("pe", lambda e, k=k: e.matmul(p[0:2, :], caT[:, k, :], w[:, k, n * 512:(n + 1) * 512],
                                                          start=(k == 0), stop=False),
                             reads=[caT.k, w.k], writes=[p.k])
                    S.op("pe", lambda e: e.matmul(p[0:2, :], G["ones_b"][0:1, 0:2], bb[0:1, l, col:col + 512],
                                                  start=False, stop=True),
                         reads=[G["ones_b"].k, bb.k], writes=[p.k])
                    S.op("act", lambda e: e.activation(msb[0:2, l, col:col + 512], p[0:2, :], AF.Copy),
                         reads=[p.k], writes=[msb.k])
                it += 1
        for l in range(2):
            S.dma("sp", lambda e, l=l: e.dma_start(out=Dr["modrow"][l, :, :], in_=msb[0:2, l, :]), reads=[msb.k])
    S.barrier()
    load_modT(ctx)


def load_modT(ctx):
    nc, S, I, Dr, G = ctx.nc, ctx.S, ctx.I, ctx.Dr, ctx.G
    for l in range(2):
        for b in range(2):
            for wch in range(6):
                S.dma("sp", lambda e, l=l, b=b, wch=wch: e.dma_start(
                    out=G["modT"][:, l * 2 + b, wch, :],
                    in_=Dr["modrow"][l, b, wch * D:(wch + 1) * D].rearrange("(k p) -> p k", p=128),
                    allow_slow_non_contiguous=True), writes=[G["modT"].k])


def load_bc(ctx, st, name, src_row):
    t = _sb(st, ctx.nc, name, [128, D], F32)
    ctx.S.dma("sp", lambda e: e.dma_start(out=t[:], in_=src_row.partition_broadcast(128)), writes=[t.k])
    return t


def add_one(ctx, t):
    ctx.S.op("pool", lambda e: e.tensor_scalar_add(t[:], t[:], 1.0), reads=[t.k], writes=[t.k])


def mod_scale_bias(ctx, st, l, w_sh, w_sc, name):
    nc, S, G = ctx.nc, ctx.S, ctx.G
    sc = _sb(st, nc, name + "_sc", [128, 2, 8], F32)
    sh = _sb(st, nc, name + "_sh", [128, 2, 8], F32)
    for b in range(2):
        S.op("dve", lambda e, b=b: e.tensor_scalar_add(sc[:, b, :], G["modT"][:, l * 2 + b, w_sc, :], 1.0),
             reads=[G["modT"].k], writes=[sc.k])
        S.op("dve", lambda e, b=b: e.tensor_copy(sh[:, b, :], G["modT"][:, l * 2 + b, w_sh, :]),
             reads=[G["modT"].k], writes=[sh.k])
    return sc, sh


def run_interleaved(gens, width):
    active = []
    it = iter(gens)
    more = True
    while True:
        while more and len(active) < width:
            try:
                active.append(next(it))
            except StopIteration:
                more = False
        if not active:
            break
        for g in list(active):
            try:
                next(g)
            except StopIteration:
                active.remove(g)


class Epi:
    def __init__(self, ctx, st, l, sub, gate_which):
        nc, S, I, Dr = ctx.nc, ctx.S, ctx.I, ctx.Dr
        self.ctx = ctx
        self.lng = load_bc(ctx, st, f"lng{l}{sub}", I["ln_g"][l * 2 + sub, :])
        self.lnb = load_bc(ctx, st, f"lnb{l}{sub}", I["ln_b"][l * 2 + sub, :])
        self.gate = []
        for b in range(2):
            g = load_bc(ctx, st, f"gate{l}{sub}{b}", Dr["modrow"][l, b, gate_which * D:(gate_which + 1) * D])
            add_one(ctx, g)
            self.gate.append(g)
        self.t1 = [_sb(st, nc, f"ep_t1_{i}", [128, D], F32) for i in range(2)]
        self.r = [_sb(st, nc, f"ep_r_{i}", [128, D], F32) for i in range(2)]
        self.xo = [_sb(st, nc, f"ep_xo_{i}", [128, D], F32) for i in range(2)]
        self.st6 = [_sb(st, nc, f"ep_st_{i}", [128, 2, 6], F32) for i in range(2)]
        self.mv = [_sb(st, nc, f"ep_mv_{i}", [128, 4], F32) for i in range(2)]
        self.n = 0

    def run_g(self, ys, x_t, b, dst):
        S = self.ctx.S
        i = self.n % 2
        self.n += 1
        t1, r, xo, st6, mv = self.t1[i], self.r[i], self.xo[i], self.st6[i], self.mv[i]
        g = self.gate[b]
        for h, (yb, yap) in enumerate(ys):
            S.op("dve", lambda e, h=h, yap=yap: e.tensor_tensor(t1[:, h * 512:(h + 1) * 512], yap,
                                                              g[:, h * 512:(h + 1) * 512], ALU.mult),
                 reads=[yb.k, g.k], writes=[t1.k])
        yield
        S.op("dve", lambda e: e.scalar_tensor_tensor(r[:], x_t[:], ALPHA_DN, t1[:], ALU.mult, ALU.add),
             reads=[x_t.k, t1.k], writes=[r.k])
        yield
        for h in range(2):
            S.op("dve", lambda e, h=h: e.bn_stats(st6[:, h, :], r[:, h * 512:(h + 1) * 512]),
                 reads=[r.k], writes=[st6.k])
        S.op("dve", lambda e: e.bn_aggr(mv[:, 0:2], st6[:].rearrange("p a b -> p (a b)")), reads=[st6.k], writes=[mv.k])
        yield
        S.op("act", lambda e: e.activation(mv[:, 2:3], mv[:, 1:2], AF.Sqrt, bias=self.ctx.G["eps"][:], scale=1.0),
             reads=[mv.k, self.ctx.G["eps"].k], writes=[mv.k])
        yield
        S.op("dve", lambda e: e.reciprocal(mv[:, 2:3], mv[:, 2:3]), reads=[mv.k], writes=[mv.k])
        S.op("dve", lambda e: e.tensor_scalar(mv[:, 3:4], mv[:, 0:1], -1.0, mv[:, 2:3], ALU.mult, ALU.mult),
             reads=[mv.k], writes=[mv.k])
        S.op("act", lambda e: e.activation(t1[:], r[:], AF.Identity, bias=mv[:, 3:4], scale=mv[:, 2:3]),
             reads=[r.k, mv.k], writes=[t1.k])
        yield
        S.op("pool", lambda e: e.tensor_tensor(xo[:], t1[:], self.lng[:], ALU.mult),
             reads=[t1.k, self.lng.k], writes=[xo.k])
        S.op("pool", lambda e: e.tensor_tensor(xo[:], xo[:], self.lnb[:], ALU.add),
             reads=[xo.k, self.lnb.k], writes=[xo.k])
        S.dma("sp", lambda e: e.dma_start(out=dst, in_=xo[:]), reads=[xo.k])
        yield

    def run(self, ys, x_t, b, dst):
        for _ in self.run_g(ys, x_t, b, dst):
            pass


def transpose_tile(ctx, src, psT, evac):
    S, G = ctx.S, ctx.G
    for k in range(8):
        p = psT[k // 4]
        S.op("pe", lambda e, k=k, p=p: e.transpose(p[:, (k % 4) * 128:(k % 4 + 1) * 128], src[:, k * 128:(k + 1) * 128],
                                                 G["ident_f"][:]),
             reads=[src.k, G["ident_f"].k], writes=[p.k])
    for k in range(8):
        p = psT[k // 4]
        evac(k, p, p[:, (k % 4) * 128:(k % 4 + 1) * 128])


def phase_glu(ctx):
    nc, S, I, Dr, G = ctx.nc, ctx.S, ctx.I, ctx.Dr, ctx.G
    with ExitStack() as st:
        w1 = _sb(st, nc, "w1", [128, 8, 2 * D], BF16)
        b1T = _sb(st, nc, "b1T", [128, 16], F32)
        sc, sh = mod_scale_bias(ctx, st, 0, 0, 1, "m1")
        xt = [_sb(st, nc, f"xt{i}", [128, D], F32) for i in range(3)]
        hT = [_sb(st, nc, f"hT{i}", [128, 8, 512], BF16) for i in range(2)]
        sig = [_sb(st, nc, f"sig{i}", [128, 512], F32) for i in range(2)]
        gl = [_sb(st, nc, f"gl{i}", [128, 8, 512], BF16) for i in range(2)]
        psT = [_ps(st, nc, f"psT{i}", [128, 512]) for i in range(2)]
        pa = [_ps(st, nc, f"pa{i}", [128, 512]) for i in range(2)]
        pg = [_ps(st, nc, f"pg{i}", [128, 512]) for i in range(2)]
        src = I["cv_w1"].rearrange("(k p) n -> p k n", p=128)
        for k in range(8):
            S.dma("pool", lambda e, k=k: e.dma_start(out=w1[:, k, :], in_=src[:, k, :]), writes=[w1.k])
        S.dma("sp", lambda e: e.dma_start(out=b1T[:], in_=I["cv_b1T"]), writes=[b1T.k])
        ti = 0
        for j in range(NTOK // 512):
            b = (j * 512) // SEQ
            h = hT[j % 2]
            for i in range(4):
                x_t = xt[ti % 3]
                ti += 1
                r0 = j * 512 + i * 128
                S.dma("sp", lambda e, x_t=x_t, r0=r0: e.dma_start(out=x_t[:], in_=I["x"][r0:r0 + 128, :]), writes=[x_t.k])
                transpose_tile(ctx, x_t, psT, lambda k, p, ap, i=i: S.op(
                    "act", lambda e: e.activation(h[:, k, i * 128:(i + 1) * 128], ap, AF.Identity,
                                                  bias=sh[:, b, k:k + 1], scale=sc[:, b, k:k + 1]),
                    reads=[p.k, sh.k, sc.k], writes=[h.k]))
            g_o = gl[j % 2]
            for m in range(8):
                a_p, g_p, sg = pa[m % 2], pg[m % 2], sig[m % 2]
                for k in range(8):
                    S.op("pe", lambda e, k=k: e.matmul(a_p[:], w1[:, k, m * 128:(m + 1) * 128], h[:, k, :],
                                                      start=(k == 0), stop=(k == 7)), reads=[w1.k, h.k], writes=[a_p.k])
                for k in range(8):
                    S.op("pe", lambda e, k=k: e.matmul(g_p[:], w1[:, k, D + m * 128:D + (m + 1) * 128], h[:, k, :],
                                                      start=(k == 0), stop=(k == 7)), reads=[w1.k, h.k], writes=[g_p.k])
                S.op("act", lambda e: e.activation(sg[:], g_p[:], AF.Sigmoid, bias=b1T[:, 8 + m:9 + m], scale=1.0),
                     reads=[g_p.k, b1T.k], writes=[sg.k])
                S.op("dve", lambda e: e.scalar_tensor_tensor(g_o[:, m, :], a_p[:], b1T[:, m:m + 1], sg[:], ALU.add, ALU.mult),
                     reads=[a_p.k, b1T.k, sg.k], writes=[g_o.k])
            S.dma("sp", lambda e, j=j: e.dma_start(out=Dr["gluT"][:, :, j * 512:(j + 1) * 512].rearrange("k p t -> p k t"),
                                                  in_=g_o[:]), reads=[g_o.k])


def phase_conv(ctx):
    nc, S, I, Dr, G = ctx.nc, ctx.S, ctx.I, ctx.Dr, ctx.G
    with ExitStack() as st:
        dwT = _sb(st, nc, "dwT", [128, 8, 31], F32)
        vecT = _sb(st, nc, "vecT", [128, 3, 8], F32)
        dg = _sb(st, nc, "dg", [128, 8 * 31, 128], BF16)
        w2 = _sb(st, nc, "w2", [128, 8, D], BF16)
        b2 = _sb(st, nc, "b2", [1, D], BF16)
        epi = Epi(ctx, st, 0, 0, 2)
        glb = [_sb(st, nc, f"glb{i}", [128, 8, 544], BF16) for i in range(2)]
        vb = _sb(st, nc, "vb", [128, 8, 512], BF16)
        vsq = [_sb(st, nc, f"vsq{i}", [128, 512], BF16) for i in range(2)]
        sT = _sb(st, nc, "sT", [128, 8, 512], BF16)
        mean = _sb(st, nc, "cmean", [128, 512], F32)
        msq = _sb(st, nc, "cmsq", [128, 512], F32)
        rstd = _sb(st, nc, "crstd", [128, 512], F32)
        nmr = _sb(st, nc, "cnmr", [128, 512], F32)
        zt = [_sb(st, nc, f"czt{i}", [128, 512], F32) for i in range(2)]
        xt = [_sb(st, nc, f"cxt{i}", [128, D], F32) for i in range(2)]
        pc = [_ps(st, nc, f"pc{i}", [128, 512]) for i in range(2)]
        ps1 = _ps(st, nc, "ps1", [128, 512])
        ps2 = _ps(st, nc, "ps2", [128, 512])
        py = [_ps(st, nc, f"py{i}", [128, 512]) for i in range(4)]
        S.dma("sp", lambda e: e.dma_start(out=dwT[:], in_=I["cv_dwT"]), writes=[dwT.k])
        S.dma("sp", lambda e: e.dma_start(out=vecT[:], in_=I["cv_vecT"]), writes=[vecT.k])
        S.dma("pool", lambda e: e.dma_start(out=b2[:], in_=I["cv_b2"]), writes=[b2.k])
        src = I["cv_w2"].rearrange("(k p) n -> p k n", p=128)
        for k in range(8):
            S.dma("pool", lambda e, k=k: e.dma_start(out=w2[:, k, :], in_=src[:, k, :]), writes=[w2.k])
        for c in range(8):
            for k in range(31):
                S.op("dve", lambda e, c=c, k=k: e.tensor_scalar(dg[:, c * 31 + k, :], G["ident_b"][:], dwT[:, c, k:k + 1], None,
                                                              ALU.mult),
                     reads=[G["ident_b"].k, dwT.k], writes=[dg.k])
        vb2 = [vb, _sb(st, nc, "vb_b", [128, 8, 512], BF16)]
        mean2 = [mean, _sb(st, nc, "cmean_b", [128, 512], F32)]
        rstd2 = [rstd, _sb(st, nc, "crstd_b", [128, 512], F32)]
        nmr2 = [nmr, _sb(st, nc, "cnmr_b", [128, 512], F32)]
        tcnt = [0]

        def conv_stage(j):
            t0 = j * 512
            g_in = glb[j % 2]
            vbj, meanj, rstdj, nmrj = vb2[j % 2], mean2[j % 2], rstd2[j % 2], nmr2[j % 2]
            if t0 % SEQ == 0:
                S.op("pool", lambda e: e.memset(g_in[:, :, 0:30], 0.0), writes=[g_in.k])
                S.dma("sp", lambda e: e.dma_start(out=g_in[:, :, 30:542],
                                                  in_=Dr["gluT"][:, :, t0:t0 + 512].rearrange("k p t -> p k t")),
                      writes=[g_in.k])
            else:
                S.dma("sp", lambda e: e.dma_start(out=g_in[:, :, 0:542],
                                                  in_=Dr["gluT"][:, :, t0 - 30:t0 + 512].rearrange("k p t -> p k t")),
                      writes=[g_in.k])
            for c in range(8):
                p = pc[c % 2]
                vq = vsq[c % 2]
                for k in range(31):
                    S.op("pe", lambda e, k=k: e.matmul(p[:], dg[:, c * 31 + k, :], g_in[:, c, k:k + 512],
                                                      start=(k == 0), stop=(k == 30)),
                         reads=[dg.k, g_in.k], writes=[p.k])
                S.op("act", lambda e: e.activation(vbj[:, c, :], p[:], AF.Identity, bias=vecT[:, 0, c:c + 1], scale=1.0),
                     reads=[p.k, vecT.k], writes=[vbj.k])
                S.op("act", lambda e: e.activation(vq[:], p[:], AF.Square, bias=vecT[:, 0, c:c + 1], scale=1.0),
                     reads=[p.k, vecT.k], writes=[vq.k])
                S.op("pe", lambda e: e.matmul(ps1[:], G["ones_b"][:], vbj[:, c, :], start=(c == 0), stop=(c == 7)),
                     reads=[G["ones_b"].k, vbj.k], writes=[ps1.k])
                S.op("pe", lambda e: e.matmul(ps2[:], G["ones_b"][:], vq[:], start=(c == 0), stop=(c == 7)),
                     reads=[G["ones_b"].k, vq.k], writes=[ps2.k])
            S.op("act", lambda e: e.activation(meanj[:], ps1[:], AF.Copy, scale=1.0 / D), reads=[ps1.k], writes=[meanj.k])
            S.op("act", lambda e: e.activation(msq[:], ps1[:], AF.Square, scale=1.0 / D), reads=[ps1.k], writes=[msq.k])
            S.op("dve", lambda e: e.scalar_tensor_tensor(rstdj[:], ps2[:], 1.0 / D, msq[:], ALU.mult, ALU.subtract),
                 reads=[ps2.k, msq.k], writes=[rstdj.k])
            S.op("act", lambda e: e.activation(rstdj[:], rstdj[:], AF.Sqrt, bias=G["eps"][:], scale=1.0),
                 reads=[rstdj.k, G["eps"].k], writes=[rstdj.k])
            S.op("dve", lambda e: e.reciprocal(rstdj[:], rstdj[:]), reads=[rstdj.k], writes=[rstdj.k])
            S.op("dve", lambda e: e.scalar_tensor_tensor(nmrj[:], meanj[:], -1.0, rstdj[:], ALU.mult, ALU.mult),
                 reads=[meanj.k, rstdj.k], writes=[nmrj.k])

        def rest_stage(j):
            b = (j * 512) // SEQ
            t0 = j * 512
            vbj, rstdj, nmrj = vb2[j % 2], rstd2[j % 2], nmr2[j % 2]
            for c in range(8):
                z = zt[c % 2]
                S.op("dve", lambda e: e.tensor_tensor(z[:], vbj[:, c, :], rstdj[:], ALU.mult), reads=[vbj.k, rstdj.k], writes=[z.k])
                S.op("pool", lambda e: e.tensor_tensor(z[:], z[:], nmrj[:], ALU.add), reads=[z.k, nmrj.k], writes=[z.k])
                S.op("act", lambda e: e.activation(sT[:, c, :], z[:], AF.Silu, bias=vecT[:, 2, c:c + 1],
                                                   scale=vecT[:, 1, c:c + 1]),
                     reads=[z.k, vecT.k], writes=[sT.k])
            for i in range(4):
                ti = tcnt[0]
                r0 = t0 + i * 128
                x_t = xt[ti % 2]
                S.dma("sp", lambda e: e.dma_start(out=x_t[:], in_=I["x"][r0:r0 + 128, :]), writes=[x_t.k])
                ys = []
                for n in range(2):
                    p = py[(ti % 2) * 2 + n]
                    for c in range(8):
                        S.op("pe", lambda e, c=c: e.matmul(p[:], sT[:, c, i * 128:(i + 1) * 128], w2[:, c, n * 512:(n + 1) * 512],
                                                          start=(c == 0), stop=False), reads=[sT.k, w2.k], writes=[p.k])
                    S.op("pe", lambda e: e.matmul(p[:], G["ones_b"][0:1, :], b2[0:1, n * 512:(n + 1) * 512], start=False, stop=True),
                         reads=[G["ones_b"].k, b2.k], writes=[p.k])
                    ys.append((p, p[:]))
                tcnt[0] += 1
                epi.run(ys, x_t, b, Dr["xa"][r0:r0 + 128, :])

        NBK = NTOK // 512
        conv_stage(0)
        for j in range(NBK):
            if j + 1 < NBK:
                conv_stage(j + 1)
            rest_stage(j)


def phase_front(ctx, l):
    nc, S, I, Dr, G = ctx.nc, ctx.S, ctx.I, ctx.Dr, ctx.G
    with ExitStack() as st:
        S2, H2 = [], []
        for b in range(2):
            s2 = load_bc(ctx, st, f"S2_{b}", Dr["modrow"][l, b, 4 * D:5 * D])
            add_one(ctx, s2)
            S2.append(s2)
            H2.append(load_bc(ctx, st, f"H2_{b}", Dr["modrow"][l, b, 3 * D:4 * D]))
        rw = _sb(st, nc, "rw", [128, 8, NE], F32)
        rb = _sb(st, nc, "rb", [1, NE], F32)
        srun = _sb(st, nc, "srun", [128, NE], F32)
        xt = [_sb(st, nc, f"fx{i}", [128, D], F32) for i in range(2)]
        h2 = [_sb(st, nc, f"fh{i}", [128, D], F32) for i in range(2)]
        h2b = [_sb(st, nc, f"fhb{i}", [128, D], BF16) for i in range(3)]
        h2T = [_sb(st, nc, f"fhT{i}", [128, 8, 128], F32) for i in range(2)]
        sm = lambda nm, w: [_sb(st, nc, f"{nm}{i}", [128, w], F32) for i in range(2)]
        lg, top8, nv0, ex, ssum, mask, slotv, bad, oh, junk, slotf, okk, g4, gtmp = (
            sm("lg", NE), sm("top8", 8), sm("nv0", 1), sm("ex", 4), sm("ssum", 1), sm("mask", NE), sm("slotv", NE),
            sm("bad", NE), sm("oh", NE), sm("junk", NE), sm("slotf", 4), sm("okk", 4), sm("g4", 4), sm("gtmp", NE))
        cur = sm("cur", NE)
        psT = [_ps(st, nc, f"fpT{i}", [128, 512]) for i in range(2)]
        plg = [_ps(st, nc, f"fplg{i}", [128, 512]) for i in range(2)]
        ppos = [_ps(st, nc, f"fpps{i}", [128, 512]) for i in range(2)]
        S.dma("sp", lambda e: e.dma_start(out=rw[:], in_=I["router_w"][l].rearrange("(k p) e -> p k e", p=128)), writes=[rw.k])
        S.dma("sp", lambda e: e.dma_start(out=rb[:], in_=I["router_b"][l:l + 1, :]), writes=[rb.k])
        S.op("dve", lambda e: e.memset(srun[:], 0.0), writes=[srun.k])
        def tile_g(t):
            b = t // 16
            i = t % 2
            x_t, h, hb, hT = xt[i], h2[i], h2b[t % 3], h2T[i]
            S.dma("sp", lambda e: e.dma_start(out=x_t[:], in_=Dr["xa"][t * 128:(t + 1) * 128, :]), writes=[x_t.k])
            S.op("dve", lambda e: e.tensor_tensor(h[:], x_t[:], S2[b][:], ALU.mult), reads=[x_t.k, S2[b].k], writes=[h.k])
            S.op("dve", lambda e: e.tensor_tensor(h[:], h[:], H2[b][:], ALU.add), reads=[h.k, H2[b].k], writes=[h.k])
            S.op("act", lambda e: e.activation(hb[:], h[:], AF.Copy), reads=[h.k], writes=[hb.k])
            yield
            transpose_tile(ctx, h, psT, lambda k, p, ap: S.op(
                "act", lambda e: e.activation(hT[:, k, :], ap, AF.Copy), reads=[p.k], writes=[hT.k]))
            yield
            pl = plg[i]
            for k in range(8):
                S.op("pe", lambda e, k=k: e.matmul(pl[:, 0:NE], hT[:, k, :], rw[:, k, :], start=(k == 0), stop=False),
                     reads=[hT.k, rw.k], writes=[pl.k])
            S.op("pe", lambda e: e.matmul(pl[:, 0:NE], G["ones_f"][0:1, :], rb[0:1, :], start=False, stop=True),
                 reads=[G["ones_f"].k, rb.k], writes=[pl.k])
            S.op("dve", lambda e: e.tensor_copy(lg[i][:], pl[:, 0:NE]), reads=[pl.k], writes=[lg[i].k])
            yield
            S.op("dve", lambda e: e.tensor_copy(cur[i][:], lg[i][:]), reads=[lg[i].k], writes=[cur[i].k])
            yield
            for k in range(4):
                S.op("dve", lambda e, k=k: e.tensor_reduce(top8[i][:, k:k + 1], cur[i][:], AX.X, ALU.max),
                     reads=[cur[i].k], writes=[top8[i].k])
                if k < 3:
                    S.op("dve", lambda e, k=k: e.tensor_scalar(oh[i][:], cur[i][:], top8[i][:, k:k + 1], None, ALU.is_equal),
                         reads=[cur[i].k, top8[i].k], writes=[oh[i].k])
                    S.op("dve", lambda e: e.scalar_tensor_tensor(cur[i][:], oh[i][:], -BIG, cur[i][:], ALU.mult, ALU.add),
                         reads=[oh[i].k, cur[i].k], writes=[cur[i].k])
                yield
            S.op("dve", lambda e: e.tensor_scalar_mul(nv0[i][:], top8[i][:, 0:1], -1.0), reads=[top8[i].k], writes=[nv0[i].k])
            S.op("act", lambda e: e.activation(ex[i][:], top8[i][:, 0:4], AF.Exp, bias=nv0[i][:], scale=1.0),
                 reads=[top8[i].k, nv0[i].k], writes=[ex[i].k])
            S.op("dve", lambda e: e.tensor_reduce(ssum[i][:], ex[i][:], AX.X, ALU.add), reads=[ex[i].k], writes=[ssum[i].k])
            S.op("dve", lambda e: e.reciprocal(ssum[i][:], ssum[i][:]), reads=[ssum[i].k], writes=[ssum[i].k])
            S.op("dve", lambda e: e.tensor_scalar(g4[i][:], ex[i][:], ssum[i][:], None, ALU.mult),
                 reads=[ex[i].k, ssum[i].k], writes=[g4[i].k])
            yield
            S.op("dve", lambda e: e.tensor_scalar(mask[i][:], lg[i][:], top8[i][:, 3:4], None, ALU.is_ge),
                 reads=[lg[i].k, top8[i].k], writes=[mask[i].k])
            pp = ppos[i]
            S.op("pe", lambda e: e.matmul(pp[:, 0:NE], G["triU"][:], mask[i][:], start=True, stop=False),
                 reads=[G["triU"].k, mask[i].k], writes=[pp.k])
            S.op("pe", lambda e: e.matmul(pp[:, 0:NE], G["ones_f"][:], srun[:], start=False, stop=True),
                 reads=[G["ones_f"].k, srun.k], writes=[pp.k])
            S.op("dve", lambda e: e.tensor_tensor(srun[:], srun[:], mask[i][:], ALU.add), reads=[srun.k, mask[i].k], writes=[srun.k])
            yield
            S.op("dve", lambda e: e.tensor_single_scalar(bad[i][:], pp[:, 0:NE], float(CAP) - 0.5, ALU.is_ge),
                 reads=[pp.k], writes=[bad[i].k])
            S.op("dve", lambda e: e.tensor_tensor(slotv[i][:], pp[:, 0:NE], G["ecap"][:], ALU.add),
                 reads=[pp.k, G["ecap"].k], writes=[slotv[i].k])
            S.op("dve", lambda e: e.scalar_tensor_tensor(slotv[i][:], bad[i][:], BIG, slotv[i][:], ALU.mult, ALU.add),
                 reads=[bad[i].k, slotv[i].k], writes=[slotv[i].k])
            yield
            for k in range(4):
                S.op("dve", lambda e, k=k: e.tensor_scalar(oh[i][:], lg[i][:], top8[i][:, k:k + 1], None, ALU.is_equal),
                     reads=[lg[i].k, top8[i].k], writes=[oh[i].k])
                S.op("dve", lambda e: e.tensor_tensor(junk[i][:], oh[i][:], slotv[i][:], ALU.mult),
                     reads=[oh[i].k, slotv[i].k], writes=[junk[i].k])
                S.op("dve", lambda e, k=k: e.tensor_reduce(slotf[i][:, k:k + 1], junk[i][:], AX.X, ALU.add),
                     reads=[junk[i].k], writes=[slotf[i].k])
                yield
            S.op("dve", lambda e: e.tensor_single_scalar(okk[i][:], slotf[i][:], 1.0e5, ALU.is_lt), reads=[slotf[i].k], writes=[okk[i].k])
            yield
            gk, sk, mk = G["gates"].kt[t], G["slots"].kt[t], G["gmat"].kt[t]
            S.op("dve", lambda e: e.tensor_tensor(G["gates"][:, t, :], g4[i][:], okk[i][:], ALU.mult),
                 reads=[g4[i].k, okk[i].k], writes=[gk])
            S.op("dve", lambda e: e.tensor_copy(G["slots"][:, t, :], slotf[i][:]), reads=[slotf[i].k], writes=[sk])
            yield
            for k in range(4):
                dstm = G["gmat"][:, t, :] if k == 0 else gtmp[i][:]
                S.op("dve", lambda e, k=k, dstm=dstm: e.tensor_scalar(dstm, lg[i][:], top8[i][:, k:k + 1], G["gates"][:, t, k:k + 1],
                                                                    ALU.is_equal, ALU.mult),
                     reads=[lg[i].k, top8[i].k, gk], writes=[mk if k == 0 else gtmp[i].k])
                if k > 0:
                    S.op("dve", lambda e: e.tensor_tensor(G["gmat"][:, t, :], G["gmat"][:, t, :], gtmp[i][:], ALU.add),
                         reads=[mk, gtmp[i].k], writes=[mk])
            for k in range(4):
                S.dma("pool", lambda e, k=k: e.indirect_dma_start(
                    out=Dr["Xe"], out_offset=bass.IndirectOffsetOnAxis(ap=G["slots"][:, t, k:k + 1], axis=0),
                    in_=hb[:], in_offset=None, bounds_check=ctx.bc_reg, oob_is_err=False), reads=[hb.k, sk])
            yield

        run_interleaved((tile_g(t) for t in range(NT)), 2)


def phase_experts(ctx, l):
    nc, S, I, Dr, G = ctx.nc, ctx.S, ctx.I, ctx.Dr, ctx.G
    with ExitStack() as st:
        win = [_sb(st, nc, f"win{i}", [128, 8, 2 * D], BF16) for i in range(2)]
        wout = [_sb(st, nc, f"wout{i}", [128, 8, D], BF16) for i in range(2)]
        bin_ = [_sb(st, nc, f"bin{i}", [1, 2 * D], BF16) for i in range(2)]
        xe = [_sb(st, nc, f"xe{i}", [128, D], BF16) for i in range(2)]
        xT = [_sb(st, nc, f"xT{i}", [128, 8, 128], BF16) for i in range(2)]
        xg = [_sb(st, nc, f"xg{i}", [128, 512], F32) for i in range(2)]
        sg = [_sb(st, nc, f"sg{i}", [128, 512], F32) for i in range(2)]
        xl = [_sb(st, nc, f"xl{i}", [128, 512], F32) for i in range(2)]
        tt = [_sb(st, nc, f"tt{i}", [128, 512], F32) for i in range(2)]
        act = [_sb(st, nc, f"act{i}", [128, D], BF16) for i in range(2)]
        actT = [_sb(st, nc, f"actT{i}", [128, 8, 128], BF16) for i in range(2)]
        yb = [_sb(st, nc, f"yb{i}", [128, D], BF16) for i in range(2)]
        psT = [_ps(st, nc, f"epT{i}", [128, 1024], BF16) for i in range(2)]
        pu = [_ps(st, nc, f"epu{i}", [128, 512]) for i in range(4)]
        py = [_ps(st, nc, f"epy{i}", [128, 512]) for i in range(2)]

        def load_w(e):
            w, wo, bi = win[e % 2], wout[e % 2], bin_[e % 2]
            s1 = I["moe_w_in"][l, e].rearrange("(k p) n -> p k n", p=128)
            s2 = I["moe_w_out"][l, e].rearrange("(k p) n -> p k n", p=128)
            for k in range(8):
                S.dma("pool", lambda q, k=k: q.dma_start(out=w[:, k, :], in_=s1[:, k, :]), writes=[w.k])
            for k in range(8):
                S.dma("pool", lambda q, k=k: q.dma_start(out=wo[:, k, :], in_=s2[:, k, :]), writes=[wo.k])
            S.dma("pool", lambda q: q.dma_start(out=bi[:], in_=I["moe_b_in"][l, e:e + 1, :]), writes=[bi.k])

        load_w(0)
        load_w(1)
        blocks = [(e_, j) for e_ in range(NE) for j in range(NBLK)]
        NB = len(blocks)
        xe4 = xe + [_sb(st, nc, f"xe{i}", [128, D], BF16) for i in range(2, 4)]

        def load_x(n):
            e_, j = blocks[n]
            r0 = e_ * CAP + j * 128
            x_e = xe4[n % 4]
            S.dma("sp", lambda q: q.dma_start(out=x_e[:], in_=Dr["Xe"][r0:r0 + 128, :]), writes=[x_e.k])

        def t_x(n):
            x_e, x_T = xe4[n % 4], xT[n % 2]
            pt = psT[0]
            for k in range(8):
                S.op("pe", lambda q, k=k: q.transpose(pt[:, k * 128:(k + 1) * 128], x_e[:, k * 128:(k + 1) * 128], G["ident_b"][:]),
                     reads=[x_e.k, G["ident_b"].k], writes=[pt.k])
            S.op("act", lambda q: q.activation(x_T[:].rearrange("p k t -> p (k t)"), pt[:], AF.Copy), reads=[pt.k], writes=[x_T.k])

        def mm1(n):
            e_, j = blocks[n]
            i = n % 2
            w, bi = win[e_ % 2], bin_[e_ % 2]
            x_T, a_ = xT[i], act[i]
            for hf in range(2):
                pg, pl = pu[hf * 2], pu[hf * 2 + 1]
                for (p, c0) in ((pg, hf * 512), (pl, D + hf * 512)):
                    for k in range(8):
                        S.op("pe", lambda q, k=k: q.matmul(p[:], x_T[:, k, :], w[:, k, c0:c0 + 512], start=(k == 0), stop=False),
                             reads=[x_T.k, w.k], writes=[p.k])
                    S.op("pe", lambda q: q.matmul(p[:], G["ones_b"][0:1, :], bi[0:1, c0:c0 + 512], start=False, stop=True),
                         reads=[G["ones_b"].k, bi.k], writes=[p.k])
                S.op("dve", lambda q: q.tensor_scalar_min(xg[hf][:], pg[:], SW_LIM), reads=[pg.k], writes=[xg[hf].k])
                S.op("act", lambda q: q.activation(sg[hf][:], xg[hf][:], AF.Sigmoid, scale=SW_ALPHA), reads=[xg[hf].k], writes=[sg[hf].k])
                S.op("dve", lambda q: q.tensor_scalar(xl[hf][:], pl[:], SW_LIM, -SW_LIM, ALU.min, ALU.max), reads=[pl.k], writes=[xl[hf].k])
                S.op("dve", lambda q: q.scalar_tensor_tensor(tt[hf][:], xl[hf][:], 1.0, xg[hf][:], ALU.add, ALU.mult),
                     reads=[xl[hf].k, xg[hf].k], writes=[tt[hf].k])
                S.op("dve", lambda q: q.tensor_tensor(a_[:, hf * 512:(hf + 1) * 512], tt[hf][:], sg[hf][:], ALU.mult),
                     reads=[tt[hf].k, sg[hf].k], writes=[a_.k])

        def t_a(n):
            i = n % 2
            a_, a_T = act[i], actT[i]
            pt2 = psT[1]
            for k in range(8):
                S.op("pe", lambda q, k=k: q.transpose(pt2[:, k * 128:(k + 1) * 128], a_[:, k * 128:(k + 1) * 128], G["ident_b"][:]),
                     reads=[a_.k, G["ident_b"].k], writes=[pt2.k])
            S.op("act", lambda q: q.activation(a_T[:].rearrange("p k t -> p (k t)"), pt2[:], AF.Copy), reads=[pt2.k], writes=[a_T.k])

        def mm2(n):
            e_, j = blocks[n]
            i = n % 2
            wo = wout[e_ % 2]
            r0 = e_ * CAP + j * 128
            a_T, y_ = actT[i], yb[i]
            for n2 in range(2):
                for k in range(8):
                    S.op("pe", lambda q, k=k: q.matmul(py[n2][:], a_T[:, k, :], wo[:, k, n2 * 512:(n2 + 1) * 512], start=(k == 0), stop=(k == 7)),
                         reads=[a_T.k, wo.k], writes=[py[n2].k])
                S.op("act", lambda q: q.activation(y_[:, n2 * 512:(n2 + 1) * 512], py[n2][:], AF.Copy), reads=[py[n2].k], writes=[y_.k])
            S.dma("act", lambda q: q.dma_start(out=Dr["Y"][r0:r0 + 128, :], in_=y_[:]), reads=[y_.k])

        for n in range(min(4, NB)):
            load_x(n)
        t_x(0)
        mm1(0)
        t_x(1)
        mm1(1)
        for m in range(NB):
            t_a(m)
            if m + 2 < NB:
                t_x(m + 2)
            mm2(m)
            if m + 4 < NB:
                load_x(m + 4)
            e_, j = blocks[m]
            if j == NBLK - 1 and e_ + 2 < NE:
                load_w(e_ + 2)
            if m + 2 < NB:
                mm1(m + 2)


def phase_combine(ctx, l):
    nc, S, I, Dr, G = ctx.nc, ctx.S, ctx.I, ctx.Dr, ctx.G
    final = (l == 1) or (ctx.upto == 5)
    with ExitStack() as st:
        epi = Epi(ctx, st, l, 1, 5)
        bo = _sb(st, nc, "bo", [NE, D], F32)
        yk = [[_sb(st, nc, f"yk{k}_{i}", [128, D], BF16) for i in range(4)] for k in range(4)]
        gmT = [_sb(st, nc, f"gmT{i}", [NE, 128], F32) for i in range(2)]
        acc = [_sb(st, nc, f"acc{i}", [128, D], F32) for i in range(2)]
        xt = [_sb(st, nc, f"cx{i}", [128, D], F32) for i in range(2)]
        pT = [_ps(st, nc, f"cpT{i}", [128, 512]) for i in range(2)]
        pb = [_ps(st, nc, f"cpb{i}", [128, 512]) for i in range(4)]
        S.dma("sp", lambda e: e.dma_start(out=bo[:], in_=I["moe_b_out"][l]), writes=[bo.k])
        for k in range(4):
            for i in range(4):
                S.op("pool", lambda e: e.memset(yk[k][i][:], 0.0), writes=[yk[k][i].k])

        def gathers(t):
            for k in range(4):
                S.dma("pool", lambda e, k=k: e.indirect_dma_start(
                    out=yk[k][t % 4][:], out_offset=None, in_=Dr["Y"],
                    in_offset=bass.IndirectOffsetOnAxis(ap=G["slots"][:, t, k:k + 1], axis=0),
                    bounds_check=ctx.bc_reg, oob_is_err=False), reads=[G["slots"].kt[t]], writes=[yk[k][t % 4].k])

        gathers(0)
        gathers(1)

        def tile_g(t):
            b = t // 16
            i = t % 2
            x_t = xt[i]
            S.dma("sp", lambda e: e.dma_start(out=x_t[:], in_=Dr["xa"][t * 128:(t + 1) * 128, :]), writes=[x_t.k])
            if t + 2 < NT:
                gathers(t + 2)
            S.op("pe", lambda e: e.transpose(pT[i][0:NE, 0:128], G["gmat"][:, t, :], G["ident_f"][:]),
                 reads=[G["gmat"].kt[t], G["ident_f"].k], writes=[pT[i].k])
            S.op("act", lambda e: e.activation(gmT[i][:], pT[i][0:NE, 0:128], AF.Copy), reads=[pT[i].k], writes=[gmT[i].k])
            yield
            a = acc[i]
            for n in range(2):
                p = pb[i * 2 + n]
                S.op("pe", lambda e: e.matmul(p[:], gmT[i][:], bo[:, n * 512:(n + 1) * 512], start=True, stop=True),
                     reads=[gmT[i].k, bo.k], writes=[p.k])
                S.op("dve", lambda e: e.scalar_tensor_tensor(a[:, n * 512:(n + 1) * 512], yk[0][t % 4][:, n * 512:(n + 1) * 512],
                                                             G["gates"][:, t, 0:1], p[:], ALU.mult, ALU.add),
                     reads=[yk[0][t % 4].k, G["gates"].kt[t], p.k], writes=[a.k])
                yield
            for k in range(1, 4):
                S.op("dve", lambda e, k=k: e.scalar_tensor_tensor(a[:], yk[k][t % 4][:], G["gates"][:, t, k:k + 1], a[:], ALU.mult, ALU.add),
                     reads=[yk[k][t % 4].k, G["gates"].kt[t], a.k], writes=[a.k])
                yield
            dst = (ctx.out if final else Dr["xb"])[t * 128:(t + 1) * 128, :]
            yield from epi.run_g([(a, a[:, 0:512]), (a, a[:, 512:1024])], x_t, b, dst)

        run_interleaved((tile_g(t) for t in range(NT)), 2)


def phase_kvq(ctx):
    nc, S, I, Dr, G = ctx.nc, ctx.S, ctx.I, ctx.Dr, ctx.G
    with ExitStack() as st:
        kvw = _sb(st, nc, "kvw", [128, 8, 2 * D], BF16)
        qw = _sb(st, nc, "qw", [128, 8, D], BF16)
        sc, sh = mod_scale_bias(ctx, st, 1, 0, 1, "m1b")
        xt = [_sb(st, nc, f"kx{i}", [128, D], F32) for i in range(3)]
        x2T = [_sb(st, nc, f"kxT{i}", [128, 8, 512], BF16) for i in range(2)]
        hT = [_sb(st, nc, f"khT{i}", [128, 8, 512], BF16) for i in range(2)]
        ko = [_sb(st, nc, f"ko{i}", [128, 8, 512], BF16) for i in range(2)]
        qo = [_sb(st, nc, f"qo{i}", [128, 8, 512], BF16) for i in range(2)]
        vo = [_sb(st, nc, f"vo{i}", [128, D], BF16) for i in range(2)]
        psT = [_ps(st, nc, f"kpT{i}", [128, 512]) for i in range(2)]
        pk = [_ps(st, nc, f"kpk{i}", [128, 512]) for i in range(2)]
        pq = [_ps(st, nc, f"kpq{i}", [128, 512]) for i in range(2)]
        pv = [_ps(st, nc, f"kpv{i}", [128, 512]) for i in range(2)]
        s1 = I["kv_w"].rearrange("(k p) n -> p k n", p=128)
        s2 = I["q_w"].rearrange("(k p) n -> p k n", p=128)
        for k in range(8):
            S.dma("pool", lambda e, k=k: e.dma_start(out=kvw[:, k, :], in_=s1[:, k, :]), writes=[kvw.k])
            S.dma("pool", lambda e, k=k: e.dma_start(out=qw[:, k, :], in_=s2[:, k, :]), writes=[qw.k])
        ti = 0
        vi = 0
        for j in range(NTOK // 512):
            b = (j * 512) // SEQ
            xT_, h = x2T[j % 2], hT[j % 2]
            for i in range(4):
                x_t = xt[ti % 3]
                ti += 1
                r0 = j * 512 + i * 128
                S.dma("sp", lambda e: e.dma_start(out=x_t[:], in_=Dr["xb"][r0:r0 + 128, :]), writes=[x_t.k])

                def ev(k, p, ap, i=i):
                    S.op("act", lambda e: e.activation(xT_[:, k, i * 128:(i + 1) * 128], ap, AF.Copy), reads=[p.k], writes=[xT_.k])
                    S.op("act", lambda e: e.activation(h[:, k, i * 128:(i + 1) * 128], ap, AF.Identity,
                                                       bias=sh[:, b, k:k + 1], scale=sc[:, b, k:k + 1]),
                         reads=[p.k, sh.k, sc.k], writes=[h.k])
                transpose_tile(ctx, x_t, psT, ev)
            import os
            part = int(os.environ.get("KVQ_PART", "9"))
            if part < 2:
                continue
            k_o, q_o = ko[j % 2], qo[j % 2]
            for m in range(8):
                p1, p2 = pk[m % 2], pq[m % 2]
                for k in range(8):
                    S.op("pe", lambda e, k=k: e.matmul(p1[:], kvw[:, k, m * 128:(m + 1) * 128], xT_[:, k, :], start=(k == 0), stop=(k == 7)),
                         reads=[kvw.k, xT_.k], writes=[p1.k])
                S.op("act", lambda e: e.activation(k_o[:, m, :], p1[:], AF.Copy), reads=[p1.k], writes=[k_o.k])
                for k in range(8):
                    S.op("pe", lambda e, k=k: e.matmul(p2[:], qw[:, k, m * 128:(m + 1) * 128], h[:, k, :], start=(k == 0), stop=(k == 7)),
                         reads=[qw.k, h.k], writes=[p2.k])
                S.op("act", lambda e: e.activation(q_o[:, m, :], p2[:], AF.Copy, scale=0.125), reads=[p2.k], writes=[q_o.k])
            S.dma("sp", lambda e: e.dma_start(out=Dr["kT"][:, :, j * 512:(j + 1) * 512].rearrange("k p t -> p k t"), in_=k_o[:]), reads=[k_o.k])
            S.dma("sp", lambda e: e.dma_start(out=Dr["qT"][:, :, j * 512:(j + 1) * 512].rearrange("k p t -> p k t"), in_=q_o[:]), reads=[q_o.k])
            if part < 3:
                continue
            for i in range(4):
                r0 = j * 512 + i * 128
                v_o = vo[vi % 2]
                vi += 1
                for n in range(2):
                    p = pv[n]
                    for k in range(8):
                        S.op("pe", lambda e, k=k: e.matmul(p[:], xT_[:, k, i * 128:(i + 1) * 128], kvw[:, k, D + n * 512:D + (n + 1) * 512],
                                                          start=(k == 0), stop=(k == 7)), reads=[xT_.k, kvw.k], writes=[p.k])
                    S.op("act", lambda e: e.activation(v_o[:, n * 512:(n + 1) * 512], p[:], AF.Copy), reads=[p.k], writes=[v_o.k])
                S.dma("sp", lambda e: e.dma_start(out=Dr["v"][r0:r0 + 128, :], in_=v_o[:]), reads=[v_o.k])


def phase_attn(ctx):
    nc, S, I, Dr, G = ctx.nc, ctx.S, ctx.I, ctx.Dr, ctx.G
    with ExitStack() as st:
        kT = _sb(st, nc, "akT", [128, 8, SEQ], BF16)
        qT = _sb(st, nc, "aqT", [128, 8, SEQ], BF16)
        v = _sb(st, nc, "av", [128, 16, D], BF16)
        oT = _sb(st, nc, "aoT", [128, 8, SEQ], BF16)
        ntri = _sb(st, nc, "ntri", [128, 128], BF16)
        nones = _sb(st, nc, "nones", [128, 128], BF16)
        mtmp = _sb(st, nc, "mtmp", [128, 512], F32)
        masks = [_sb(st, nc, f"amask{r}", [128, 512], BF16) for r in range(4)]
        e_sb = [_sb(st, nc, f"ae{i}", [128, 512], F32) for i in range(2)]
        sp = [[_sb(st, nc, f"asp{s_}{i}", [128, 512], BF16) for i in range(2)] for s_ in range(2)]
        a_sb = [[_sb(st, nc, f"aa{s_}{i}", [128, 512], BF16) for i in range(2)] for s_ in range(2)]
        acc = [[_sb(st, nc, f"aacc{s_}{i}", [128, 512], BF16) for i in range(2)] for s_ in range(2)]
        pz = [[_ps(st, nc, f"apz{s_}{i}", [128, 512]) for i in range(2)] for s_ in range(2)]
        pw = [_ps(st, nc, f"apw{i}", [128, 512]) for i in range(2)]
        po = [_ps(st, nc, f"apo{i}", [128, 512]) for i in range(2)]
        S.op("dve", lambda e: e.tensor_scalar(ntri[:], G["iota"][:], 0.0, -1.0, ALU.is_le, ALU.mult), reads=[G["iota"].k], writes=[ntri.k])
        S.op("dve", lambda e: e.memset(nones[:], -1.0), writes=[nones.k])
        for r in range(4):
            S.op("pool", lambda e, r=r: e.iota(mtmp[:], pattern=[[1, 512]], base=-128 * r, channel_multiplier=-1,
                                              allow_small_or_imprecise_dtypes=True), writes=[mtmp.k])
            S.op("dve", lambda e, r=r: e.tensor_single_scalar(masks[r][:], mtmp[:], 0.0, ALU.is_gt), reads=[mtmp.k], writes=[masks[r].k])
        it = 0
        for b in range(2):
            t0 = b * SEQ
            S.dma("sp", lambda e: e.dma_start(out=kT[:], in_=Dr["kT"][:, :, t0:t0 + SEQ].rearrange("k p t -> p k t")), writes=[kT.k])
            S.dma("sp", lambda e: e.dma_start(out=qT[:], in_=Dr["qT"][:, :, t0:t0 + SEQ].rearrange("k p t -> p k t")), writes=[qT.k])
            S.dma("sp", lambda e: e.dma_start(out=v[:], in_=Dr["v"][t0:t0 + SEQ, :].rearrange("(s p) d -> p s d", p=128)), writes=[v.k])
            for ch in range(8):
                pbs = [0, 64]
                for c in range(4):
                    nkb = 4 * c + 4
                    kbs = list(range(nkb - 1, -1, -1))
                    q_ap = [qT[pb:pb + 64, ch, c * 512:(c + 1) * 512] for pb in pbs]

                    def k_ap(kb, s_):
                        return kT[pbs[s_]:pbs[s_] + 64, ch, kb * 128:(kb + 1) * 128]

                    def stP(idx):
                        kb = kbs[idx]
                        r = kb - 4 * c
                        i2 = idx % 2
                        for s_ in (0, 1):
                            z = pz[s_][i2]
                            S.op("pe", lambda q: q.matmul(z[:], k_ap(kb, s_), q_ap[s_], start=True, stop=True), reads=[kT.k, qT.k], writes=[z.k])
                        for s_ in (0, 1):
                            z = pz[s_][i2]
                            S.op("act", lambda q: q.activation(e_sb[s_][:], z[:], AF.Exp), reads=[z.k], writes=[e_sb[s_].k])
                            S.op("act", lambda q: q.activation(sp[s_][i2][:], e_sb[s_][:], AF.Ln, bias=G["one"][:], scale=1.0),
                                 reads=[e_sb[s_].k, G["one"].k], writes=[sp[s_][i2].k])
                        if r >= 0:
                            for s_ in (0, 1):
                                S.op("dve", lambda q: q.tensor_tensor(sp[s_][i2][:], sp[s_][i2][:], masks[r][:], ALU.mult),
                                     reads=[sp[s_][i2].k, masks[r].k], writes=[sp[s_][i2].k])

                    def stQ(idx):
                        kb = kbs[idx]
                        r = kb - 4 * c
                        i2 = idx % 2
                        for s_ in (0, 1):
                            w = pw[s_]
                            S.op("pe", lambda q: q.matmul(w[:], k_ap(kb, s_), q_ap[s_], start=True, stop=False), reads=[kT.k, qT.k], writes=[w.k])
                            S.op("pe", lambda q: q.matmul(w[:], ntri[:], sp[s_][i2][:], start=False, stop=(idx == 0)),
                                 reads=[ntri.k, sp[s_][i2].k], writes=[w.k])
                            if idx > 0:
                                ac = acc[s_][(idx - 1) % 2]
                                S.op("pe", lambda q: q.matmul(w[:], nones[:], ac[:], start=False, stop=True), reads=[nones.k, ac.k], writes=[w.k])
                        for s_ in (0, 1):
                            S.op("act", lambda q: q.activation(a_sb[s_][i2][:], pw[s_][:], AF.Exp), reads=[pw[s_].k], writes=[a_sb[s_][i2].k])
                        if r >= 0:
                            for s_ in (0, 1):
                                S.op("dve", lambda q: q.tensor_tensor(a_sb[s_][i2][:], a_sb[s_][i2][:], masks[r][:], ALU.mult),
                                     reads=[a_sb[s_][i2].k, masks[r].k], writes=[a_sb[s_][i2].k])
                        for s_ in (0, 1):
                            h = 2 * ch + s_
                            S.op("pe", lambda q: q.matmul(po[s_][pbs[s_]:pbs[s_] + 64, :], v[:, kb, h * 64:(h + 1) * 64], a_sb[s_][i2][:],
                                                          start=(idx == 0), stop=(idx == nkb - 1)),
                                 reads=[v.k, a_sb[s_][i2].k], writes=[po[s_].k])
                        if idx < nkb - 1:
                            for s_ in (0, 1):
                                an = acc[s_][idx % 2]
                                if idx == 0:
                                    S.op("pool", lambda q: q.tensor_copy(an[:], sp[s_][i2][:]), reads=[sp[s_][i2].k], writes=[an.k])
                                else:
                                    S.op("pool", lambda q: q.tensor_tensor(an[:], acc[s_][(idx - 1) % 2][:], sp[s_][i2][:], ALU.add),
                                         reads=[acc[s_][(idx - 1) % 2].k, sp[s_][i2].k], writes=[an.k])

                    stP(0)
                    for idx in range(nkb):
                        if idx + 1 < nkb:
                            stP(idx + 1)
                        stQ(idx)
                    for s_ in (0, 1):
                        pb = 64 * s_
                        S.op("act", lambda q: q.activation(oT[pb:pb + 64, ch, c * 512:(c + 1) * 512], po[s_][pb:pb + 64, :], AF.Copy),
                             reads=[po[s_].k], writes=[oT.k])
            S.dma("sp", lambda e: e.dma_start(out=Dr["oT"][:, :, t0:t0 + SEQ].rearrange("k p t -> p k t"), in_=oT[:]), reads=[oT.k])


def phase_oproj(ctx):
    nc, S, I, Dr, G = ctx.nc, ctx.S, ctx.I, ctx.Dr, ctx.G
    with ExitStack() as st:
        ow = _sb(st, nc, "ow", [128, 8, D], BF16)
        epi = Epi(ctx, st, 1, 0, 2)
        oTb = [_sb(st, nc, f"ooT{i}", [128, 8, 512], BF16) for i in range(2)]
        xt = [_sb(st, nc, f"ox{i}", [128, D], F32) for i in range(2)]
        py = [_ps(st, nc, f"opy{i}", [128, 512]) for i in range(4)]
        s1 = I["o_w"].rearrange("(k p) n -> p k n", p=128)
        for k in range(8):
            S.dma("pool", lambda e, k=k: e.dma_start(out=ow[:, k, :], in_=s1[:, k, :]), writes=[ow.k])
        ti = 0
        for j in range(NTOK // 512):
            b = (j * 512) // SEQ
            o_b = oTb[j % 2]
            S.dma("sp", lambda e: e.dma_start(out=o_b[:], in_=Dr["oT"][:, :, j * 512:(j + 1) * 512].rearrange("k p t -> p k t")), writes=[o_b.k])
            for i in range(4):
                r0 = j * 512 + i * 128
                x_t = xt[ti % 2]
                S.dma("sp", lambda e: e.dma_start(out=x_t[:], in_=Dr["xb"][r0:r0 + 128, :]), writes=[x_t.k])
                ys = []
                for n in range(2):
                    p = py[(ti % 2) * 2 + n]
                    for c in range(8):
                        S.op("pe", lambda e, c=c: e.matmul(p[:], o_b[:, c, i * 128:(i + 1) * 128], ow[:, c, n * 512:(n + 1) * 512],
                                                          start=(c == 0), stop=(c == 7)), reads=[o_b.k, ow.k], writes=[p.k])
                    ys.append((p, p[:]))
                ti += 1
                epi.run(ys, x_t, b, Dr["xa"][r0:r0 + 128, :])


def cast_load(S, dst_ap_fn, src_ap_fn, ncols, wkey, q="pool", step=2048):
    for c0 in range(0, ncols, step):
        c1 = min(ncols, c0 + step)
        S.dma(q, lambda e, c0=c0, c1=c1: e.dma_start(out=dst_ap_fn(c0, c1), in_=src_ap_fn(c0, c1)), writes=[wkey])


def make_in_maps(inp, cores):
    f = lambda a: np.ascontiguousarray(a, dtype=np.float32)
    x, c = inp["x"], inp["c"]
    shared = {
        "ada_w": f(inp["ada_w"]), "ada_b": f(inp["ada_b"]),
        "ln_g": f(inp["ln_g"].reshape(4, D)), "ln_b": f(inp["ln_b"].reshape(4, D)),
        "cv_w1": f(inp["cv_w1"][0]),
        "cv_b1T": f(inp["cv_b1"][0].reshape(16, 128).T),
        "cv_dwT": f(inp["cv_dw"][0].reshape(31, 8, 128).transpose(2, 1, 0)),
        "cv_vecT": f(np.stack([inp["cv_db"][0].reshape(8, 128).T, inp["cv_ln_g"][0].reshape(8, 128).T,
                               inp["cv_ln_b"][0].reshape(8, 128).T], axis=1)),
        "cv_w2": f(inp["cv_w2"][0]), "cv_b2": f(inp["cv_b2"][0].reshape(1, D)),
        "kv_w": f(inp["kv_w"]), "q_w": f(inp["q_w"][0]), "o_w": f(inp["o_w"][0]),
        "router_w": f(inp["router_w"]), "router_b": f(inp["router_b"]),
        "moe_w_in": f(inp["moe_w_in"]), "moe_b_in": f(inp["moe_b_in"]),
        "moe_w_out": f(inp["moe_w_out"]), "moe_b_out": f(inp["moe_b_out"]),
    }
    maps = []
    for ci in cores:
        m = dict(shared)
        m["x"] = f(x[2 * ci:2 * ci + 2].reshape(NTOK, D))
        m["cT"] = f(c[2 * ci:2 * ci + 2].reshape(2, 8, 128).transpose(2, 1, 0))
        maps.append(m)
    return maps


def kernel(**inputs):
    inp = {k: np.asarray(v) for k, v in inputs.items()}
    nc = build_program()
    maps = make_in_maps(inp, list(range(8)))
    res = run_bass_kernel_spmd(nc, maps, core_ids=list(range(8)))
    outs = [r["out"].reshape(2, SEQ, D) for r in res.results]
    return np.concatenate(outs, axis=0).astype(np.float32)
```

```python
import numpy as np
from contextlib import ExitStack
import concourse.bass as bass
import concourse.mybir as mybir
from concourse.bass_utils import run_bass_kernel_spmd

F32 = mybir.dt.float32
BF16 = mybir.dt.bfloat16
I32 = mybir.dt.int32
U32 = mybir.dt.uint32
AF = mybir.ActivationFunctionType
ALU = mybir.AluOpType
AX = mybir.AxisListType


class Tk:
    __slots__ = ("name", "w", "r")

    def __init__(self, name=""):
        self.name = name
        self.w = None
        self.r = {}


class Sched:
    SEM_ROLL = 30000
    NDMA = 8

    def __init__(self, nc, stack):
        self.nc = nc
        self.stack = stack
        self.engs = {"pe": nc.tensor, "dve": nc.vector, "act": nc.scalar,
                     "pool": nc.gpsimd, "sp": nc.sync}
        self.sems = []
        self.owner = []
        self.esem = {}
        self.ecnt = {}
        self.seen = {e: {} for e in self.engs}
        for e in self.engs:
            self._new_esem(e)
        self.dq = {}
        for q in ("sp", "pool", "act"):
            ids = [self._alloc_sem(f"dma_{q}_{i}", None) for i in range(self.NDMA)]
            self.dq[q] = {"ids": ids, "uses": [0] * self.NDMA, "next": 0}
        self.n_inst = 0
        self.n_wait = 0

    def _alloc_sem(self, name, owner):
        h = self.stack.enter_context(self.nc.semaphore(name))
        self.sems.append(h)
        self.owner.append(owner)
        return len(self.sems) - 1

    def _new_esem(self, e):
        sid = self._alloc_sem(f"e_{e}_{len(self.sems)}", e)
        self.esem[e] = sid
        self.ecnt[e] = 0

    def _wait(self, eng, deps):
        e = self.engs[eng]
        seen = self.seen[eng]
        for sid, val in deps.items():
            if seen.get(sid, 0) >= val:
                continue
            e.wait_ge(self.sems[sid], val)
            self.n_wait += 1
            seen[sid] = val

    def _deps(self, eng, reads, writes):
        deps = {}

        def add(tok, raw):
            sid, val = tok
            if self.owner[sid] == eng and not raw:
                return
            if deps.get(sid, 0) < val:
                deps[sid] = val
        for t in reads:
            if t.w is not None:
                add(t.w, True)
        for t in writes:
            if t.w is not None:
                add(t.w, False)
            for sid, val in t.r.items():
                add((sid, val), False)
        return deps

    def _commit(self, tok, reads, writes):
        sid, val = tok
        for t in reads:
            if t.r.get(sid, 0) < val:
                t.r[sid] = val
        for t in writes:
            t.w = tok
            t.r = {}

    def op(self, eng, fn, reads=(), writes=()):
        if self.ecnt[eng] >= self.SEM_ROLL:
            self._new_esem(eng)
        self._wait(eng, self._deps(eng, reads, writes))
        ins = fn(self.engs[eng])
        self.ecnt[eng] += 1
        ins.then_inc(self.sems[self.esem[eng]], 1)
        tok = (self.esem[eng], self.ecnt[eng])
        self._commit(tok, reads, writes)
        self.n_inst += 1
        return tok

    def dma(self, q, fn, reads=(), writes=()):
        d = self.dq[q]
        i = d["next"]
        d["next"] = (i + 1) % self.NDMA
        sid = d["ids"][i]
        deps = self._deps(q, reads, writes)
        if d["uses"][i] > 0:
            v = 16 * d["uses"][i]
            if deps.get(sid, 0) < v:
                deps[sid] = v
        self._wait(q, deps)
        ins = fn(self.engs[q])
        d["uses"][i] += 1
        ins.then_inc(self.sems[sid], 16)
        tok = (sid, 16 * d["uses"][i])
        self._commit(tok, reads, writes)
        self.n_inst += 1
        return tok

    def barrier(self):
        deps = {}
        for e in self.engs:
            if self.ecnt[e] > 0:
                deps[self.esem[e]] = self.ecnt[e]
        for q, d in self.dq.items():
            for i, sid in enumerate(d["ids"]):
                if d["uses"][i] > 0:
                    deps[sid] = 16 * d["uses"][i]
        for e in self.engs:
            dd = {s: v for s, v in deps.items() if self.owner[s] != e}
            self._wait(e, dd)

    def final_wait(self, toks, eng="sp"):
        deps = {}
        for sid, val in toks:
            if deps.get(sid, 0) < val:
                deps[sid] = val
        self._wait(eng, deps)


D = 1024
NTOK = 4096
SEQ = 2048
NT = NTOK // 128
NE = 32
CAP = 1024
NBLK = CAP // 128
ALPHA_DN = 4.0 ** 0.25
LN_EPS = 1e-5
SW_LIM = 7.0
SW_ALPHA = 1.702
BIG = 1.0e6


class B:
    def __init__(self, t):
        self.t = t
        self.k = Tk()

    def __getitem__(self, i):
        return self.t[i]


class K:
    def __init__(self, nc, S, dbg):
        self.nc = nc
        self.S = S
        self.dbg = dbg


_UID = [0]


def _sb(st, nc, name, shape, dt):
    _UID[0] += 1
    return B(st.enter_context(nc.sbuf_tensor(f"s{_UID[0]}_{name}", list(shape), dt)))


def _ps(st, nc, name, shape, dt=F32):
    _UID[0] += 1
    return B(st.enter_context(nc.psum_tensor(f"p{_UID[0]}_{name}", list(shape), dt)))


def build_program(upto=99, dbg=False, start=0, small_moe=False):
    nc = bass.Bass("TRN2", target_bir_lowering=False)
    dt_in = lambda n, s, d=F32: nc.dram_tensor(n, list(s), d, kind="ExternalInput").ap()
    I = {}
    I["x"] = dt_in("x", [NTOK, D])
    I["cT"] = dt_in("cT", [128, 8, 2])
    I["ada_w"] = dt_in("ada_w", [2, D, 6 * D] if start == 0 else [2, 8, 8])
    I["ada_b"] = dt_in("ada_b", [2, 6 * D])
    I["ln_g"] = dt_in("ln_g", [4, D])
    I["ln_b"] = dt_in("ln_b", [4, D])
    I["cv_w1"] = dt_in("cv_w1", [D, 2 * D])
    I["cv_b1T"] = dt_in("cv_b1T", [128, 16])
    I["cv_dwT"] = dt_in("cv_dwT", [128, 8, 31])
    I["cv_vecT"] = dt_in("cv_vecT", [128, 3, 8])
    I["cv_w2"] = dt_in("cv_w2", [D, D])
    I["cv_b2"] = dt_in("cv_b2", [1, D])
    I["kv_w"] = dt_in("kv_w", [D, 2 * D])
    I["q_w"] = dt_in("q_w", [D, D])
    I["o_w"] = dt_in("o_w", [D, D])
    I["router_w"] = dt_in("router_w", [2, D, NE])
    I["router_b"] = dt_in("router_b", [2, NE])
    I["moe_w_in"] = dt_in("moe_w_in", [2, NE, D, 2 * D] if not small_moe else [2, NE, 8, 16])
    I["moe_b_in"] = dt_in("moe_b_in", [2, NE, 2 * D])
    I["moe_w_out"] = dt_in("moe_w_out", [2, NE, D, D] if not small_moe else [2, NE, 8, 8])
    I["moe_b_out"] = dt_in("moe_b_out", [2, NE, D])
    out = nc.dram_tensor("out", [NTOK, D], F32, kind="ExternalOutput").ap()
    scr = lambda n, s, d: nc.dram_tensor(n, list(s), d, kind="Internal").ap()
    Dr = {}
    Dr["modrow"] = scr("modrow", [2, 2, 6 * D], F32)
    Dr["gluT"] = scr("gluT", [8, 128, NTOK], BF16)
    Dr["xa"] = scr("xa", [NTOK, D], F32)
    Dr["xb"] = scr("xb", [NTOK, D], F32)
    Dr["Xe"] = scr("Xe", [NE * CAP, D], BF16)
    Dr["Y"] = scr("Y", [NE * CAP, D], BF16)
    Dr["kT"] = scr("kT", [8, 128, NTOK], BF16)
    Dr["qT"] = scr("qT", [8, 128, NTOK], BF16)
    Dr["v"] = scr("v", [NTOK, D], BF16)
    Dr["oT"] = scr("oT", [8, 128, NTOK], BF16)
    dbg_out = {}
    if dbg:
        dbg_out["d_mod"] = nc.dram_tensor("d_mod", [2, 2, 6 * D], F32, kind="ExternalOutput").ap()
        dbg_out["d_xa"] = nc.dram_tensor("d_xa", [NTOK, D], F32, kind="ExternalOutput").ap()
        dbg_out["d_lg"] = nc.dram_tensor("d_lg", [NTOK, NE], F32, kind="ExternalOutput").ap()
        dbg_out["d_sl"] = nc.dram_tensor("d_sl", [NTOK, 4], I32, kind="ExternalOutput").ap()
        dbg_out["d_gt"] = nc.dram_tensor("d_gt", [NTOK, 4], F32, kind="ExternalOutput").ap()
        if start == 6:
            dbg_out["d_kT"] = nc.dram_tensor("d_kT", [8, 128, NTOK], BF16, kind="ExternalOutput").ap()
            dbg_out["d_v"] = nc.dram_tensor("d_v", [NTOK, D], BF16, kind="ExternalOutput").ap()

    with ExitStack() as gst:
        S = Sched(nc, gst)
        G = {}
        G["ident_f"] = _sb(gst, nc, "ident_f", [128, 128], F32)
        G["ident_b"] = _sb(gst, nc, "ident_b", [128, 128], BF16)
        G["ones_f"] = _sb(gst, nc, "ones_f", [128, 128], F32)
        G["ones_b"] = _sb(gst, nc, "ones_b", [128, 128], BF16)
        G["triU"] = _sb(gst, nc, "triU", [128, 128], F32)
        G["eps"] = _sb(gst, nc, "eps", [128, 1], F32)
        G["one"] = _sb(gst, nc, "one", [128, 1], F32)
        G["slots"] = _sb(gst, nc, "slots", [128, NT, 4], I32)
        G["gates"] = _sb(gst, nc, "gates", [128, NT, 4], F32)
        G["gmat"] = _sb(gst, nc, "gmat", [128, NT, NE], F32)
        G["modT"] = _sb(gst, nc, "modT", [128, 4, 6, 8], F32)
        G["ecap"] = _sb(gst, nc, "ecap", [128, NE], F32)
        tmp = _sb(gst, nc, "tmp_iota", [128, 128], F32)
        G["iota"] = tmp
        S.op("pool", lambda e: e.iota(tmp[:], pattern=[[1, 128]], base=0, channel_multiplier=-1,
                                      allow_small_or_imprecise_dtypes=True), writes=[tmp.k])
        S.op("dve", lambda e: e.tensor_single_scalar(G["ident_f"][:], tmp[:], 0.0, ALU.is_equal),
             reads=[tmp.k], writes=[G["ident_f"].k])
        S.op("dve", lambda e: e.tensor_single_scalar(G["ident_b"][:], tmp[:], 0.0, ALU.is_equal),
             reads=[tmp.k], writes=[G["ident_b"].k])
        S.op("dve", lambda e: e.tensor_single_scalar(G["triU"][:], tmp[:], 0.0, ALU.is_gt),
             reads=[tmp.k], writes=[G["triU"].k])
        S.op("dve", lambda e: e.memset(G["ones_f"][:], 1.0), writes=[G["ones_f"].k])
        S.op("dve", lambda e: e.memset(G["ones_b"][:], 1.0), writes=[G["ones_b"].k])
        S.op("dve", lambda e: e.memset(G["eps"][:], LN_EPS), writes=[G["eps"].k])
        S.op("dve", lambda e: e.memset(G["one"][:], 1.0), writes=[G["one"].k])
        S.op("pool", lambda e: e.iota(G["ecap"][:], pattern=[[CAP, NE]], base=0, channel_multiplier=0,
                                      allow_small_or_imprecise_dtypes=True), writes=[G["ecap"].k])
        ctx = K(nc, S, dbg)
        ctx.I, ctx.Dr, ctx.G, ctx.out, ctx.dbg_out = I, Dr, G, out, dbg_out
        ctx.upto = upto
        ctx.bc_reg = nc.gpsimd.to_reg(NE * CAP - 1)
        for nm in ('slots', 'gates', 'gmat'):
            G[nm].kt = [Tk() for _ in range(NT)]

        phases = [
            ("mod", lambda: phase_mod(ctx)),
            ("glu", lambda: phase_glu(ctx)),
            ("conv", lambda: phase_conv(ctx)),
            ("front0", lambda: phase_front(ctx, 0)),
            ("experts0", lambda: phase_experts(ctx, 0)),
            ("combine0", lambda: phase_combine(ctx, 0)),
            ("kvq", lambda: phase_kvq(ctx)),
            ("attn", lambda: phase_attn(ctx)),
            ("oproj", lambda: phase_oproj(ctx)),
            ("front1", lambda: phase_front(ctx, 1)),
            ("experts1", lambda: phase_experts(ctx, 1)),
            ("combine1", lambda: phase_combine(ctx, 1)),
        ]
        if start > 0:
            din = nc.dram_tensor("d_in_x", [NTOK, D], F32, kind="ExternalInput").ap()
            dstn = "xb" if start in (6, 7, 8) else "xa"
            S.dma("sp", lambda e: e.dma_start(out=Dr[dstn], in_=din))
            dmod = nc.dram_tensor("d_in_mod", [2, 2, 6 * D], F32, kind="ExternalInput").ap()
            S.dma("sp", lambda e: e.dma_start(out=Dr["modrow"], in_=dmod))
            S.barrier()
            load_modT(ctx)
            S.barrier()
        for i, (name, fn) in enumerate(phases):
            if i > upto:
                break
            if i < start:
                continue
            fn()
            S.barrier()
        if dbg:
            dump_dbg(ctx)
            S.barrier()
    return nc


def dump_dbg(ctx):
    nc, S = ctx.nc, ctx.S
    if "d_kT" in ctx.dbg_out:
        S.dma("sp", lambda e: e.dma_start(out=ctx.dbg_out["d_kT"], in_=ctx.Dr["kT"]))
        S.dma("sp", lambda e: e.dma_start(out=ctx.dbg_out["d_v"], in_=ctx.Dr["v"]))
    S.dma("sp", lambda e: e.dma_start(out=ctx.dbg_out["d_mod"], in_=ctx.Dr["modrow"]))
    S.dma("sp", lambda e: e.dma_start(out=ctx.dbg_out["d_xa"], in_=ctx.Dr["xa"]))
    S.dma("sp", lambda e: e.dma_start(out=ctx.dbg_out["d_sl"].rearrange("(t p) k -> p t k", p=128),
                                      in_=ctx.G["slots"][:]), reads=ctx.G["slots"].kt)
    S.dma("sp", lambda e: e.dma_start(out=ctx.dbg_out["d_gt"].rearrange("(t p) k -> p t k", p=128),
                                      in_=ctx.G["gates"][:]), reads=ctx.G["gates"].kt)


def phase_mod(ctx):
    nc, S, I, Dr, G = ctx.nc, ctx.S, ctx.I, ctx.Dr, ctx.G
    with ExitStack() as st:
        cT = _sb(st, nc, "cT", [128, 8, 2], F32)
        caT = _sb(st, nc, "caT", [128, 8, 2], BF16)
        wb = [_sb(st, nc, f"adaw{i}", [128, 8, 3 * D], BF16) for i in range(2)]
        bb = _sb(st, nc, "adab", [1, 2, 6 * D], BF16)
        msb = _sb(st, nc, "modsb", [2, 2, 6 * D], F32)
        pp = [_ps(st, nc, f"pmod{i}", [128, 512]) for i in range(2)]
        S.dma("sp", lambda e: e.dma_start(out=cT[:], in_=I["cT"]), writes=[cT.k])
        for l in range(2):
            cast_load(S, lambda c0, c1, l=l: bb[0:1, l, c0:c1], lambda c0, c1, l=l: I["ada_b"][l:l + 1, c0:c1], 6 * D, bb.k)
        S.op("act", lambda e: e.activation(caT[:], cT[:], AF.Silu), reads=[cT.k], writes=[caT.k])
        it = 0
        for l in range(2):
            for hf in range(2):
                w = wb[it % 2]
                src = I["ada_w"][l, :, hf * 3 * D:(hf + 1) * 3 * D].rearrange("(k p) n -> p k n", p=128)
                for k in range(8):
                    cast_load(S, lambda c0, c1, k=k: w[:, k, c0:c1], lambda c0, c1, k=k: src[:, k, c0:c1], 3 * D, w.k, step=1536)
                for n in range(6):
                    p = pp[n % 2]
                    col = hf * 3 * D + n * 512
                    for k in range(8):
                        S.op("pe", lambda e, k=k: e.matmul(p[0:2, :], caT[:, k, :], w[:, k, n * 512:(n + 1) * 512],
                                                          start=(k == 0), stop=False),
                             reads=[caT.k, w.k], writes=[p.k])
                    S.op("pe", lambda e: e.matmul(p[0:2, :], G["ones_b"][0:1, 0:2], bb[0:1, l, col:col + 512],
                                                  start=False, stop=True),
                         reads=[G["ones_b"].k, bb.k], writes=[p.k])
                    S.op("act", lambda e: e.activation(msb[0:2, l, col:col + 512], p[0:2, :], AF.Copy),
                         reads=[p.k], writes=[msb.k])
                it += 1
        for l in range(2):
            S.dma("sp", lambda e, l=l: e.dma_start(out=Dr["modrow"][l, :, :], in_=msb[0:2, l, :]), reads=[msb.k])
    S.barrier()
    load_modT(ctx)


def load_modT(ctx):
    nc, S, I, Dr, G = ctx.nc, ctx.S, ctx.I, ctx.Dr, ctx.G
    for l in range(2):
        for b in range(2):
            for wch in range(6):
                S.dma("sp", lambda e, l=l, b=b, wch=wch: e.dma_start(
                    out=G["modT"][:, l * 2 + b, wch, :],
                    in_=Dr["modrow"][l, b, wch * D:(wch + 1) * D].rearrange("(k p) -> p k", p=128),
                    allow_slow_non_contiguous=True), writes=[G["modT"].k])


def load_bc(ctx, st, name, src_row):
    t = _sb(st, ctx.nc, name, [128, D], F32)
    ctx.S.dma("sp", lambda e: e.dma_start(out=t[:], in_=src_row.partition_broadcast(128)), writes=[t.k])
    return t


def add_one(ctx, t):
    ctx.S.op("pool", lambda e: e.tensor_scalar_add(t[:], t[:], 1.0), reads=[t.k], writes=[t.k])


def mod_scale_bias(ctx, st, l, w_sh, w_sc, name):
    nc, S, G = ctx.nc, ctx.S, ctx.G
    sc = _sb(st, nc, name + "_sc", [128, 2, 8], F32)
    sh = _sb(st, nc, name + "_sh", [128, 2, 8], F32)
    for b in range(2):
        S.op("dve", lambda e, b=b: e.tensor_scalar_add(sc[:, b, :], G["modT"][:, l * 2 + b, w_sc, :], 1.0),
             reads=[G["modT"].k], writes=[sc.k])
        S.op("dve", lambda e, b=b: e.tensor_copy(sh[:, b, :], G["modT"][:, l * 2 + b, w_sh, :]),
             reads=[G["modT"].k], writes=[sh.k])
    return sc, sh


def run_interleaved(gens, width):
    active = []
    it = iter(gens)
    more = True
    while True:
        while more and len(active) < width:
            try:
                active.append(next(it))
            except StopIteration:
                more = False
        if not active:
            break
        for g in list(active):
            try:
                next(g)
            except StopIteration:
                active.remove(g)


class Epi:
    def __init__(self, ctx, st, l, sub, gate_which):
        nc, S, I, Dr = ctx.nc, ctx.S, ctx.I, ctx.Dr
        self.ctx = ctx
        self.lng = load_bc(ctx, st, f"lng{l}{sub}", I["ln_g"][l * 2 + sub, :])
        self.lnb = load_bc(ctx, st, f"lnb{l}{sub}", I["ln_b"][l * 2 + sub, :])
        self.gate = []
        for b in range(2):
            g = load_bc(ctx, st, f"gate{l}{sub}{b}", Dr["modrow"][l, b, gate_which * D:(gate_which + 1) * D])
            add_one(ctx, g)
            self.gate.append(g)
        self.t1 = [_sb(st, nc, f"ep_t1_{i}", [128, D], F32) for i in range(2)]
        self.r = [_sb(st, nc, f"ep_r_{i}", [128, D], F32) for i in range(2)]
        self.xo = [_sb(st, nc, f"ep_xo_{i}", [128, D], F32) for i in range(2)]
        self.st6 = [_sb(st, nc, f"ep_st_{i}", [128, 2, 6], F32) for i in range(2)]
        self.mv = [_sb(st, nc, f"ep_mv_{i}", [128, 4], F32) for i in range(2)]
        self.n = 0

    def run_g(self, ys, x_t, b, dst):
        S = self.ctx.S
        i = self.n % 2
        self.n += 1
        t1, r, xo, st6, mv = self.t1[i], self.r[i], self.xo[i], self.st6[i], self.mv[i]
        g = self.gate[b]
        for h, (yb, yap) in enumerate(ys):
            S.op("dve", lambda e, h=h, yap=yap: e.tensor_tensor(t1[:, h * 512:(h + 1) * 512], yap,
                                                              g[:, h * 512:(h + 1) * 512], ALU.mult),
                 reads=[yb.k, g.k], writes=[t1.k])
        yield
        S.op("dve", lambda e: e.scalar_tensor_tensor(r[:], x_t[:], ALPHA_DN, t1[:], ALU.mult, ALU.add),
             reads=[x_t.k, t1.k], writes=[r.k])
        yield
        for h in range(2):
            S.op("dve", lambda e, h=h: e.bn_stats(st6[:, h, :], r[:, h * 512:(h + 1) * 512]),
                 reads=[r.k], writes=[st6.k])
        S.op("dve", lambda e: e.bn_aggr(mv[:, 0:2], st6[:].rearrange("p a b -> p (a b)")), reads=[st6.k], writes=[mv.k])
        yield
        S.op("act", lambda e: e.activation(mv[:, 2:3], mv[:, 1:2], AF.Sqrt, bias=self.ctx.G["eps"][:], scale=1.0),
             reads=[mv.k, self.ctx.G["eps"].k], writes=[mv.k])
        yield
        S.op("dve", lambda e: e.reciprocal(mv[:, 2:3], mv[:, 2:3]), reads=[mv.k], writes=[mv.k])
        S.op("dve", lambda e: e.tensor_scalar(mv[:, 3:4], mv[:, 0:1], -1.0, mv[:, 2:3], ALU.mult, ALU.mult),
             reads=[mv.k], writes=[mv.k])
        S.op("act", lambda e: e.activation(t1[:], r[:], AF.Identity, bias=mv[:, 3:4], scale=mv[:, 2:3]),
             reads=[r.k, mv.k], writes=[t1.k])
        yield
        S.op("pool", lambda e: e.tensor_tensor(xo[:], t1[:], self.lng[:], ALU.mult),
             reads=[t1.k, self.lng.k], writes=[xo.k])
        S.op("pool", lambda e: e.tensor_tensor(xo[:], xo[:], self.lnb[:], ALU.add),
             reads=[xo.k, self.lnb.k], writes=[xo.k])
        S.dma("sp", lambda e: e.dma_start(out=dst, in_=xo[:]), reads=[xo.k])
        yield

    def run(self, ys, x_t, b, dst):
        for _ in self.run_g(ys, x_t, b, dst):
            pass


def transpose_tile(ctx, src, psT, evac):
    S, G = ctx.S, ctx.G
    for k in range(8):
        p = psT[k // 4]
        S.op("pe", lambda e, k=k, p=p: e.transpose(p[:, (k % 4) * 128:(k % 4 + 1) * 128], src[:, k * 128:(k + 1) * 128],
                                                 G["ident_f"][:]),
             reads=[src.k, G["ident_f"].k], writes=[p.k])
    for k in range(8):
        p = psT[k // 4]
        evac(k, p, p[:, (k % 4) * 128:(k % 4 + 1) * 128])


def phase_glu(ctx):
    nc, S, I, Dr, G = ctx.nc, ctx.S, ctx.I, ctx.Dr, ctx.G
    with ExitStack() as st:
        w1 = _sb(st, nc, "w1", [128, 8, 2 * D], BF16)
        b1T = _sb(st, nc, "b1T", [128, 16], F32)
        sc, sh = mod_scale_bias(ctx, st, 0, 0, 1, "m1")
        xt = [_sb(st, nc, f"xt{i}", [128, D], F32) for i in range(3)]
        hT = [_sb(st, nc, f"hT{i}", [128, 8, 512], BF16) for i in range(2)]
        sig = [_sb(st, nc, f"sig{i}", [128, 512], F32) for i in range(2)]
        gl = [_sb(st, nc, f"gl{i}", [128, 8, 512], BF16) for i in range(2)]
        psT = [_ps(st, nc, f"psT{i}", [128, 512]) for i in range(2)]
        pa = [_ps(st, nc, f"pa{i}", [128, 512]) for i in range(2)]
        pg = [_ps(st, nc, f"pg{i}", [128, 512]) for i in range(2)]
        src = I["cv_w1"].rearrange("(k p) n -> p k n", p=128)
        for k in range(8):
            S.dma("pool", lambda e, k=k: e.dma_start(out=w1[:, k, :], in_=src[:, k, :]), writes=[w1.k])
        S.dma("sp", lambda e: e.dma_start(out=b1T[:], in_=I["cv_b1T"]), writes=[b1T.k])
        ti = 0
        for j in range(NTOK // 512):
            b = (j * 512) // SEQ
            h = hT[j % 2]
            for i in range(4):
                x_t = xt[ti % 3]
                ti += 1
                r0 = j * 512 + i * 128
                S.dma("sp", lambda e, x_t=x_t, r0=r0: e.dma_start(out=x_t[:], in_=I["x"][r0:r0 + 128, :]), writes=[x_t.k])
                transpose_tile(ctx, x_t, psT, lambda k, p, ap, i=i: S.op(
                    "act", lambda e: e.activation(h[:, k, i * 128:(i + 1) * 128], ap, AF.Identity,
                                                  bias=sh[:, b, k:k + 1], scale=sc[:, b, k:k + 1]),
                    reads=[p.k, sh.k, sc.k], writes=[h.k]))
            g_o = gl[j % 2]
            for m in range(8):
                a_p, g_p, sg = pa[m % 2], pg[m % 2], sig[m % 2]
                for k in range(8):
                    S.op("pe", lambda e, k=k: e.matmul(a_p[:], w1[:, k, m * 128:(m + 1) * 128], h[:, k, :],
                                                      start=(k == 0), stop=(k == 7)), reads=[w1.k, h.k], writes=[a_p.k])
                for k in range(8):
                    S.op("pe", lambda e, k=k: e.matmul(g_p[:], w1[:, k, D + m * 128:D + (m + 1) * 128], h[:, k, :],
                                                      start=(k == 0), stop=(k == 7)), reads=[w1.k, h.k], writes=[g_p.k])
                S.op("act", lambda e: e.activation(sg[:], g_p[:], AF.Sigmoid, bias=b1T[:, 8 + m:9 + m], scale=1.0),
                     reads=[g_p.k, b1T.k], writes=[sg.k])
                S.op("dve", lambda e: e.scalar_tensor_tensor(g_o[:, m, :], a_p[:], b1T[:, m:m + 1], sg[:], ALU.add, ALU.mult),
                     reads=[a_p.k, b1T.k, sg.k], writes=[g_o.k])
            S.dma("sp", lambda e, j=j: e.dma_start(out=Dr["gluT"][:, :, j * 512:(j + 1) * 512].rearrange("k p t -> p k t"),
                                                  in_=g_o[:]), reads=[g_o.k])


def phase_conv(ctx):
    nc, S, I, Dr, G = ctx.nc, ctx.S, ctx.I, ctx.Dr, ctx.G
    with ExitStack() as st:
        dwT = _sb(st, nc, "dwT", [128, 8, 31], F32)
        vecT = _sb(st, nc, "vecT", [128, 3, 8], F32)
        dg = _sb(st, nc, "dg", [128, 8 * 31, 128], BF16)
        w2 = _sb(st, nc, "w2", [128, 8, D], BF16)
        b2 = _sb(st, nc, "b2", [1, D], BF16)
        epi = Epi(ctx, st, 0, 0, 2)
        glb = [_sb(st, nc, f"glb{i}", [128, 8, 544], BF16) for i in range(2)]
        vb = _sb(st, nc, "vb", [128, 8, 512], BF16)
        vsq = [_sb(st, nc, f"vsq{i}", [128, 512], BF16) for i in range(2)]
        sT = _sb(st, nc, "sT", [128, 8, 512], BF16)
        mean = _sb(st, nc, "cmean", [128, 512], F32)
        msq = _sb(st, nc, "cmsq", [128, 512], F32)
        rstd = _sb(st, nc, "crstd", [128, 512], F32)
        nmr = _sb(st, nc, "cnmr", [128, 512], F32)
        zt = [_sb(st, nc, f"czt{i}", [128, 512], F32) for i in range(2)]
        xt = [_sb(st, nc, f"cxt{i}", [128, D], F32) for i in range(2)]
        pc = [_ps(st, nc, f"pc{i}", [128, 512]) for i in range(2)]
        ps1 = _ps(st, nc, "ps1", [128, 512])
        ps2 = _ps(st, nc, "ps2", [128, 512])
        py = [_ps(st, nc, f"py{i}", [128, 512]) for i in range(4)]
        S.dma("sp", lambda e: e.dma_start(out=dwT[:], in_=I["cv_dwT"]), writes=[dwT.k])
        S.dma("sp", lambda e: e.dma_start(out=vecT[:], in_=I["cv_vecT"]), writes=[vecT.k])
        S.dma("pool", lambda e: e.dma_start(out=b2[:], in_=I["cv_b2"]), writes=[b2.k])
        src = I["cv_w2"].rearrange("(k p) n -> p k n", p=128)
        for k in range(8):
            S.dma("pool", lambda e, k=k: e.dma_start(out=w2[:, k, :], in_=src[:, k, :]), writes=[w2.k])
        for c in range(8):
            for k in range(31):
                S.op("dve", lambda e, c=c, k=k: e.tensor_scalar(dg[:, c * 31 + k, :], G["ident_b"][:], dwT[:, c, k:k + 1], None,
                                                              ALU.mult),
                     reads=[G["ident_b"].k, dwT.k], writes=[dg.k])
        vb2 = [vb, _sb(st, nc, "vb_b", [128, 8, 512], BF16)]
        mean2 = [mean, _sb(st, nc, "cmean_b", [128, 512], F32)]
        rstd2 = [rstd, _sb(st, nc, "crstd_b", [128, 512], F32)]
        nmr2 = [nmr, _sb(st, nc, "cnmr_b", [128, 512], F32)]
        tcnt = [0]

        def conv_stage(j):
            t0 = j * 512
            g_in = glb[j % 2]
            vbj, meanj, rstdj, nmrj = vb2[j % 2], mean2[j % 2], rstd2[j % 2], nmr2[j % 2]
            if t0 % SEQ == 0:
                S.op("pool", lambda e: e.memset(g_in[:, :, 0:30], 0.0), writes=[g_in.k])
                S.dma("sp", lambda e: e.dma_start(out=g_in[:, :, 30:542],
                                                  in_=Dr["gluT"][:, :, t0:t0 + 512].rearrange("k p t -> p k t")),
                      writes=[g_in.k])
            else:
                S.dma("sp", lambda e: e.dma_start(out=g_in[:, :, 0:542],
                                                  in_=Dr["gluT"][:, :, t0 - 30:t0 + 512].rearrange("k p t -> p k t")),
                      writes=[g_in.k])
            for c in range(8):
                p = pc[c % 2]
                vq = vsq[c % 2]
                for k in range(31):
                    S.op("pe", lambda e, k=k: e.matmul(p[:], dg[:, c * 31 + k, :], g_in[:, c, k:k + 512],
                                                      start=(k == 0), stop=(k == 30)),
                         reads=[dg.k, g_in.k], writes=[p.k])
                S.op("act", lambda e: e.activation(vbj[:, c, :], p[:], AF.Identity, bias=vecT[:, 0, c:c + 1], scale=1.0),
                     reads=[p.k, vecT.k], writes=[vbj.k])
                S.op("act", lambda e: e.activation(vq[:], p[:], AF.Square, bias=vecT[:, 0, c:c + 1], scale=1.0),
                     reads=[p.k, vecT.k], writes=[vq.k])
                S.op("pe", lambda e: e.matmul(ps1[:], G["ones_b"][:], vbj[:, c, :], start=(c == 0), stop=(c == 7)),
                     reads=[G["ones_b"].k, vbj.k], writes=[ps1.k])
                S.op("pe", lambda e: e.matmul(ps2[:], G["ones_b"][:], vq[:], start=(c == 0), stop=(c == 7)),
                     reads=[G["ones_b"].k, vq.k], writes=[ps2.k])
            S.op("act", lambda e: e.activation(meanj[:], ps1[:], AF.Copy, scale=1.0 / D), reads=[ps1.k], writes=[meanj.k])
            S.op("act", lambda e: e.activation(msq[:], ps1[:], AF.Square, scale=1.0 / D), reads=[ps1.k], writes=[msq.k])
            S.op("dve", lambda e: e.scalar_tensor_tensor(rstdj[:], ps2[:], 1.0 / D, msq[:], ALU.mult, ALU.subtract),
                 reads=[ps2.k, msq.k], writes=[rstdj.k])
            S.op("act", lambda e: e.activation(rstdj[:], rstdj[:], AF.Sqrt, bias=G["eps"][:], scale=1.0),
                 reads=[rstdj.k, G["eps"].k], writes=[rstdj.k])
            S.op("dve", lambda e: e.reciprocal(rstdj[:], rstdj[:]), reads=[rstdj.k], writes=[rstdj.k])
            S.op("dve", lambda e: e.scalar_tensor_tensor(nmrj[:], meanj[:], -1.0, rstdj[:], ALU.mult, ALU.mult),
                 reads=[meanj.k, rstdj.k], writes=[nmrj.k])

        def rest_stage(j):
            b = (j * 512) // SEQ
            t0 = j * 512
            vbj, rstdj, nmrj = vb2[j % 2], rstd2[j % 2], nmr2[j % 2]
            for c in range(8):
                z = zt[c % 2]
                S.op("dve", lambda e: e.tensor_tensor(z[:], vbj[:, c, :], rstdj[:], ALU.mult), reads=[vbj.k, rstdj.k], writes=[z.k])
                S.op("pool", lambda e: e.tensor_tensor(z[:], z[:], nmrj[:], ALU.add), reads=[z.k, nmrj.k], writes=[z.k])
                S.op("act", lambda e: e.activation(sT[:, c, :], z[:], AF.Silu, bias=vecT[:, 2, c:c + 1],
                                                   scale=vecT[:, 1, c:c + 1]),
                     reads=[z.k, vecT.k], writes=[sT.k])
            for i in range(4):
                ti = tcnt[0]
                r0 = t0 + i * 128
                x_t = xt[ti % 2]
                S.dma("sp", lambda e: e.dma_start(out=x_t[:], in_=I["x"][r0:r0 + 128, :]), writes=[x_t.k])
                ys = []
                for n in range(2):
                    p = py[(ti % 2) * 2 + n]
                    for c in range(8):
                        S.op("pe", lambda e, c=c: e.matmul(p[:], sT[:, c, i * 128:(i + 1) * 128], w2[:, c, n * 512:(n + 1) * 512],
                                                          start=(c == 0), stop=False), reads=[sT.k, w2.k], writes=[p.k])
                    S.op("pe", lambda e: e.matmul(p[:], G["ones_b"][0:1, :], b2[0:1, n * 512:(n + 1) * 512], start=False, stop=True),
                         reads=[G["ones_b"].k, b2.k], writes=[p.k])
                    ys.append((p, p[:]))
                tcnt[0] += 1
                epi.run(ys, x_t, b, Dr["xa"][r0:r0 + 128, :])

        NBK = NTOK // 512
        conv_stage(0)
        for j in range(NBK):
            if j + 1 < NBK:
                conv_stage(j + 1)
            rest_stage(j)


def phase_front(ctx, l):
    nc, S, I, Dr, G = ctx.nc, ctx.S, ctx.I, ctx.Dr, ctx.G
    with ExitStack() as st:
        S2, H2 = [], []
        for b in range(2):
            s2 = load_bc(ctx, st, f"S2_{b}", Dr["modrow"][l, b, 4 * D:5 * D])
            add_one(ctx, s2)
            S2.append(s2)
            H2.append(load_bc(ctx, st, f"H2_{b}", Dr["modrow"][l, b, 3 * D:4 * D]))
        rw = _sb(st, nc, "rw", [128, 8, NE], F32)
        rb = _sb(st, nc, "rb", [1, NE], F32)
        srun = _sb(st, nc, "srun", [128, NE], F32)
        xt = [_sb(st, nc, f"fx{i}", [128, D], F32) for i in range(2)]
        h2 = [_sb(st, nc, f"fh{i}", [128, D], F32) for i in range(2)]
        h2b = [_sb(st, nc, f"fhb{i}", [128, D], BF16) for i in range(3)]
        h2T = [_sb(st, nc, f"fhT{i}", [128, 8, 128], F32) for i in range(2)]
        sm = lambda nm, w: [_sb(st, nc, f"{nm}{i}", [128, w], F32) for i in range(2)]
        lg, top8, nv0, ex, ssum, mask, slotv, bad, oh, junk, slotf, okk, g4, gtmp = (
            sm("lg", NE), sm("top8", 8), sm("nv0", 1), sm("ex", 4), sm("ssum", 1), sm("mask", NE), sm("slotv", NE),
            sm("bad", NE), sm("oh", NE), sm("junk", NE), sm("slotf", 4), sm("okk", 4), sm("g4", 4), sm("gtmp", NE))
        cur = sm("cur", NE)
        psT = [_ps(st, nc, f"fpT{i}", [128, 512]) for i in range(2)]
        plg = [_ps(st, nc, f"fplg{i}", [128, 512]) for i in range(2)]
        ppos = [_ps(st, nc, f"fpps{i}", [128, 512]) for i in range(2)]
        S.dma("sp", lambda e: e.dma_start(out=rw[:], in_=I["router_w"][l].rearrange("(k p) e -> p k e", p=128)), writes=[rw.k])
        S.dma("sp", lambda e: e.dma_start(out=rb[:], in_=I["router_b"][l:l + 1, :]), writes=[rb.k])
        S.op("dve", lambda e: e.memset(srun[:], 0.0), writes=[srun.k])
        def tile_g(t):
            b = t // 16
            i = t % 2
            x_t, h, hb, hT = xt[i], h2[i], h2b[t % 3], h2T[i]
            S.dma("sp", lambda e: e.dma_start(out=x_t[:], in_=Dr["xa"][t * 128:(t + 1) * 128, :]), writes=[x_t.k])
            S.op("dve", lambda e: e.tensor_tensor(h[:], x_t[:], S2[b][:], ALU.mult), reads=[x_t.k, S2[b].k], writes=[h.k])
            S.op("dve", lambda e: e.tensor_tensor(h[:], h[:], H2[b][:], ALU.add), reads=[h.k, H2[b].k], writes=[h.k])
            S.op("act", lambda e: e.activation(hb[:], h[:], AF.Copy), reads=[h.k], writes=[hb.k])
            yield
            transpose_tile(ctx, h, psT, lambda k, p, ap: S.op(
                "act", lambda e: e.activation(hT[:, k, :], ap, AF.Copy), reads=[p.k], writes=[hT.k]))
            yield
            pl = plg[i]
            for k in range(8):
                S.op("pe", lambda e, k=k: e.matmul(pl[:, 0:NE], hT[:, k, :], rw[:, k, :], start=(k == 0), stop=False),
                     reads=[hT.k, rw.k], writes=[pl.k])
            S.op("pe", lambda e: e.matmul(pl[:, 0:NE], G["ones_f"][0:1, :], rb[0:1, :], start=False, stop=True),
                 reads=[G["ones_f"].k, rb.k], writes=[pl.k])
            S.op("dve", lambda e: e.tensor_copy(lg[i][:], pl[:, 0:NE]), reads=[pl.k], writes=[lg[i].k])
            yield
            S.op("dve", lambda e: e.tensor_copy(cur[i][:], lg[i][:]), reads=[lg[i].k], writes=[cur[i].k])
            yield
            for k in range(4):
                S.op("dve", lambda e, k=k: e.tensor_reduce(top8[i][:, k:k + 1], cur[i][:], AX.X, ALU.max),
                     reads=[cur[i].k], writes=[top8[i].k])
                if k < 3:
                    S.op("dve", lambda e, k=k: e.tensor_scalar(oh[i][:], cur[i][:], top8[i][:, k:k + 1], None, ALU.is_equal),
                         reads=[cur[i].k, top8[i].k], writes=[oh[i].k])
                    S.op("dve", lambda e: e.scalar_tensor_tensor(cur[i][:], oh[i][:], -BIG, cur[i][:], ALU.mult, ALU.add),
                         reads=[oh[i].k, cur[i].k], writes=[cur[i].k])
                yield
            S.op("dve", lambda e: e.tensor_scalar_mul(nv0[i][:], top8[i][:, 0:1], -1.0), reads=[top8[i].k], writes=[nv0[i].k])
            S.op("act", lambda e: e.activation(ex[i][:], top8[i][:, 0:4], AF.Exp, bias=nv0[i][:], scale=1.0),
                 reads=[top8[i].k, nv0[i].k], writes=[ex[i].k])
            S.op("dve", lambda e: e.tensor_reduce(ssum[i][:], ex[i][:], AX.X, ALU.add), reads=[ex[i].k], writes=[ssum[i].k])
            S.op("dve", lambda e: e.reciprocal(ssum[i][:], ssum[i][:]), reads=[ssum[i].k], writes=[ssum[i].k])
            S.op("dve", lambda e: e.tensor_scalar(g4[i][:], ex[i][:], ssum[i][:], None, ALU.mult),
                 reads=[ex[i].k, ssum[i].k], writes=[g4[i].k])
            yield
            S.op("dve", lambda e: e.tensor_scalar(mask[i][:], lg[i][:], top8[i][:, 3:4], None, ALU.is_ge),
                 reads=[lg[i].k, top8[i].k], writes=[mask[i].k])
            pp = ppos[i]
            S.op("pe", lambda e: e.matmul(pp[:, 0:NE], G["triU"][:], mask[i][:], start=True, stop=False),
                 reads=[G["triU"].k, mask[i].k], writes=[pp.k])
            S.op("pe", lambda e: e.matmul(pp[:, 0:NE], G["ones_f"][:], srun[:], start=False, stop=True),
                 reads=[G["ones_f"].k, srun.k], writes=[pp.k])
            S.op("dve", lambda e: e.tensor_tensor(srun[:], srun[:], mask[i][:], ALU.add), reads=[srun.k, mask[i].k], writes=[srun.k])
            yield
            S.op("dve", lambda e: e.tensor_single_scalar(bad[i][:], pp[:, 0:NE], float(CAP) - 0.5, ALU.is_ge),
                 reads=[pp.k], writes=[bad[i].k])
            S.op("dve", lambda e: e.tensor_tensor(slotv[i][:], pp[:, 0:NE], G["ecap"][:], ALU.add),
                 reads=[pp.k, G["ecap"].k], writes=[slotv[i].k])
            S.op("dve", lambda e: e.scalar_tensor_tensor(slotv[i][:], bad[i][:], BIG, slotv[i][:], ALU.mult, ALU.add),
                 reads=[bad[i].k, slotv[i].k], writes=[slotv[i].k])
            yield
            for k in range(4):
                S.op("dve", lambda e, k=k: e.tensor_scalar(oh[i][:], lg[i][:], top8[i][:, k:k + 1], None, ALU.is_equal),
                     reads=[lg[i].k, top8[i].k], writes=[oh[i].k])
                S.op("dve", lambda e: e.tensor_tensor(junk[i][:], oh[i][:], slotv[i][:], ALU.mult),
                     reads=[oh[i].k, slotv[i].k], writes=[junk[i].k])
                S.op("dve", lambda e, k=k: e.tensor_reduce(slotf[i][:, k:k + 1], junk[i][:], AX.X, ALU.add),
                     reads=[junk[i].k], writes=[slotf[i].k])
                yield
            S.op("dve", lambda e: e.tensor_single_scalar(okk[i][:], slotf[i][:], 1.0e5, ALU.is_lt), reads=[slotf[i].k], writes=[okk[i].k])
            yield
            gk, sk, mk = G["gates"].kt[t], G["slots"].kt[t], G["gmat"].kt[t]
            S.op("dve", lambda e: e.tensor_tensor(G["gates"][:, t, :], g4[i][:], okk[i][:], ALU.mult),
                 reads=[g4[i].k, okk[i].k], writes=[gk])
            S.op("dve", lambda e: e.tensor_copy(G["slots"][:, t, :], slotf[i][:]), reads=[slotf[i].k], writes=[sk])
            yield
            for k in range(4):
                dstm = G["gmat"][:, t, :] if k == 0 else gtmp[i][:]
                S.op("dve", lambda e, k=k, dstm=dstm: e.tensor_scalar(dstm, lg[i][:], top8[i][:, k:k + 1], G["gates"][:, t, k:k + 1],
                                                                    ALU.is_equal, ALU.mult),
                     reads=[lg[i].k, top8[i].k, gk], writes=[mk if k == 0 else gtmp[i].k])
                if k > 0:
                    S.op("dve", lambda e: e.tensor_tensor(G["gmat"][:, t, :], G["gmat"][:, t, :], gtmp[i][:], ALU.add),
                         reads=[mk, gtmp[i].k], writes=[mk])
            for k in range(4):
                S.dma("pool", lambda e, k=k: e.indirect_dma_start(
                    out=Dr["Xe"], out_offset=bass.IndirectOffsetOnAxis(ap=G["slots"][:, t, k:k + 1], axis=0),
                    in_=hb[:], in_offset=None, bounds_check=ctx.bc_reg, oob_is_err=False), reads=[hb.k, sk])
            yield

        run_interleaved((tile_g(t) for t in range(NT)), 2)


def phase_experts(ctx, l):
    nc, S, I, Dr, G = ctx.nc, ctx.S, ctx.I, ctx.Dr, ctx.G
    with ExitStack() as st:
        win = [_sb(st, nc, f"win{i}", [128, 8, 2 * D], BF16) for i in range(2)]
        wout = [_sb(st, nc, f"wout{i}", [128, 8, D], BF16) for i in range(2)]
        bin_ = [_sb(st, nc, f"bin{i}", [128, 2 * D], F32) for i in range(2)]
        xe = [_sb(st, nc, f"xe{i}", [128, D], BF16) for i in range(2)]
        xT = [_sb(st, nc, f"xT{i}", [128, 8, 128], BF16) for i in range(2)]
        xg = [_sb(st, nc, f"xg{i}", [128, 512], F32) for i in range(2)]
        sg = [_sb(st, nc, f"sg{i}", [128, 512], F32) for i in range(2)]
        xl = [_sb(st, nc, f"xl{i}", [128, 512], F32) for i in range(2)]
        tt = [_sb(st, nc, f"tt{i}", [128, 512], F32) for i in range(2)]
        act = [_sb(st, nc, f"act{i}", [128, D], BF16) for i in range(2)]
        actT = [_sb(st, nc, f"actT{i}", [128, 8, 128], BF16) for i in range(2)]
        yb = [_sb(st, nc, f"yb{i}", [128, D], BF16) for i in range(2)]
        psT = [_ps(st, nc, f"epT{i}", [128, 1024], BF16) for i in range(2)]
        pu = [_ps(st, nc, f"epu{i}", [128, 512]) for i in range(4)]
        py = [_ps(st, nc, f"epy{i}", [128, 512]) for i in range(2)]

        def load_w(e):
            w, wo, bi = win[e % 2], wout[e % 2], bin_[e % 2]
            s1 = I["moe_w_in"][l, e].rearrange("(k p) n -> p k n", p=128)
            s2 = I["moe_w_out"][l, e].rearrange("(k p) n -> p k n", p=128)
            for k in range(8):
                S.dma("pool", lambda q, k=k: q.dma_start(out=w[:, k, :], in_=s1[:, k, :]), writes=[w.k])
            for k in range(8):
                S.dma("pool", lambda q, k=k: q.dma_start(out=wo[:, k, :], in_=s2[:, k, :]), writes=[wo.k])
            S.dma("sp", lambda q: q.dma_start(out=bi[:], in_=I["moe_b_in"][l, e, :].partition_broadcast(128)), writes=[bi.k])

        load_w(0)
        load_w(1)
        blocks = [(e_, j) for e_ in range(NE) for j in range(NBLK)]
        NB = len(blocks)
        xe4 = xe + [_sb(st, nc, f"xe{i}", [128, D], BF16) for i in range(2, 4)]

        def load_x(n):
            e_, j = blocks[n]
            r0 = e_ * CAP + j * 128
            x_e = xe4[n % 4]
            S.dma("sp", lambda q: q.dma_start(out=x_e[:], in_=Dr["Xe"][r0:r0 + 128, :]), writes=[x_e.k])

        def t_x(n):
            x_e, x_T = xe4[n % 4], xT[n % 2]
            pt = psT[0]
            for k in range(8):
                S.op("pe", lambda q, k=k: q.transpose(pt[:, k * 128:(k + 1) * 128], x_e[:, k * 128:(k + 1) * 128], G["ident_b"][:]),
                     reads=[x_e.k, G["ident_b"].k], writes=[pt.k])
            S.op("act", lambda q: q.activation(x_T[:].rearrange("p k t -> p (k t)"), pt[:], AF.Copy), reads=[pt.k], writes=[x_T.k])

        def mm1(n):
            e_, j = blocks[n]
            i = n % 2
            w, bi = win[e_ % 2], bin_[e_ % 2]
            x_T, a_ = xT[i], act[i]
            for hf in range(2):
                pg, pl = pu[hf * 2], pu[hf * 2 + 1]
                for (p, c0) in ((pg, hf * 512), (pl, D + hf * 512)):
                    for k in range(8):
                        S.op("pe", lambda q, k=k: q.matmul(p[:], x_T[:, k, :], w[:, k, c0:c0 + 512], start=(k == 0), stop=(k == 7)),
                             reads=[x_T.k, w.k], writes=[p.k])
                S.op("dve", lambda q: q.tensor_tensor(xg[hf][:], pg[:], bi[:, hf * 512:(hf + 1) * 512], ALU.add), reads=[pg.k, bi.k], writes=[xg[hf].k])
                S.op("dve", lambda q: q.tensor_scalar_min(xg[hf][:], xg[hf][:], SW_LIM), reads=[xg[hf].k], writes=[xg[hf].k])
                S.op("act", lambda q: q.activation(sg[hf][:], xg[hf][:], AF.Sigmoid, scale=SW_ALPHA), reads=[xg[hf].k], writes=[sg[hf].k])
                S.op("dve", lambda q: q.tensor_tensor(xl[hf][:], pl[:], bi[:, D + hf * 512:D + (hf + 1) * 512], ALU.add), reads=[pl.k, bi.k], writes=[xl[hf].k])
                S.op("dve", lambda q: q.tensor_scalar(xl[hf][:], xl[hf][:], SW_LIM, -SW_LIM, ALU.min, ALU.max), reads=[xl[hf].k], writes=[xl[hf].k])
                S.op("dve", lambda q: q.scalar_tensor_tensor(tt[hf][:], xl[hf][:], 1.0, xg[hf][:], ALU.add, ALU.mult),
                     reads=[xl[hf].k, xg[hf].k], writes=[tt[hf].k])
                S.op("dve", lambda q: q.tensor_tensor(a_[:, hf * 512:(hf + 1) * 512], tt[hf][:], sg[hf][:], ALU.mult),
                     reads=[tt[hf].k, sg[hf].k], writes=[a_.k])

        def t_a(n):
            i = n % 2
            a_, a_T = act[i], actT[i]
            pt2 = psT[1]
            for k in range(8):
                S.op("pe", lambda q, k=k: q.transpose(pt2[:, k * 128:(k + 1) * 128], a_[:, k * 128:(k + 1) * 128], G["ident_b"][:]),
                     reads=[a_.k, G["ident_b"].k], writes=[pt2.k])
            S.op("act", lambda q: q.activation(a_T[:].rearrange("p k t -> p (k t)"), pt2[:], AF.Copy), reads=[pt2.k], writes=[a_T.k])

        def mm2(n):
            e_, j = blocks[n]
            i = n % 2
            wo = wout[e_ % 2]
            r0 = e_ * CAP + j * 128
            a_T, y_ = actT[i], yb[i]
            for n2 in range(2):
                for k in range(8):
                    S.op("pe", lambda q, k=k: q.matmul(py[n2][:], a_T[:, k, :], wo[:, k, n2 * 512:(n2 + 1) * 512], start=(k == 0), stop=(k == 7)),
                         reads=[a_T.k, wo.k], writes=[py[n2].k])
                S.op("act", lambda q: q.activation(y_[:, n2 * 512:(n2 + 1) * 512], py[n2][:], AF.Copy), reads=[py[n2].k], writes=[y_.k])
            S.dma("act", lambda q: q.dma_start(out=Dr["Y"][r0:r0 + 128, :], in_=y_[:]), reads=[y_.k])

        for n in range(min(4, NB)):
            load_x(n)
        t_x(0)
        mm1(0)
        t_x(1)
        mm1(1)
        for m in range(NB):
            t_a(m)
            if m + 2 < NB:
                t_x(m + 2)
            mm2(m)
            if m + 4 < NB:
                load_x(m + 4)
            e_, j = blocks[m]
            if j == NBLK - 1 and e_ + 2 < NE:
                load_w(e_ + 2)
            if m + 2 < NB:
                mm1(m + 2)


def phase_combine(ctx, l):
    nc, S, I, Dr, G = ctx.nc, ctx.S, ctx.I, ctx.Dr, ctx.G
    final = (l == 1) or (ctx.upto == 5)
    with ExitStack() as st:
        epi = Epi(ctx, st, l, 1, 5)
        bo = _sb(st, nc, "bo", [NE, D], F32)
        yk = [[_sb(st, nc, f"yk{k}_{i}", [128, D], BF16) for i in range(4)] for k in range(4)]
        gmT = [_sb(st, nc, f"gmT{i}", [NE, 128], F32) for i in range(2)]
        acc = [_sb(st, nc, f"acc{i}", [128, D], F32) for i in range(2)]
        xt = [_sb(st, nc, f"cx{i}", [128, D], F32) for i in range(2)]
        pT = [_ps(st, nc, f"cpT{i}", [128, 512]) for i in range(2)]
        pb = [_ps(st, nc, f"cpb{i}", [128, 512]) for i in range(4)]
        S.dma("sp", lambda e: e.dma_start(out=bo[:], in_=I["moe_b_out"][l]), writes=[bo.k])
        for k in range(4):
            for i in range(4):
                S.op("pool", lambda e: e.memset(yk[k][i][:], 0.0), writes=[yk[k][i].k])

        def gathers(t):
            for k in range(4):
                S.dma("pool", lambda e, k=k: e.indirect_dma_start(
                    out=yk[k][t % 4][:], out_offset=None, in_=Dr["Y"],
                    in_offset=bass.IndirectOffsetOnAxis(ap=G["slots"][:, t, k:k + 1], axis=0),
                    bounds_check=ctx.bc_reg, oob_is_err=False), reads=[G["slots"].kt[t]], writes=[yk[k][t % 4].k])

        gathers(0)
        gathers(1)

        def tile_g(t):
            b = t // 16
            i = t % 2
            x_t = xt[i]
            S.dma("sp", lambda e: e.dma_start(out=x_t[:], in_=Dr["xa"][t * 128:(t + 1) * 128, :]), writes=[x_t.k])
            if t + 2 < NT:
                gathers(t + 2)
            S.op("pe", lambda e: e.transpose(pT[i][0:NE, 0:128], G["gmat"][:, t, :], G["ident_f"][:]),
                 reads=[G["gmat"].kt[t], G["ident_f"].k], writes=[pT[i].k])
            S.op("act", lambda e: e.activation(gmT[i][:], pT[i][0:NE, 0:128], AF.Copy), reads=[pT[i].k], writes=[gmT[i].k])
            yield
            a = acc[i]
            for n in range(2):
                p = pb[i * 2 + n]
                S.op("pe", lambda e: e.matmul(p[:], gmT[i][:], bo[:, n * 512:(n + 1) * 512], start=True, stop=True),
                     reads=[gmT[i].k, bo.k], writes=[p.k])
                S.op("dve", lambda e: e.scalar_tensor_tensor(a[:, n * 512:(n + 1) * 512], yk[0][t % 4][:, n * 512:(n + 1) * 512],
                                                             G["gates"][:, t, 0:1], p[:], ALU.mult, ALU.add),
                     reads=[yk[0][t % 4].k, G["gates"].kt[t], p.k], writes=[a.k])
                yield
            for k in range(1, 4):
                S.op("dve", lambda e, k=k: e.scalar_tensor_tensor(a[:], yk[k][t % 4][:], G["gates"][:, t, k:k + 1], a[:], ALU.mult, ALU.add),
                     reads=[yk[k][t % 4].k, G["gates"].kt[t], a.k], writes=[a.k])
                yield
            dst = (ctx.out if final else Dr["xb"])[t * 128:(t + 1) * 128, :]
            yield from epi.run_g([(a, a[:, 0:512]), (a, a[:, 512:1024])], x_t, b, dst)

        run_interleaved((tile_g(t) for t in range(NT)), 2)


def phase_kvq(ctx):
    nc, S, I, Dr, G = ctx.nc, ctx.S, ctx.I, ctx.Dr, ctx.G
    with ExitStack() as st:
        kvw = _sb(st, nc, "kvw", [128, 8, 2 * D], BF16)
        qw = _sb(st, nc, "qw", [128, 8, D], BF16)
        sc, sh = mod_scale_bias(ctx, st, 1, 0, 1, "m1b")
        xt = [_sb(st, nc, f"kx{i}", [128, D], F32) for i in range(3)]
        x2T = [_sb(st, nc, f"kxT{i}", [128, 8, 512], BF16) for i in range(2)]
        hT = [_sb(st, nc, f"khT{i}", [128, 8, 512], BF16) for i in range(2)]
        ko = [_sb(st, nc, f"ko{i}", [128, 8, 512], BF16) for i in range(2)]
        qo = [_sb(st, nc, f"qo{i}", [128, 8, 512], BF16) for i in range(2)]
        vo = [_sb(st, nc, f"vo{i}", [128, D], BF16) for i in range(2)]
        psT = [_ps(st, nc, f"kpT{i}", [128, 512]) for i in range(2)]
        pk = [_ps(st, nc, f"kpk{i}", [128, 512]) for i in range(2)]
        pq = [_ps(st, nc, f"kpq{i}", [128, 512]) for i in range(2)]
        pv = [_ps(st, nc, f"kpv{i}", [128, 512]) for i in range(2)]
        s1 = I["kv_w"].rearrange("(k p) n -> p k n", p=128)
        s2 = I["q_w"].rearrange("(k p) n -> p k n", p=128)
        for k in range(8):
            S.dma("pool", lambda e, k=k: e.dma_start(out=kvw[:, k, :], in_=s1[:, k, :]), writes=[kvw.k])
            S.dma("pool", lambda e, k=k: e.dma_start(out=qw[:, k, :], in_=s2[:, k, :]), writes=[qw.k])
        ti = 0
        vi = 0
        for j in range(NTOK // 512):
            b = (j * 512) // SEQ
            xT_, h = x2T[j % 2], hT[j % 2]
            for i in range(4):
                x_t = xt[ti % 3]
                ti += 1
                r0 = j * 512 + i * 128
                S.dma("sp", lambda e: e.dma_start(out=x_t[:], in_=Dr["xb"][r0:r0 + 128, :]), writes=[x_t.k])

                def ev(k, p, ap, i=i):
                    S.op("act", lambda e: e.activation(xT_[:, k, i * 128:(i + 1) * 128], ap, AF.Copy), reads=[p.k], writes=[xT_.k])
                    S.op("act", lambda e: e.activation(h[:, k, i * 128:(i + 1) * 128], ap, AF.Identity,
                                                       bias=sh[:, b, k:k + 1], scale=sc[:, b, k:k + 1]),
                         reads=[p.k, sh.k, sc.k], writes=[h.k])
                transpose_tile(ctx, x_t, psT, ev)
            import os
            part = int(os.environ.get("KVQ_PART", "9"))
            if part < 2:
                continue
            k_o, q_o = ko[j % 2], qo[j % 2]
            for m in range(8):
                p1, p2 = pk[m % 2], pq[m % 2]
                for k in range(8):
                    S.op("pe", lambda e, k=k: e.matmul(p1[:], kvw[:, k, m * 128:(m + 1) * 128], xT_[:, k, :], start=(k == 0), stop=(k == 7)),
                         reads=[kvw.k, xT_.k], writes=[p1.k])
                S.op("act", lambda e: e.activation(k_o[:, m, :], p1[:], AF.Copy), reads=[p1.k], writes=[k_o.k])
                for k in range(8):
                    S.op("pe", lambda e, k=k: e.matmul(p2[:], qw[:, k, m * 128:(m + 1) * 128], h[:, k, :], start=(k == 0), stop=(k == 7)),
                         reads=[qw.k, h.k], writes=[p2.k])
                S.op("act", lambda e: e.activation(q_o[:, m, :], p2[:], AF.Copy, scale=0.125), reads=[p2.k], writes=[q_o.k])
            S.dma("sp", lambda e: e.dma_start(out=Dr["kT"][:, :, j * 512:(j + 1) * 512].rearrange("k p t -> p k t"), in_=k_o[:]), reads=[k_o.k])
            S.dma("sp", lambda e: e.dma_start(out=Dr["qT"][:, :, j * 512:(j + 1) * 512].rearrange("k p t -> p k t"), in_=q_o[:]), reads=[q_o.k])
            if part < 3:
                continue
            for i in range(4):
                r0 = j * 512 + i * 128
                v_o = vo[vi % 2]
                vi += 1
                for n in range(2):
                    p = pv[n]
                    for k in range(8):
                        S.op("pe", lambda e, k=k: e.matmul(p[:], xT_[:, k, i * 128:(i + 1) * 128], kvw[:, k, D + n * 512:D + (n + 1) * 512],
                                                          start=(k == 0), stop=(k == 7)), reads=[xT_.k, kvw.k], writes=[p.k])
                    S.op("act", lambda e: e.activation(v_o[:, n * 512:(n + 1) * 512], p[:], AF.Copy), reads=[p.k], writes=[v_o.k])
                S.dma("sp", lambda e: e.dma_start(out=Dr["v"][r0:r0 + 128, :], in_=v_o[:]), reads=[v_o.k])


def phase_attn(ctx):
    nc, S, I, Dr, G = ctx.nc, ctx.S, ctx.I, ctx.Dr, ctx.G
    with ExitStack() as st:
        kT = _sb(st, nc, "akT", [128, 8, SEQ], BF16)
        qT = _sb(st, nc, "aqT", [128, 8, SEQ], BF16)
        v = _sb(st, nc, "av", [128, 16, D], BF16)
        oT = _sb(st, nc, "aoT", [128, 8, SEQ], BF16)
        ntri = _sb(st, nc, "ntri", [128, 128], BF16)
        nones = _sb(st, nc, "nones", [128, 128], BF16)
        mtmp = _sb(st, nc, "mtmp", [128, 512], F32)
        masks = [_sb(st, nc, f"amask{r}", [128, 512], BF16) for r in range(4)]
        e_sb = [_sb(st, nc, f"ae{i}", [128, 512], F32) for i in range(2)]
        sp = [[_sb(st, nc, f"asp{s_}{i}", [128, 512], BF16) for i in range(2)] for s_ in range(2)]
        a_sb = [[_sb(st, nc, f"aa{s_}{i}", [128, 512], BF16) for i in range(2)] for s_ in range(2)]
        acc = [[_sb(st, nc, f"aacc{s_}{i}", [128, 512], BF16) for i in range(2)] for s_ in range(2)]
        pz1 = [_ps(st, nc, f"apz{s_}", [128, 512]) for s_ in range(2)]
        pz = [[pz1[s_], pz1[s_]] for s_ in range(2)]
        pdum = _ps(st, nc, "apdum", [128, 512])
        NWARM = 3

        def warm(n=NWARM):
            for _ in range(n):
                S.op("pe", lambda q: q.matmul(pdum[:], ntri[:], masks[0][:], start=True, stop=True),
                     reads=[ntri.k, masks[0].k], writes=[pdum.k])
        pw = [_ps(st, nc, f"apw{i}", [128, 512]) for i in range(2)]
        po = [_ps(st, nc, f"apo{i}", [128, 512]) for i in range(2)]
        S.op("dve", lambda e: e.tensor_scalar(ntri[:], G["iota"][:], 0.0, -1.0, ALU.is_le, ALU.mult), reads=[G["iota"].k], writes=[ntri.k])
        S.op("dve", lambda e: e.memset(nones[:], -1.0), writes=[nones.k])
        for r in range(4):
            S.op("pool", lambda e, r=r: e.iota(mtmp[:], pattern=[[1, 512]], base=-128 * r, channel_multiplier=-1,
                                              allow_small_or_imprecise_dtypes=True), writes=[mtmp.k])
            S.op("dve", lambda e, r=r: e.tensor_single_scalar(masks[r][:], mtmp[:], 0.0, ALU.is_gt), reads=[mtmp.k], writes=[masks[r].k])
        it = 0
        for b in range(2):
            t0 = b * SEQ
            S.dma("sp", lambda e: e.dma_start(out=kT[:], in_=Dr["kT"][:, :, t0:t0 + SEQ].rearrange("k p t -> p k t")), writes=[kT.k])
            S.dma("sp", lambda e: e.dma_start(out=qT[:], in_=Dr["qT"][:, :, t0:t0 + SEQ].rearrange("k p t -> p k t")), writes=[qT.k])
            S.dma("sp", lambda e: e.dma_start(out=v[:], in_=Dr["v"][t0:t0 + SEQ, :].rearrange("(s p) d -> p s d", p=128)), writes=[v.k])
            for ch in range(8):
                pbs = [0, 64]
                for c in range(4):
                    nkb = 4 * c + 4
                    kbs = list(range(nkb - 1, -1, -1))
                    q_ap = [qT[pb:pb + 64, ch, c * 512:(c + 1) * 512] for pb in pbs]

                    def k_ap(kb, s_):
                        return kT[pbs[s_]:pbs[s_] + 64, ch, kb * 128:(kb + 1) * 128]

                    def stP(idx):
                        kb = kbs[idx]
                        r = kb - 4 * c
                        i2 = idx % 2
                        for s_ in (0, 1):
                            z = pz[s_][i2]
                            S.op("pe", lambda q: q.matmul(z[:], k_ap(kb, s_), q_ap[s_], start=True, stop=True), reads=[kT.k, qT.k], writes=[z.k])
                        warm()
                        for s_ in (0, 1):
                            z = pz[s_][i2]
                            S.op("act", lambda q: q.activation(e_sb[s_][:], z[:], AF.Exp), reads=[z.k], writes=[e_sb[s_].k])
                            S.op("act", lambda q: q.activation(sp[s_][i2][:], e_sb[s_][:], AF.Ln, bias=G["one"][:], scale=1.0),
                                 reads=[e_sb[s_].k, G["one"].k], writes=[sp[s_][i2].k])
                        if r >= 0:
                            for s_ in (0, 1):
                                S.op("dve", lambda q: q.tensor_tensor(sp[s_][i2][:], sp[s_][i2][:], masks[r][:], ALU.mult),
                                     reads=[sp[s_][i2].k, masks[r].k], writes=[sp[s_][i2].k])

                    def stQ(idx):
                        kb = kbs[idx]
                        r = kb - 4 * c
                        i2 = idx % 2
                        for s_ in (0, 1):
                            w = pw[s_]
                            S.op("pe", lambda q: q.matmul(w[:], k_ap(kb, s_), q_ap[s_], start=True, stop=False), reads=[kT.k, qT.k], writes=[w.k])
                            S.op("pe", lambda q: q.matmul(w[:], ntri[:], sp[s_][i2][:], start=False, stop=(idx == 0)),
                                 reads=[ntri.k, sp[s_][i2].k], writes=[w.k])
                            if idx > 0:
                                ac = acc[s_][(idx - 1) % 2]
                                S.op("pe", lambda q: q.matmul(w[:], nones[:], ac[:], start=False, stop=True), reads=[nones.k, ac.k], writes=[w.k])
                        warm()
                        for s_ in (0, 1):
                            S.op("act", lambda q: q.activation(a_sb[s_][i2][:], pw[s_][:], AF.Exp), reads=[pw[s_].k], writes=[a_sb[s_][i2].k])
                        if r >= 0:
                            for s_ in (0, 1):
                                S.op("dve", lambda q: q.tensor_tensor(a_sb[s_][i2][:], a_sb[s_][i2][:], masks[r][:], ALU.mult),
                                     reads=[a_sb[s_][i2].k, masks[r].k], writes=[a_sb[s_][i2].k])
                        for s_ in (0, 1):
                            h = 2 * ch + s_
                            S.op("pe", lambda q: q.matmul(po[s_][pbs[s_]:pbs[s_] + 64, :], v[:, kb, h * 64:(h + 1) * 64], a_sb[s_][i2][:],
                                                          start=(idx == 0), stop=(idx == nkb - 1)),
                                 reads=[v.k, a_sb[s_][i2].k], writes=[po[s_].k])
                        if idx < nkb - 1:
                            for s_ in (0, 1):
                                an = acc[s_][idx % 2]
                                if idx == 0:
                                    S.op("pool", lambda q: q.tensor_copy(an[:], sp[s_][i2][:]), reads=[sp[s_][i2].k], writes=[an.k])
                                else:
                                    S.op("pool", lambda q: q.tensor_tensor(an[:], acc[s_][(idx - 1) % 2][:], sp[s_][i2][:], ALU.add),
                                         reads=[acc[s_][(idx - 1) % 2].k, sp[s_][i2].k], writes=[an.k])

                    stP(0)
                    for idx in range(nkb):
                        if idx + 1 < nkb:
                            stP(idx + 1)
                        stQ(idx)
                    for s_ in (0, 1):
                        pb = 64 * s_
                        S.op("act", lambda q: q.activation(oT[pb:pb + 64, ch, c * 512:(c + 1) * 512], po[s_][pb:pb + 64, :], AF.Copy),
                             reads=[po[s_].k], writes=[oT.k])
            S.dma("sp", lambda e: e.dma_start(out=Dr["oT"][:, :, t0:t0 + SEQ].rearrange("k p t -> p k t"), in_=oT[:]), reads=[oT.k])


def phase_oproj(ctx):
    nc, S, I, Dr, G = ctx.nc, ctx.S, ctx.I, ctx.Dr, ctx.G
    with ExitStack() as st:
        ow = _sb(st, nc, "ow", [128, 8, D], BF16)
        epi = Epi(ctx, st, 1, 0, 2)
        oTb = [_sb(st, nc, f"ooT{i}", [128, 8, 512], BF16) for i in range(2)]
        xt = [_sb(st, nc, f"ox{i}", [128, D], F32) for i in range(2)]
        py = [_ps(st, nc, f"opy{i}", [128, 512]) for i in range(4)]
        s1 = I["o_w"].rearrange("(k p) n -> p k n", p=128)
        for k in range(8):
            S.dma("pool", lambda e, k=k: e.dma_start(out=ow[:, k, :], in_=s1[:, k, :]), writes=[ow.k])
        ti = 0
        for j in range(NTOK // 512):
            b = (j * 512) // SEQ
            o_b = oTb[j % 2]
            S.dma("sp", lambda e: e.dma_start(out=o_b[:], in_=Dr["oT"][:, :, j * 512:(j + 1) * 512].rearrange("k p t -> p k t")), writes=[o_b.k])
            for i in range(4):
                r0 = j * 512 + i * 128
                x_t = xt[ti % 2]
                S.dma("sp", lambda e: e.dma_start(out=x_t[:], in_=Dr["xb"][r0:r0 + 128, :]), writes=[x_t.k])
                ys = []
                for n in range(2):
                    p = py[(ti % 2) * 2 + n]
                    for c in range(8):
                        S.op("pe", lambda e, c=c: e.matmul(p[:], o_b[:, c, i * 128:(i + 1) * 128], ow[:, c, n * 512:(n + 1) * 512],
                                                          start=(c == 0), stop=(c == 7)), reads=[o_b.k, ow.k], writes=[p.k])
                    ys.append((p, p[:]))
                ti += 1
                epi.run(ys, x_t, b, Dr["xa"][r0:r0 + 128, :])


def cast_load(S, dst_ap_fn, src_ap_fn, ncols, wkey, q="pool", step=2048):
    for c0 in range(0, ncols, step):
        c1 = min(ncols, c0 + step)
        S.dma(q, lambda e, c0=c0, c1=c1: e.dma_start(out=dst_ap_fn(c0, c1), in_=src_ap_fn(c0, c1)), writes=[wkey])


def make_in_maps(inp, cores):
    f = lambda a: np.ascontiguousarray(a, dtype=np.float32)
    x, c = inp["x"], inp["c"]
    shared = {
        "ada_w": f(inp["ada_w"]), "ada_b": f(inp["ada_b"]),
        "ln_g": f(inp["ln_g"].reshape(4, D)), "ln_b": f(inp["ln_b"].reshape(4, D)),
        "cv_w1": f(inp["cv_w1"][0]),
        "cv_b1T": f(inp["cv_b1"][0].reshape(16, 128).T),
        "cv_dwT": f(inp["cv_dw"][0].reshape(31, 8, 128).transpose(2, 1, 0)),
        "cv_vecT": f(np.stack([inp["cv_db"][0].reshape(8, 128).T, inp["cv_ln_g"][0].reshape(8, 128).T,
                               inp["cv_ln_b"][0].reshape(8, 128).T], axis=1)),
        "cv_w2": f(inp["cv_w2"][0]), "cv_b2": f(inp["cv_b2"][0].reshape(1, D)),
        "kv_w": f(inp["kv_w"]), "q_w": f(inp["q_w"][0]), "o_w": f(inp["o_w"][0]),
        "router_w": f(inp["router_w"]), "router_b": f(inp["router_b"]),
        "moe_w_in": f(inp["moe_w_in"]), "moe_b_in": f(inp["moe_b_in"]),
        "moe_w_out": f(inp["moe_w_out"]), "moe_b_out": f(inp["moe_b_out"]),
    }
    maps = []
    for ci in cores:
        m = dict(shared)
        m["x"] = f(x[2 * ci:2 * ci + 2].reshape(NTOK, D))
        m["cT"] = f(c[2 * ci:2 * ci + 2].reshape(2, 8, 128).transpose(2, 1, 0))
        maps.append(m)
    return maps


def kernel(**inputs):
    inp = {k: np.asarray(v) for k, v in inputs.items()}
    nc = build_program()
    maps = make_in_maps(inp, list(range(8)))
    res = run_bass_kernel_spmd(nc, maps, core_ids=list(range(8)))
    outs = [r["out"].reshape(2, SEQ, D) for r in res.results]
    return np.concatenate(outs, axis=0).astype(np.float32)
```

```python
import numpy as np
from contextlib import ExitStack
import concourse.bass as bass
import concourse.mybir as mybir
from concourse.bass_utils import run_bass_kernel_spmd

F32 = mybir.dt.float32
BF16 = mybir.dt.bfloat16
I32 = mybir.dt.int32
U32 = mybir.dt.uint32
AF = mybir.ActivationFunctionType
ALU = mybir.AluOpType
AX = mybir.AxisListType


class Tk:
    __slots__ = ("name", "w", "r")

    def __init__(self, name=""):
        self.name = name
        self.w = None
        self.r = {}


class Sched:
    SEM_ROLL = 30000
    NDMA = 8

    def __init__(self, nc, stack):
        self.nc = nc
        self.stack = stack
        self.engs = {"pe": nc.tensor, "dve": nc.vector, "act": nc.scalar,
                     "pool": nc.gpsimd, "sp": nc.sync}
        self.sems = []
        self.owner = []
        self.esem = {}
        self.ecnt = {}
        self.seen = {e: {} for e in self.engs}
        for e in self.engs:
            self._new_esem(e)
        self.dq = {}
        for q in ("sp", "pool", "act"):
            ids = [self._alloc_sem(f"dma_{q}_{i}", None) for i in range(self.NDMA)]
            self.dq[q] = {"ids": ids, "uses": [0] * self.NDMA, "next": 0}
        self.n_inst = 0
        self.n_wait = 0

    def _alloc_sem(self, name, owner):
        h = self.stack.enter_context(self.nc.semaphore(name))
        self.sems.append(h)
        self.owner.append(owner)
        return len(self.sems) - 1

    def _new_esem(self, e):
        sid = self._alloc_sem(f"e_{e}_{len(self.sems)}", e)
        self.esem[e] = sid
        self.ecnt[e] = 0

    def _wait(self, eng, deps):
        e = self.engs[eng]
        seen = self.seen[eng]
        for sid, val in deps.items():
            if seen.get(sid, 0) >= val:
                continue
            e.wait_ge(self.sems[sid], val)
            self.n_wait += 1
            seen[sid] = val

    def _deps(self, eng, reads, writes):
        deps = {}

        def add(tok, raw):
            sid, val = tok
            if self.owner[sid] == eng and not raw:
                return
            if deps.get(sid, 0) < val:
                deps[sid] = val
        for t in reads:
            if t.w is not None:
                add(t.w, True)
        for t in writes:
            if t.w is not None:
                add(t.w, False)
            for sid, val in t.r.items():
                add((sid, val), False)
        return deps

    def _commit(self, tok, reads, writes):
        sid, val = tok
        for t in reads:
            if t.r.get(sid, 0) < val:
                t.r[sid] = val
        for t in writes:
            t.w = tok
            t.r = {}

    def op(self, eng, fn, reads=(), writes=()):
        if self.ecnt[eng] >= self.SEM_ROLL:
            self._new_esem(eng)
        self._wait(eng, self._deps(eng, reads, writes))
        ins = fn(self.engs[eng])
        self.ecnt[eng] += 1
        ins.then_inc(self.sems[self.esem[eng]], 1)
        tok = (self.esem[eng], self.ecnt[eng])
        self._commit(tok, reads, writes)
        self.n_inst += 1
        return tok

    def dma(self, q, fn, reads=(), writes=()):
        d = self.dq[q]
        i = d["next"]
        d["next"] = (i + 1) % self.NDMA
        sid = d["ids"][i]
        deps = self._deps(q, reads, writes)
        if d["uses"][i] > 0:
            v = 16 * d["uses"][i]
            if deps.get(sid, 0) < v:
                deps[sid] = v
        self._wait(q, deps)
        ins = fn(self.engs[q])
        d["uses"][i] += 1
        ins.then_inc(self.sems[sid], 16)
        tok = (sid, 16 * d["uses"][i])
        self._commit(tok, reads, writes)
        self.n_inst += 1
        return tok

    def barrier(self):
        deps = {}
        for e in self.engs:
            if self.ecnt[e] > 0:
                deps[self.esem[e]] = self.ecnt[e]
        for q, d in self.dq.items():
            for i, sid in enumerate(d["ids"]):
                if d["uses"][i] > 0:
                    deps[sid] = 16 * d["uses"][i]
        for e in self.engs:
            dd = {s: v for s, v in deps.items() if self.owner[s] != e}
            self._wait(e, dd)

    def final_wait(self, toks, eng="sp"):
        deps = {}
        for sid, val in toks:
            if deps.get(sid, 0) < val:
                deps[sid] = val
        self._wait(eng, deps)


D = 1024
NTOK = 4096
SEQ = 2048
NT = NTOK // 128
NE = 32
CAP = 1024
NBLK = CAP // 128
ALPHA_DN = 4.0 ** 0.25
LN_EPS = 1e-5
SW_LIM = 7.0
SW_ALPHA = 1.702
BIG = 1.0e6


class B:
    def __init__(self, t):
        self.t = t
        self.k = Tk()

    def __getitem__(self, i):
        return self.t[i]


class K:
    def __init__(self, nc, S, dbg):
        self.nc = nc
        self.S = S
        self.dbg = dbg


_UID = [0]


def _sb(st, nc, name, shape, dt):
    _UID[0] += 1
    return B(st.enter_context(nc.sbuf_tensor(f"s{_UID[0]}_{name}", list(shape), dt)))


def _ps(st, nc, name, shape, dt=F32):
    _UID[0] += 1
    return B(st.enter_context(nc.psum_tensor(f"p{_UID[0]}_{name}", list(shape), dt)))


def build_program(upto=99, dbg=False, start=0, small_moe=False):
    nc = bass.Bass("TRN2", target_bir_lowering=False)
    dt_in = lambda n, s, d=F32: nc.dram_tensor(n, list(s), d, kind="ExternalInput").ap()
    I = {}
    I["x"] = dt_in("x", [NTOK, D])
    I["cT"] = dt_in("cT", [128, 8, 2])
    I["ada_w"] = dt_in("ada_w", [2, D, 6 * D] if start == 0 else [2, 8, 8])
    I["ada_b"] = dt_in("ada_b", [2, 6 * D])
    I["ln_g"] = dt_in("ln_g", [4, D])
    I["ln_b"] = dt_in("ln_b", [4, D])
    I["cv_w1"] = dt_in("cv_w1", [D, 2 * D])
    I["cv_b1T"] = dt_in("cv_b1T", [128, 16])
    I["cv_dwT"] = dt_in("cv_dwT", [128, 8, 31])
    I["cv_vecT"] = dt_in("cv_vecT", [128, 3, 8])
    I["cv_w2"] = dt_in("cv_w2", [D, D])
    I["cv_b2"] = dt_in("cv_b2", [1, D])
    I["kv_w"] = dt_in("kv_w", [D, 2 * D])
    I["q_w"] = dt_in("q_w", [D, D])
    I["o_w"] = dt_in("o_w", [D, D])
    I["router_w"] = dt_in("router_w", [2, D, NE])
    I["router_b"] = dt_in("router_b", [2, NE])
    I["moe_w_in"] = dt_in("moe_w_in", [2, NE, D, 2 * D] if not small_moe else [2, NE, 8, 16])
    I["moe_b_in"] = dt_in("moe_b_in", [2, NE, 2 * D])
    I["moe_w_out"] = dt_in("moe_w_out", [2, NE, D, D] if not small_moe else [2, NE, 8, 8])
    I["moe_b_out"] = dt_in("moe_b_out", [2, NE, D])
    out = nc.dram_tensor("out", [NTOK, D], F32, kind="ExternalOutput").ap()
    scr = lambda n, s, d: nc.dram_tensor(n, list(s), d, kind="Internal").ap()
    Dr = {}
    Dr["modrow"] = scr("modrow", [2, 2, 6 * D], F32)
    Dr["gluT"] = scr("gluT", [8, 128, NTOK], BF16)
    Dr["xa"] = scr("xa", [NTOK, D], F32)
    Dr["xb"] = scr("xb", [NTOK, D], F32)
    Dr["Xe"] = scr("Xe", [NE * CAP, D], BF16)
    Dr["Y"] = scr("Y", [NE * CAP, D], BF16)
    Dr["kT"] = scr("kT", [8, 128, NTOK], BF16)
    Dr["qT"] = scr("qT", [8, 128, NTOK], BF16)
    Dr["v"] = scr("v", [NTOK, D], BF16)
    Dr["oT"] = scr("oT", [8, 128, NTOK], BF16)
    dbg_out = {}
    if dbg:
        dbg_out["d_mod"] = nc.dram_tensor("d_mod", [2, 2, 6 * D], F32, kind="ExternalOutput").ap()
        dbg_out["d_xa"] = nc.dram_tensor("d_xa", [NTOK, D], F32, kind="ExternalOutput").ap()
        dbg_out["d_lg"] = nc.dram_tensor("d_lg", [NTOK, NE], F32, kind="ExternalOutput").ap()
        dbg_out["d_sl"] = nc.dram_tensor("d_sl", [NTOK, 4], I32, kind="ExternalOutput").ap()
        dbg_out["d_gt"] = nc.dram_tensor("d_gt", [NTOK, 4], F32, kind="ExternalOutput").ap()
        if start == 6:
            dbg_out["d_kT"] = nc.dram_tensor("d_kT", [8, 128, NTOK], BF16, kind="ExternalOutput").ap()
            dbg_out["d_v"] = nc.dram_tensor("d_v", [NTOK, D], BF16, kind="ExternalOutput").ap()

    with ExitStack() as gst:
        S = Sched(nc, gst)
        G = {}
        G["ident_f"] = _sb(gst, nc, "ident_f", [128, 128], F32)
        G["ident_b"] = _sb(gst, nc, "ident_b", [128, 128], BF16)
        G["ones_f"] = _sb(gst, nc, "ones_f", [128, 128], F32)
        G["ones_b"] = _sb(gst, nc, "ones_b", [128, 128], BF16)
        G["triU"] = _sb(gst, nc, "triU", [128, 128], F32)
        G["eps"] = _sb(gst, nc, "eps", [128, 1], F32)
        G["one"] = _sb(gst, nc, "one", [128, 1], F32)
        G["slots"] = _sb(gst, nc, "slots", [128, NT, 4], I32)
        G["gates"] = _sb(gst, nc, "gates", [128, NT, 4], F32)
        G["gmat"] = _sb(gst, nc, "gmat", [128, NT, NE], F32)
        G["modT"] = _sb(gst, nc, "modT", [128, 4, 6, 8], F32)
        G["ecap"] = _sb(gst, nc, "ecap", [128, NE], F32)
        tmp = _sb(gst, nc, "tmp_iota", [128, 128], F32)
        G["iota"] = tmp
        S.op("pool", lambda e: e.iota(tmp[:], pattern=[[1, 128]], base=0, channel_multiplier=-1,
                                      allow_small_or_imprecise_dtypes=True), writes=[tmp.k])
        S.op("dve", lambda e: e.tensor_single_scalar(G["ident_f"][:], tmp[:], 0.0, ALU.is_equal),
             reads=[tmp.k], writes=[G["ident_f"].k])
        S.op("dve", lambda e: e.tensor_single_scalar(G["ident_b"][:], tmp[:], 0.0, ALU.is_equal),
             reads=[tmp.k], writes=[G["ident_b"].k])
        S.op("dve", lambda e: e.tensor_single_scalar(G["triU"][:], tmp[:], 0.0, ALU.is_gt),
             reads=[tmp.k], writes=[G["triU"].k])
        S.op("dve", lambda e: e.memset(G["ones_f"][:], 1.0), writes=[G["ones_f"].k])
        S.op("dve", lambda e: e.memset(G["ones_b"][:], 1.0), writes=[G["ones_b"].k])
        S.op("dve", lambda e: e.memset(G["eps"][:], LN_EPS), writes=[G["eps"].k])
        S.op("dve", lambda e: e.memset(G["one"][:], 1.0), writes=[G["one"].k])
        S.op("pool", lambda e: e.iota(G["ecap"][:], pattern=[[CAP, NE]], base=0, channel_multiplier=0,
                                      allow_small_or_imprecise_dtypes=True), writes=[G["ecap"].k])
        ctx = K(nc, S, dbg)
        ctx.I, ctx.Dr, ctx.G, ctx.out, ctx.dbg_out = I, Dr, G, out, dbg_out
        ctx.upto = upto
        ctx.bc_reg = nc.gpsimd.to_reg(NE * CAP - 1)
        for nm in ('slots', 'gates', 'gmat'):
            G[nm].kt = [Tk() for _ in range(NT)]

        phases = [
            ("mod", lambda: phase_mod(ctx)),
            ("glu", lambda: phase_glu(ctx)),
            ("conv", lambda: phase_conv(ctx)),
            ("front0", lambda: phase_front(ctx, 0)),
            ("experts0", lambda: phase_experts(ctx, 0)),
            ("combine0", lambda: phase_combine(ctx, 0)),
            ("kvq", lambda: phase_kvq(ctx)),
            ("attn", lambda: phase_attn(ctx)),
            ("oproj", lambda: phase_oproj(ctx)),
            ("front1", lambda: phase_front(ctx, 1)),
            ("experts1", lambda: phase_experts(ctx, 1)),
            ("combine1", lambda: phase_combine(ctx, 1)),
        ]
        if start > 0:
            din = nc.dram_tensor("d_in_x", [NTOK, D], F32, kind="ExternalInput").ap()
            dstn = "xb" if start in (6, 7, 8) else "xa"
            S.dma("sp", lambda e: e.dma_start(out=Dr[dstn], in_=din))
            dmod = nc.dram_tensor("d_in_mod", [2, 2, 6 * D], F32, kind="ExternalInput").ap()
            S.dma("sp", lambda e: e.dma_start(out=Dr["modrow"], in_=dmod))
            S.barrier()
            load_modT(ctx)
            S.barrier()
        for i, (name, fn) in enumerate(phases):
            if i > upto:
                break
            if i < start:
                continue
            fn()
            S.barrier()
        if dbg:
            dump_dbg(ctx)
            S.barrier()
    return nc


def dump_dbg(ctx):
    nc, S = ctx.nc, ctx.S
    if "d_kT" in ctx.dbg_out:
        S.dma("sp", lambda e: e.dma_start(out=ctx.dbg_out["d_kT"], in_=ctx.Dr["kT"]))
        S.dma("sp", lambda e: e.dma_start(out=ctx.dbg_out["d_v"], in_=ctx.Dr["v"]))
    S.dma("sp", lambda e: e.dma_start(out=ctx.dbg_out["d_mod"], in_=ctx.Dr["modrow"]))
    S.dma("sp", lambda e: e.dma_start(out=ctx.dbg_out["d_xa"], in_=ctx.Dr["xa"]))
    S.dma("sp", lambda e: e.dma_start(out=ctx.dbg_out["d_sl"].rearrange("(t p) k -> p t k", p=128),
                                      in_=ctx.G["slots"][:]), reads=ctx.G["slots"].kt)
    S.dma("sp", lambda e: e.dma_start(out=ctx.dbg_out["d_gt"].rearrange("(t p) k -> p t k", p=128),
                                      in_=ctx.G["gates"][:]), reads=ctx.G["gates"].kt)


def phase_mod(ctx):
    nc, S, I, Dr, G = ctx.nc, ctx.S, ctx.I, ctx.Dr, ctx.G
    with ExitStack() as st:
        cT = _sb(st, nc, "cT", [128, 8, 2], F32)
        caT = _sb(st, nc, "caT", [128, 8, 2], BF16)
        wb = [_sb(st, nc, f"adaw{i}", [128, 8, 3 * D], BF16) for i in range(2)]
        bb = _sb(st, nc, "adab", [1, 2, 6 * D], BF16)
        msb = _sb(st, nc, "modsb", [2, 2, 6 * D], F32)
        pp = [_ps(st, nc, f"pmod{i}", [128, 512]) for i in range(2)]
        S.dma("sp", lambda e: e.dma_start(out=cT[:], in_=I["cT"]), writes=[cT.k])
        for l in range(2):
            cast_load(S, lambda c0, c1, l=l: bb[0:1, l, c0:c1], lambda c0, c1, l=l: I["ada_b"][l:l + 1, c0:c1], 6 * D, bb.k)
        S.op("act", lambda e: e.activation(caT[:], cT[:], AF.Silu), reads=[cT.k], writes=[caT.k])
        it = 0
        for l in range(2):
            for hf in range(2):
                w = wb[it % 2]
                src = I["ada_w"][l, :, hf * 3 * D:(hf + 1) * 3 * D].rearrange("(k p) n -> p k n", p=128)
                for k in range(8):
                    cast_load(S, lambda c0, c1, k=k: w[:, k, c0:c1], lambda c0, c1, k=k: src[:, k, c0:c1], 3 * D, w.k, step=1536)
                for n in range(6):
                    p = pp[n % 2]
                    col = hf * 3 * D + n * 512
                    for k in range(8):
                        S.op("pe", lambda e, k=k: e.matmul(p[0:2, :], caT[:, k, :], w[:, k, n * 512:(n + 1) * 512],
                                                          start=(k == 0), stop=False),
                             reads=[caT.k, w.k], writes=[p.k])
                    S.op("pe", lambda e: e.matmul(p[0:2, :], G["ones_b"][0:1, 0:2], bb[0:1, l, col:col + 512],
                                                  start=False, stop=True),
                         reads=[G["ones_b"].k, bb.k], writes=[p.k])
                    S.op("act", lambda e: e.activation(msb[0:2, l, col:col + 512], p[0:2, :], AF.Copy),
                         reads=[p.k], writes=[msb.k])
                it += 1
        for l in range(2):
            S.dma("sp", lambda e, l=l: e.dma_start(out=Dr["modrow"][l, :, :], in_=msb[0:2, l, :]), reads=[msb.k])
    S.barrier()
    load_modT(ctx)


def load_modT(ctx):
    nc, S, I, Dr, G = ctx.nc, ctx.S, ctx.I, ctx.Dr, ctx.G
    for l in range(2):
        for b in range(2):
            for wch in range(6):
                S.dma("sp", lambda e, l=l, b=b, wch=wch: e.dma_start(
                    out=G["modT"][:, l * 2 + b, wch, :],
                    in_=Dr["modrow"][l, b, wch * D:(wch + 1) * D].rearrange("(k p) -> p k", p=128),
                    allow_slow_non_contiguous=True), writes=[G["modT"].k])


def load_bc(ctx, st, name, src_row):
    t = _sb(st, ctx.nc, name, [128, D], F32)
    ctx.S.dma("sp", lambda e: e.dma_start(out=t[:], in_=src_row.partition_broadcast(128)), writes=[t.k])
    return t


def add_one(ctx, t):
    ctx.S.op("pool", lambda e: e.tensor_scalar_add(t[:], t[:], 1.0), reads=[t.k], writes=[t.k])


def mod_scale_bias(ctx, st, l, w_sh, w_sc, name):
    nc, S, G = ctx.nc, ctx.S, ctx.G
    sc = _sb(st, nc, name + "_sc", [128, 2, 8], F32)
    sh = _sb(st, nc, name + "_sh", [128, 2, 8], F32)
    for b in range(2):
        S.op("dve", lambda e, b=b: e.tensor_scalar_add(sc[:, b, :], G["modT"][:, l * 2 + b, w_sc, :], 1.0),
             reads=[G["modT"].k], writes=[sc.k])
        S.op("dve", lambda e, b=b: e.tensor_copy(sh[:, b, :], G["modT"][:, l * 2 + b, w_sh, :]),
             reads=[G["modT"].k], writes=[sh.k])
    return sc, sh


def run_interleaved(gens, width):
    active = []
    it = iter(gens)
    more = True
    while True:
        while more and len(active) < width:
            try:
                active.append(next(it))
            except StopIteration:
                more = False
        if not active:
            break
        for g in list(active):
            try:
                next(g)
            except StopIteration:
                active.remove(g)


class Epi:
    def __init__(self, ctx, st, l, sub, gate_which):
        nc, S, I, Dr = ctx.nc, ctx.S, ctx.I, ctx.Dr
        self.ctx = ctx
        self.lng = load_bc(ctx, st, f"lng{l}{sub}", I["ln_g"][l * 2 + sub, :])
        self.lnb = load_bc(ctx, st, f"lnb{l}{sub}", I["ln_b"][l * 2 + sub, :])
        self.gate = []
        for b in range(2):
            g = load_bc(ctx, st, f"gate{l}{sub}{b}", Dr["modrow"][l, b, gate_which * D:(gate_which + 1) * D])
            add_one(ctx, g)
            self.gate.append(g)
        self.t1 = [_sb(st, nc, f"ep_t1_{i}", [128, D], F32) for i in range(2)]
        self.r = [_sb(st, nc, f"ep_r_{i}", [128, D], F32) for i in range(2)]
        self.xo = [_sb(st, nc, f"ep_xo_{i}", [128, D], F32) for i in range(2)]
        self.st6 = [_sb(st, nc, f"ep_st_{i}", [128, 2, 6], F32) for i in range(2)]
        self.mv = [_sb(st, nc, f"ep_mv_{i}", [128, 4], F32) for i in range(2)]
        self.n = 0

    def run_g(self, ys, x_t, b, dst):
        S = self.ctx.S
        i = self.n % 2
        self.n += 1
        t1, r, xo, st6, mv = self.t1[i], self.r[i], self.xo[i], self.st6[i], self.mv[i]
        g = self.gate[b]
        for h, (yb, yap) in enumerate(ys):
            S.op("dve", lambda e, h=h, yap=yap: e.tensor_tensor(t1[:, h * 512:(h + 1) * 512], yap,
                                                              g[:, h * 512:(h + 1) * 512], ALU.mult),
                 reads=[yb.k, g.k], writes=[t1.k])
        yield
        S.op("dve", lambda e: e.scalar_tensor_tensor(r[:], x_t[:], ALPHA_DN, t1[:], ALU.mult, ALU.add),
             reads=[x_t.k, t1.k], writes=[r.k])
        yield
        for h in range(2):
            S.op("dve", lambda e, h=h: e.bn_stats(st6[:, h, :], r[:, h * 512:(h + 1) * 512]),
                 reads=[r.k], writes=[st6.k])
        S.op("dve", lambda e: e.bn_aggr(mv[:, 0:2], st6[:].rearrange("p a b -> p (a b)")), reads=[st6.k], writes=[mv.k])
        yield
        S.op("act", lambda e: e.activation(mv[:, 2:3], mv[:, 1:2], AF.Sqrt, bias=self.ctx.G["eps"][:], scale=1.0),
             reads=[mv.k, self.ctx.G["eps"].k], writes=[mv.k])
        yield
        S.op("dve", lambda e: e.reciprocal(mv[:, 2:3], mv[:, 2:3]), reads=[mv.k], writes=[mv.k])
        S.op("dve", lambda e: e.tensor_scalar(mv[:, 3:4], mv[:, 0:1], -1.0, mv[:, 2:3], ALU.mult, ALU.mult),
             reads=[mv.k], writes=[mv.k])
        S.op("act", lambda e: e.activation(t1[:], r[:], AF.Identity, bias=mv[:, 3:4], scale=mv[:, 2:3]),
             reads=[r.k, mv.k], writes=[t1.k])
        yield
        S.op("pool", lambda e: e.tensor_tensor(xo[:], t1[:], self.lng[:], ALU.mult),
             reads=[t1.k, self.lng.k], writes=[xo.k])
        S.op("pool", lambda e: e.tensor_tensor(xo[:], xo[:], self.lnb[:], ALU.add),
             reads=[xo.k, self.lnb.k], writes=[xo.k])
        S.dma("sp", lambda e: e.dma_start(out=dst, in_=xo[:]), reads=[xo.k])
        yield

    def run(self, ys, x_t, b, dst):
        for _ in self.run_g(ys, x_t, b, dst):
            pass


def transpose_tile(ctx, src, psT, evac):
    S, G = ctx.S, ctx.G
    for k in range(8):
        p = psT[k // 4]
        S.op("pe", lambda e, k=k, p=p: e.transpose(p[:, (k % 4) * 128:(k % 4 + 1) * 128], src[:, k * 128:(k + 1) * 128],
                                                 G["ident_f"][:]),
             reads=[src.k, G["ident_f"].k], writes=[p.k])
    for k in range(8):
        p = psT[k // 4]
        evac(k, p, p[:, (k % 4) * 128:(k % 4 + 1) * 128])


def phase_glu(ctx):
    nc, S, I, Dr, G = ctx.nc, ctx.S, ctx.I, ctx.Dr, ctx.G
    with ExitStack() as st:
        w1 = _sb(st, nc, "w1", [128, 8, 2 * D], BF16)
        b1T = _sb(st, nc, "b1T", [128, 16], F32)
        sc, sh = mod_scale_bias(ctx, st, 0, 0, 1, "m1")
        xt = [_sb(st, nc, f"xt{i}", [128, D], F32) for i in range(3)]
        hT = [_sb(st, nc, f"hT{i}", [128, 8, 512], BF16) for i in range(2)]
        sig = [_sb(st, nc, f"sig{i}", [128, 512], F32) for i in range(2)]
        gl = [_sb(st, nc, f"gl{i}", [128, 8, 512], BF16) for i in range(2)]
        psT = [_ps(st, nc, f"psT{i}", [128, 512]) for i in range(2)]
        pa = [_ps(st, nc, f"pa{i}", [128, 512]) for i in range(2)]
        pg = [_ps(st, nc, f"pg{i}", [128, 512]) for i in range(2)]
        src = I["cv_w1"].rearrange("(k p) n -> p k n", p=128)
        for k in range(8):
            S.dma("pool", lambda e, k=k: e.dma_start(out=w1[:, k, :], in_=src[:, k, :]), writes=[w1.k])
        S.dma("sp", lambda e: e.dma_start(out=b1T[:], in_=I["cv_b1T"]), writes=[b1T.k])
        ti = 0
        for j in range(NTOK // 512):
            b = (j * 512) // SEQ
            h = hT[j % 2]
            for i in range(4):
                x_t = xt[ti % 3]
                ti += 1
                r0 = j * 512 + i * 128
                S.dma("sp", lambda e, x_t=x_t, r0=r0: e.dma_start(out=x_t[:], in_=I["x"][r0:r0 + 128, :]), writes=[x_t.k])
                transpose_tile(ctx, x_t, psT, lambda k, p, ap, i=i: S.op(
                    "act", lambda e: e.activation(h[:, k, i * 128:(i + 1) * 128], ap, AF.Identity,
                                                  bias=sh[:, b, k:k + 1], scale=sc[:, b, k:k + 1]),
                    reads=[p.k, sh.k, sc.k], writes=[h.k]))
            g_o = gl[j % 2]
            for m in range(8):
                a_p, g_p, sg = pa[m % 2], pg[m % 2], sig[m % 2]
                for k in range(8):
                    S.op("pe", lambda e, k=k: e.matmul(a_p[:], w1[:, k, m * 128:(m + 1) * 128], h[:, k, :],
                                                      start=(k == 0), stop=(k == 7)), reads=[w1.k, h.k], writes=[a_p.k])
                for k in range(8):
                    S.op("pe", lambda e, k=k: e.matmul(g_p[:], w1[:, k, D + m * 128:D + (m + 1) * 128], h[:, k, :],
                                                      start=(k == 0), stop=(k == 7)), reads=[w1.k, h.k], writes=[g_p.k])
                S.op("act", lambda e: e.activation(sg[:], g_p[:], AF.Sigmoid, bias=b1T[:, 8 + m:9 + m], scale=1.0),
                     reads=[g_p.k, b1T.k], writes=[sg.k])
                S.op("dve", lambda e: e.scalar_tensor_tensor(g_o[:, m, :], a_p[:], b1T[:, m:m + 1], sg[:], ALU.add, ALU.mult),
                     reads=[a_p.k, b1T.k, sg.k], writes=[g_o.k])
            S.dma("sp", lambda e, j=j: e.dma_start(out=Dr["gluT"][:, :, j * 512:(j + 1) * 512].rearrange("k p t -> p k t"),
                                                  in_=g_o[:]), reads=[g_o.k])


def phase_conv(ctx):
    nc, S, I, Dr, G = ctx.nc, ctx.S, ctx.I, ctx.Dr, ctx.G
    with ExitStack() as st:
        dwT = _sb(st, nc, "dwT", [128, 8, 31], F32)
        vecT = _sb(st, nc, "vecT", [128, 3, 8], F32)
        dg = _sb(st, nc, "dg", [128, 8 * 31, 128], BF16)
        w2 = _sb(st, nc, "w2", [128, 8, D], BF16)
        b2 = _sb(st, nc, "b2", [1, D], BF16)
        epi = Epi(ctx, st, 0, 0, 2)
        glb = [_sb(st, nc, f"glb{i}", [128, 8, 544], BF16) for i in range(2)]
        vb = _sb(st, nc, "vb", [128, 8, 512], BF16)
        vsq = [_sb(st, nc, f"vsq{i}", [128, 512], BF16) for i in range(2)]
        sT = _sb(st, nc, "sT", [128, 8, 512], BF16)
        mean = _sb(st, nc, "cmean", [128, 512], F32)
        msq = _sb(st, nc, "cmsq", [128, 512], F32)
        rstd = _sb(st, nc, "crstd", [128, 512], F32)
        nmr = _sb(st, nc, "cnmr", [128, 512], F32)
        zt = [_sb(st, nc, f"czt{i}", [128, 512], F32) for i in range(2)]
        xt = [_sb(st, nc, f"cxt{i}", [128, D], F32) for i in range(2)]
        pc = [_ps(st, nc, f"pc{i}", [128, 512]) for i in range(2)]
        ps1 = _ps(st, nc, "ps1", [128, 512])
        ps2 = _ps(st, nc, "ps2", [128, 512])
        py = [_ps(st, nc, f"py{i}", [128, 512]) for i in range(4)]
        S.dma("sp", lambda e: e.dma_start(out=dwT[:], in_=I["cv_dwT"]), writes=[dwT.k])
        S.dma("sp", lambda e: e.dma_start(out=vecT[:], in_=I["cv_vecT"]), writes=[vecT.k])
        S.dma("pool", lambda e: e.dma_start(out=b2[:], in_=I["cv_b2"]), writes=[b2.k])
        src = I["cv_w2"].rearrange("(k p) n -> p k n", p=128)
        for k in range(8):
            S.dma("pool", lambda e, k=k: e.dma_start(out=w2[:, k, :], in_=src[:, k, :]), writes=[w2.k])
        for c in range(8):
            for k in range(31):
                S.op("dve", lambda e, c=c, k=k: e.tensor_scalar(dg[:, c * 31 + k, :], G["ident_b"][:], dwT[:, c, k:k + 1], None,
                                                              ALU.mult),
                     reads=[G["ident_b"].k, dwT.k], writes=[dg.k])
        vb2 = [vb, _sb(st, nc, "vb_b", [128, 8, 512], BF16)]
        mean2 = [mean, _sb(st, nc, "cmean_b", [128, 512], F32)]
        rstd2 = [rstd, _sb(st, nc, "crstd_b", [128, 512], F32)]
        nmr2 = [nmr, _sb(st, nc, "cnmr_b", [128, 512], F32)]
        tcnt = [0]

        def conv_stage(j):
            t0 = j * 512
            g_in = glb[j % 2]
            vbj, meanj, rstdj, nmrj = vb2[j % 2], mean2[j % 2], rstd2[j % 2], nmr2[j % 2]
            if t0 % SEQ == 0:
                S.op("pool", lambda e: e.memset(g_in[:, :, 0:30], 0.0), writes=[g_in.k])
                S.dma("sp", lambda e: e.dma_start(out=g_in[:, :, 30:542],
                                                  in_=Dr["gluT"][:, :, t0:t0 + 512].rearrange("k p t -> p k t")),
                      writes=[g_in.k])
            else:
                S.dma("sp", lambda e: e.dma_start(out=g_in[:, :, 0:542],
                                                  in_=Dr["gluT"][:, :, t0 - 30:t0 + 512].rearrange("k p t -> p k t")),
                      writes=[g_in.k])
            for c in range(8):
                p = pc[c % 2]
                vq = vsq[c % 2]
                for k in range(31):
                    S.op("pe", lambda e, k=k: e.matmul(p[:], dg[:, c * 31 + k, :], g_in[:, c, k:k + 512],
                                                      start=(k == 0), stop=(k == 30)),
                         reads=[dg.k, g_in.k], writes=[p.k])
                S.op("act", lambda e: e.activation(vbj[:, c, :], p[:], AF.Identity, bias=vecT[:, 0, c:c + 1], scale=1.0),
                     reads=[p.k, vecT.k], writes=[vbj.k])
                S.op("act", lambda e: e.activation(vq[:], p[:], AF.Square, bias=vecT[:, 0, c:c + 1], scale=1.0),
                     reads=[p.k, vecT.k], writes=[vq.k])
                S.op("pe", lambda e: e.matmul(ps1[:], G["ones_b"][:], vbj[:, c, :], start=(c == 0), stop=(c == 7)),
                     reads=[G["ones_b"].k, vbj.k], writes=[ps1.k])
                S.op("pe", lambda e: e.matmul(ps2[:], G["ones_b"][:], vq[:], start=(c == 0), stop=(c == 7)),
                     reads=[G["ones_b"].k, vq.k], writes=[ps2.k])
            S.op("act", lambda e: e.activation(meanj[:], ps1[:], AF.Copy, scale=1.0 / D), reads=[ps1.k], writes=[meanj.k])
            S.op("act", lambda e: e.activation(msq[:], ps1[:], AF.Square, scale=1.0 / D), reads=[ps1.k], writes=[msq.k])
            S.op("dve", lambda e: e.scalar_tensor_tensor(rstdj[:], ps2[:], 1.0 / D, msq[:], ALU.mult, ALU.subtract),
                 reads=[ps2.k, msq.k], writes=[rstdj.k])
            S.op("act", lambda e: e.activation(rstdj[:], rstdj[:], AF.Sqrt, bias=G["eps"][:], scale=1.0),
                 reads=[rstdj.k, G["eps"].k], writes=[rstdj.k])
            S.op("dve", lambda e: e.reciprocal(rstdj[:], rstdj[:]), reads=[rstdj.k], writes=[rstdj.k])
            S.op("dve", lambda e: e.scalar_tensor_tensor(nmrj[:], meanj[:], -1.0, rstdj[:], ALU.mult, ALU.mult),
                 reads=[meanj.k, rstdj.k], writes=[nmrj.k])

        def rest_stage(j):
            b = (j * 512) // SEQ
            t0 = j * 512
            vbj, rstdj, nmrj = vb2[j % 2], rstd2[j % 2], nmr2[j % 2]
            for c in range(8):
                z = zt[c % 2]
                S.op("dve", lambda e: e.tensor_tensor(z[:], vbj[:, c, :], rstdj[:], ALU.mult), reads=[vbj.k, rstdj.k], writes=[z.k])
                S.op("pool", lambda e: e.tensor_tensor(z[:], z[:], nmrj[:], ALU.add), reads=[z.k, nmrj.k], writes=[z.k])
                S.op("act", lambda e: e.activation(sT[:, c, :], z[:], AF.Silu, bias=vecT[:, 2, c:c + 1],
                                                   scale=vecT[:, 1, c:c + 1]),
                     reads=[z.k, vecT.k], writes=[sT.k])
            for i in range(4):
                ti = tcnt[0]
                r0 = t0 + i * 128
                x_t = xt[ti % 2]
                S.dma("sp", lambda e: e.dma_start(out=x_t[:], in_=I["x"][r0:r0 + 128, :]), writes=[x_t.k])
                ys = []
                for n in range(2):
                    p = py[(ti % 2) * 2 + n]
                    for c in range(8):
                        S.op("pe", lambda e, c=c: e.matmul(p[:], sT[:, c, i * 128:(i + 1) * 128], w2[:, c, n * 512:(n + 1) * 512],
                                                          start=(c == 0), stop=False), reads=[sT.k, w2.k], writes=[p.k])
                    S.op("pe", lambda e: e.matmul(p[:], G["ones_b"][0:1, :], b2[0:1, n * 512:(n + 1) * 512], start=False, stop=True),
                         reads=[G["ones_b"].k, b2.k], writes=[p.k])
                    ys.append((p, p[:]))
                tcnt[0] += 1
                epi.run(ys, x_t, b, Dr["xa"][r0:r0 + 128, :])

        NBK = NTOK // 512
        conv_stage(0)
        for j in range(NBK):
            if j + 1 < NBK:
                conv_stage(j + 1)
            rest_stage(j)


def phase_front(ctx, l):
    nc, S, I, Dr, G = ctx.nc, ctx.S, ctx.I, ctx.Dr, ctx.G
    with ExitStack() as st:
        S2, H2 = [], []
        for b in range(2):
            s2 = load_bc(ctx, st, f"S2_{b}", Dr["modrow"][l, b, 4 * D:5 * D])
            add_one(ctx, s2)
            S2.append(s2)
            H2.append(load_bc(ctx, st, f"H2_{b}", Dr["modrow"][l, b, 3 * D:4 * D]))
        rw = _sb(st, nc, "rw", [128, 8, NE], F32)
        rb = _sb(st, nc, "rb", [1, NE], F32)
        srun = _sb(st, nc, "srun", [128, NE], F32)
        xt = [_sb(st, nc, f"fx{i}", [128, D], F32) for i in range(4)]
        h2 = [_sb(st, nc, f"fh{i}", [128, D], F32) for i in range(4)]
        h2b = [_sb(st, nc, f"fhb{i}", [128, D], BF16) for i in range(5)]
        h2T = [_sb(st, nc, f"fhT{i}", [128, 8, 128], F32) for i in range(4)]
        sm = lambda nm, w: [_sb(st, nc, f"{nm}{i}", [128, w], F32) for i in range(4)]
        lg, top8, nv0, ex, ssum, mask, slotv, bad, oh, junk, slotf, okk, g4, gtmp = (
            sm("lg", NE), sm("top8", 8), sm("nv0", 1), sm("ex", 4), sm("ssum", 1), sm("mask", NE), sm("slotv", NE),
            sm("bad", NE), sm("oh", NE), sm("junk", NE), sm("slotf", 4), sm("okk", 4), sm("g4", 4), sm("gtmp", NE))
        cur = sm("cur", NE)
        psT = [_ps(st, nc, f"fpT{i}", [128, 512]) for i in range(2)]
        plg = [_ps(st, nc, f"fplg{i}", [128, 512]) for i in range(4)]
        S.dma("sp", lambda e: e.dma_start(out=rw[:], in_=I["router_w"][l].rearrange("(k p) e -> p k e", p=128)), writes=[rw.k])
        S.dma("sp", lambda e: e.dma_start(out=rb[:], in_=I["router_b"][l:l + 1, :]), writes=[rb.k])
        S.op("dve", lambda e: e.memset(srun[:], 0.0), writes=[srun.k])
        def tile_g(t):
            b = t // 16
            i = t % 4
            x_t, h, hb, hT = xt[i], h2[i], h2b[t % 5], h2T[i]
            S.dma("sp", lambda e: e.dma_start(out=x_t[:], in_=Dr["xa"][t * 128:(t + 1) * 128, :]), writes=[x_t.k])
            S.op("dve", lambda e: e.tensor_tensor(h[:], x_t[:], S2[b][:], ALU.mult), reads=[x_t.k, S2[b].k], writes=[h.k])
            S.op("dve", lambda e: e.tensor_tensor(h[:], h[:], H2[b][:], ALU.add), reads=[h.k, H2[b].k], writes=[h.k])
            S.op("act", lambda e: e.activation(hb[:], h[:], AF.Copy), reads=[h.k], writes=[hb.k])
            yield
            transpose_tile(ctx, h, psT, lambda k, p, ap: S.op(
                "act", lambda e: e.activation(hT[:, k, :], ap, AF.Copy), reads=[p.k], writes=[hT.k]))
            yield
            pl = plg[i]
            for k in range(8):
                S.op("pe", lambda e, k=k: e.matmul(pl[:, 0:NE], hT[:, k, :], rw[:, k, :], start=(k == 0), stop=False),
                     reads=[hT.k, rw.k], writes=[pl.k])
            S.op("pe", lambda e: e.matmul(pl[:, 0:NE], G["ones_f"][0:1, :], rb[0:1, :], start=False, stop=True),
                 reads=[G["ones_f"].k, rb.k], writes=[pl.k])
            S.op("dve", lambda e: e.tensor_copy(lg[i][:], pl[:, 0:NE]), reads=[pl.k], writes=[lg[i].k])
            yield
            S.op("dve", lambda e: e.tensor_copy(cur[i][:], lg[i][:]), reads=[lg[i].k], writes=[cur[i].k])
            yield
            for k in range(4):
                S.op("dve", lambda e, k=k: e.tensor_reduce(top8[i][:, k:k + 1], cur[i][:], AX.X, ALU.max),
                     reads=[cur[i].k], writes=[top8[i].k])
                if k < 3:
                    S.op("dve", lambda e, k=k: e.tensor_scalar(oh[i][:], cur[i][:], top8[i][:, k:k + 1], None, ALU.is_equal),
                         reads=[cur[i].k, top8[i].k], writes=[oh[i].k])
                    S.op("dve", lambda e: e.scalar_tensor_tensor(cur[i][:], oh[i][:], -BIG, cur[i][:], ALU.mult, ALU.add),
                         reads=[oh[i].k, cur[i].k], writes=[cur[i].k])
                yield
            S.op("dve", lambda e: e.tensor_scalar_mul(nv0[i][:], top8[i][:, 0:1], -1.0), reads=[top8[i].k], writes=[nv0[i].k])
            S.op("act", lambda e: e.activation(ex[i][:], top8[i][:, 0:4], AF.Exp, bias=nv0[i][:], scale=1.0),
                 reads=[top8[i].k, nv0[i].k], writes=[ex[i].k])
            S.op("dve", lambda e: e.tensor_reduce(ssum[i][:], ex[i][:], AX.X, ALU.add), reads=[ex[i].k], writes=[ssum[i].k])
            S.op("dve", lambda e: e.reciprocal(ssum[i][:], ssum[i][:]), reads=[ssum[i].k], writes=[ssum[i].k])
            S.op("dve", lambda e: e.tensor_scalar(g4[i][:], ex[i][:], ssum[i][:], None, ALU.mult),
                 reads=[ex[i].k, ssum[i].k], writes=[g4[i].k])
            yield
            S.op("dve", lambda e: e.tensor_scalar(mask[i][:], lg[i][:], top8[i][:, 3:4], None, ALU.is_ge),
                 reads=[lg[i].k, top8[i].k], writes=[mask[i].k])
            pp = plg[i]
            S.op("pe", lambda e: e.matmul(pp[:, 64:64 + NE], G["triU"][:], mask[i][:], start=True, stop=False),
                 reads=[G["triU"].k, mask[i].k], writes=[pp.k])
            S.op("pe", lambda e: e.matmul(pp[:, 64:64 + NE], G["ones_f"][:], srun[:], start=False, stop=True),
                 reads=[G["ones_f"].k, srun.k], writes=[pp.k])
            S.op("dve", lambda e: e.tensor_tensor(srun[:], srun[:], mask[i][:], ALU.add), reads=[srun.k, mask[i].k], writes=[srun.k])
            yield
            S.op("dve", lambda e: e.tensor_single_scalar(bad[i][:], pp[:, 64:64 + NE], float(CAP) - 0.5, ALU.is_ge),
                 reads=[pp.k], writes=[bad[i].k])
            S.op("dve", lambda e: e.tensor_tensor(slotv[i][:], pp[:, 64:64 + NE], G["ecap"][:], ALU.add),
                 reads=[pp.k, G["ecap"].k], writes=[slotv[i].k])
            S.op("dve", lambda e: e.scalar_tensor_tensor(slotv[i][:], bad[i][:], BIG, slotv[i][:], ALU.mult, ALU.add),
                 reads=[bad[i].k, slotv[i].k], writes=[slotv[i].k])
            yield
            for k in range(4):
                S.op("dve", lambda e, k=k: e.tensor_scalar(oh[i][:], lg[i][:], top8[i][:, k:k + 1], None, ALU.is_equal),
                     reads=[lg[i].k, top8[i].k], writes=[oh[i].k])
                S.op("dve", lambda e: e.tensor_tensor(junk[i][:], oh[i][:], slotv[i][:], ALU.mult),
                     reads=[oh[i].k, slotv[i].k], writes=[junk[i].k])
                S.op("dve", lambda e, k=k: e.tensor_reduce(slotf[i][:, k:k + 1], junk[i][:], AX.X, ALU.add),
                     reads=[junk[i].k], writes=[slotf[i].k])
                yield
            S.op("dve", lambda e: e.tensor_single_scalar(okk[i][:], slotf[i][:], 1.0e5, ALU.is_lt), reads=[slotf[i].k], writes=[okk[i].k])
            yield
            gk, sk, mk = G["gates"].kt[t], G["slots"].kt[t], G["gmat"].kt[t]
            S.op("dve", lambda e: e.tensor_tensor(G["gates"][:, t, :], g4[i][:], okk[i][:], ALU.mult),
                 reads=[g4[i].k, okk[i].k], writes=[gk])
            S.op("dve", lambda e: e.tensor_copy(G["slots"][:, t, :], slotf[i][:]), reads=[slotf[i].k], writes=[sk])
            yield
            for k in range(4):
                dstm = G["gmat"][:, t, :] if k == 0 else gtmp[i][:]
                S.op("dve", lambda e, k=k, dstm=dstm: e.tensor_scalar(dstm, lg[i][:], top8[i][:, k:k + 1], G["gates"][:, t, k:k + 1],
                                                                    ALU.is_equal, ALU.mult),
                     reads=[lg[i].k, top8[i].k, gk], writes=[mk if k == 0 else gtmp[i].k])
                if k > 0:
                    S.op("dve", lambda e: e.tensor_tensor(G["gmat"][:, t, :], G["gmat"][:, t, :], gtmp[i][:], ALU.add),
                         reads=[mk, gtmp[i].k], writes=[mk])
            for k in range(4):
                S.dma("pool", lambda e, k=k: e.indirect_dma_start(
                    out=Dr["Xe"], out_offset=bass.IndirectOffsetOnAxis(ap=G["slots"][:, t, k:k + 1], axis=0),
                    in_=hb[:], in_offset=None, bounds_check=ctx.bc_reg, oob_is_err=False), reads=[hb.k, sk])
            yield

        run_interleaved((tile_g(t) for t in range(NT)), 4)


def phase_experts(ctx, l):
    nc, S, I, Dr, G = ctx.nc, ctx.S, ctx.I, ctx.Dr, ctx.G
    with ExitStack() as st:
        win = [_sb(st, nc, f"win{i}", [128, 8, 2 * D], BF16) for i in range(2)]
        wout = [_sb(st, nc, f"wout{i}", [128, 8, D], BF16) for i in range(2)]
        bin_ = [_sb(st, nc, f"bin{i}", [128, 2 * D], F32) for i in range(2)]
        xe = [_sb(st, nc, f"xe{i}", [128, D], BF16) for i in range(2)]
        xT = [_sb(st, nc, f"xT{i}", [128, 8, 128], BF16) for i in range(2)]
        xg = [_sb(st, nc, f"xg{i}", [128, 512], F32) for i in range(2)]
        sg = [_sb(st, nc, f"sg{i}", [128, 512], F32) for i in range(2)]
        xl = [_sb(st, nc, f"xl{i}", [128, 512], F32) for i in range(2)]
        tt = [_sb(st, nc, f"tt{i}", [128, 512], F32) for i in range(2)]
        act = [_sb(st, nc, f"act{i}", [128, D], BF16) for i in range(2)]
        actT = [_sb(st, nc, f"actT{i}", [128, 8, 128], BF16) for i in range(2)]
        yb = [_sb(st, nc, f"yb{i}", [128, D], BF16) for i in range(2)]
        psT = [_ps(st, nc, f"epT{i}", [128, 1024], BF16) for i in range(2)]
        pu = [_ps(st, nc, f"epu{i}", [128, 512]) for i in range(4)]
        py = [_ps(st, nc, f"epy{i}", [128, 512]) for i in range(2)]

        def load_w(e):
            w, wo, bi = win[e % 2], wout[e % 2], bin_[e % 2]
            s1 = I["moe_w_in"][l, e].rearrange("(k p) n -> p k n", p=128)
            s2 = I["moe_w_out"][l, e].rearrange("(k p) n -> p k n", p=128)
            for k in range(8):
                S.dma("pool", lambda q, k=k: q.dma_start(out=w[:, k, :], in_=s1[:, k, :]), writes=[w.k])
            for k in range(8):
                S.dma("pool", lambda q, k=k: q.dma_start(out=wo[:, k, :], in_=s2[:, k, :]), writes=[wo.k])
            S.dma("sp", lambda q: q.dma_start(out=bi[:], in_=I["moe_b_in"][l, e, :].partition_broadcast(128)), writes=[bi.k])

        load_w(0)
        load_w(1)
        blocks = [(e_, j) for e_ in range(NE) for j in range(NBLK)]
        NB = len(blocks)
        xe4 = xe + [_sb(st, nc, f"xe{i}", [128, D], BF16) for i in range(2, 4)]

        def load_x(n):
            e_, j = blocks[n]
            r0 = e_ * CAP + j * 128
            x_e = xe4[n % 4]
            S.dma("sp", lambda q: q.dma_start(out=x_e[:], in_=Dr["Xe"][r0:r0 + 128, :]), writes=[x_e.k])

        def t_x(n):
            x_e, x_T = xe4[n % 4], xT[n % 2]
            pt = psT[0]
            for k in range(8):
                S.op("pe", lambda q, k=k: q.transpose(pt[:, k * 128:(k + 1) * 128], x_e[:, k * 128:(k + 1) * 128], G["ident_b"][:]),
                     reads=[x_e.k, G["ident_b"].k], writes=[pt.k])
            S.op("act", lambda q: q.activation(x_T[:].rearrange("p k t -> p (k t)"), pt[:], AF.Copy), reads=[pt.k], writes=[x_T.k])

        def mm1(n):
            e_, j = blocks[n]
            i = n % 2
            w, bi = win[e_ % 2], bin_[e_ % 2]
            x_T, a_ = xT[i], act[i]
            for hf in range(2):
                pg, pl = pu[hf * 2], pu[hf * 2 + 1]
                for (p, c0) in ((pg, hf * 512), (pl, D + hf * 512)):
                    for k in range(8):
                        S.op("pe", lambda q, k=k: q.matmul(p[:], x_T[:, k, :], w[:, k, c0:c0 + 512], start=(k == 0), stop=(k == 7)),
                             reads=[x_T.k, w.k], writes=[p.k])
                S.op("dve", lambda q: q.tensor_tensor(xg[hf][:], pg[:], bi[:, hf * 512:(hf + 1) * 512], ALU.add), reads=[pg.k, bi.k], writes=[xg[hf].k])
                S.op("dve", lambda q: q.tensor_scalar_min(xg[hf][:], xg[hf][:], SW_LIM), reads=[xg[hf].k], writes=[xg[hf].k])
                S.op("act", lambda q: q.activation(sg[hf][:], xg[hf][:], AF.Sigmoid, scale=SW_ALPHA), reads=[xg[hf].k], writes=[sg[hf].k])
                S.op("dve", lambda q: q.tensor_tensor(xl[hf][:], pl[:], bi[:, D + hf * 512:D + (hf + 1) * 512], ALU.add), reads=[pl.k, bi.k], writes=[xl[hf].k])
                S.op("dve", lambda q: q.tensor_scalar(xl[hf][:], xl[hf][:], SW_LIM, -SW_LIM, ALU.min, ALU.max), reads=[xl[hf].k], writes=[xl[hf].k])
                S.op("dve", lambda q: q.scalar_tensor_tensor(tt[hf][:], xl[hf][:], 1.0, xg[hf][:], ALU.add, ALU.mult),
                     reads=[xl[hf].k, xg[hf].k], writes=[tt[hf].k])
                S.op("dve", lambda q: q.tensor_tensor(a_[:, hf * 512:(hf + 1) * 512], tt[hf][:], sg[hf][:], ALU.mult),
                     reads=[tt[hf].k, sg[hf].k], writes=[a_.k])

        def t_a(n):
            i = n % 2
            a_, a_T = act[i], actT[i]
            pt2 = psT[1]
            for k in range(8):
                S.op("pe", lambda q, k=k: q.transpose(pt2[:, k * 128:(k + 1) * 128], a_[:, k * 128:(k + 1) * 128], G["ident_b"][:]),
                     reads=[a_.k, G["ident_b"].k], writes=[pt2.k])
            S.op("act", lambda q: q.activation(a_T[:].rearrange("p k t -> p (k t)"), pt2[:], AF.Copy), reads=[pt2.k], writes=[a_T.k])

        def mm2(n):
            e_, j = blocks[n]
            i = n % 2
            wo = wout[e_ % 2]
            r0 = e_ * CAP + j * 128
            a_T, y_ = actT[i], yb[i]
            for n2 in range(2):
                for k in range(8):
                    S.op("pe", lambda q, k=k: q.matmul(py[n2][:], a_T[:, k, :], wo[:, k, n2 * 512:(n2 + 1) * 512], start=(k == 0), stop=(k == 7)),
                         reads=[a_T.k, wo.k], writes=[py[n2].k])
                S.op("act", lambda q: q.activation(y_[:, n2 * 512:(n2 + 1) * 512], py[n2][:], AF.Copy), reads=[py[n2].k], writes=[y_.k])
            S.dma("act", lambda q: q.dma_start(out=Dr["Y"][r0:r0 + 128, :], in_=y_[:]), reads=[y_.k])

        for n in range(min(4, NB)):
            load_x(n)
        t_x(0)
        mm1(0)
        t_x(1)
        mm1(1)
        for m in range(NB):
            t_a(m)
            if m + 2 < NB:
                t_x(m + 2)
            mm2(m)
            if m + 4 < NB:
                load_x(m + 4)
            e_, j = blocks[m]
            if j == NBLK - 1 and e_ + 2 < NE:
                load_w(e_ + 2)
            if m + 2 < NB:
                mm1(m + 2)


def phase_combine(ctx, l):
    nc, S, I, Dr, G = ctx.nc, ctx.S, ctx.I, ctx.Dr, ctx.G
    final = (l == 1) or (ctx.upto == 5)
    with ExitStack() as st:
        epi = Epi(ctx, st, l, 1, 5)
        bo = _sb(st, nc, "bo", [NE, D], F32)
        yk = [[_sb(st, nc, f"yk{k}_{i}", [128, D], BF16) for i in range(4)] for k in range(4)]
        gmT = [_sb(st, nc, f"gmT{i}", [NE, 128], F32) for i in range(2)]
        acc = [_sb(st, nc, f"acc{i}", [128, D], F32) for i in range(2)]
        xt = [_sb(st, nc, f"cx{i}", [128, D], F32) for i in range(2)]
        pT = [_ps(st, nc, f"cpT{i}", [128, 512]) for i in range(2)]
        pb = [_ps(st, nc, f"cpb{i}", [128, 512]) for i in range(4)]
        S.dma("sp", lambda e: e.dma_start(out=bo[:], in_=I["moe_b_out"][l]), writes=[bo.k])
        for k in range(4):
            for i in range(4):
                S.op("pool", lambda e: e.memset(yk[k][i][:], 0.0), writes=[yk[k][i].k])

        def gathers(t):
            for k in range(4):
                S.dma("pool", lambda e, k=k: e.indirect_dma_start(
                    out=yk[k][t % 4][:], out_offset=None, in_=Dr["Y"],
                    in_offset=bass.IndirectOffsetOnAxis(ap=G["slots"][:, t, k:k + 1], axis=0),
                    bounds_check=ctx.bc_reg, oob_is_err=False), reads=[G["slots"].kt[t]], writes=[yk[k][t % 4].k])

        gathers(0)
        gathers(1)

        def tile_g(t):
            b = t // 16
            i = t % 2
            x_t = xt[i]
            S.dma("sp", lambda e: e.dma_start(out=x_t[:], in_=Dr["xa"][t * 128:(t + 1) * 128, :]), writes=[x_t.k])
            if t + 2 < NT:
                gathers(t + 2)
            S.op("pe", lambda e: e.transpose(pT[i][0:NE, 0:128], G["gmat"][:, t, :], G["ident_f"][:]),
                 reads=[G["gmat"].kt[t], G["ident_f"].k], writes=[pT[i].k])
            S.op("act", lambda e: e.activation(gmT[i][:], pT[i][0:NE, 0:128], AF.Copy), reads=[pT[i].k], writes=[gmT[i].k])
            yield
            a = acc[i]
            for n in range(2):
                p = pb[i * 2 + n]
                S.op("pe", lambda e: e.matmul(p[:], gmT[i][:], bo[:, n * 512:(n + 1) * 512], start=True, stop=True),
                     reads=[gmT[i].k, bo.k], writes=[p.k])
                S.op("dve", lambda e: e.scalar_tensor_tensor(a[:, n * 512:(n + 1) * 512], yk[0][t % 4][:, n * 512:(n + 1) * 512],
                                                             G["gates"][:, t, 0:1], p[:], ALU.mult, ALU.add),
                     reads=[yk[0][t % 4].k, G["gates"].kt[t], p.k], writes=[a.k])
                yield
            for k in range(1, 4):
                S.op("dve", lambda e, k=k: e.scalar_tensor_tensor(a[:], yk[k][t % 4][:], G["gates"][:, t, k:k + 1], a[:], ALU.mult, ALU.add),
                     reads=[yk[k][t % 4].k, G["gates"].kt[t], a.k], writes=[a.k])
                yield
            dst = (ctx.out if final else Dr["xb"])[t * 128:(t + 1) * 128, :]
            yield from epi.run_g([(a, a[:, 0:512]), (a, a[:, 512:1024])], x_t, b, dst)

        run_interleaved((tile_g(t) for t in range(NT)), 2)


def phase_kvq(ctx):
    nc, S, I, Dr, G = ctx.nc, ctx.S, ctx.I, ctx.Dr, ctx.G
    with ExitStack() as st:
        kvw = _sb(st, nc, "kvw", [128, 8, 2 * D], BF16)
        qw = _sb(st, nc, "qw", [128, 8, D], BF16)
        sc, sh = mod_scale_bias(ctx, st, 1, 0, 1, "m1b")
        xt = [_sb(st, nc, f"kx{i}", [128, D], F32) for i in range(3)]
        x2T = [_sb(st, nc, f"kxT{i}", [128, 8, 512], BF16) for i in range(2)]
        hT = [_sb(st, nc, f"khT{i}", [128, 8, 512], BF16) for i in range(2)]
        ko = [_sb(st, nc, f"ko{i}", [128, 8, 512], BF16) for i in range(2)]
        qo = [_sb(st, nc, f"qo{i}", [128, 8, 512], BF16) for i in range(2)]
        vo = [_sb(st, nc, f"vo{i}", [128, D], BF16) for i in range(2)]
        psT = [_ps(st, nc, f"kpT{i}", [128, 512]) for i in range(2)]
        pk = [_ps(st, nc, f"kpk{i}", [128, 512]) for i in range(2)]
        pq = [_ps(st, nc, f"kpq{i}", [128, 512]) for i in range(2)]
        pv = [_ps(st, nc, f"kpv{i}", [128, 512]) for i in range(2)]
        s1 = I["kv_w"].rearrange("(k p) n -> p k n", p=128)
        s2 = I["q_w"].rearrange("(k p) n -> p k n", p=128)
        for k in range(8):
            S.dma("pool", lambda e, k=k: e.dma_start(out=kvw[:, k, :], in_=s1[:, k, :]), writes=[kvw.k])
            S.dma("pool", lambda e, k=k: e.dma_start(out=qw[:, k, :], in_=s2[:, k, :]), writes=[qw.k])
        ti = 0
        vi = 0
        for j in range(NTOK // 512):
            b = (j * 512) // SEQ
            xT_, h = x2T[j % 2], hT[j % 2]
            for i in range(4):
                x_t = xt[ti % 3]
                ti += 1
                r0 = j * 512 + i * 128
                S.dma("sp", lambda e: e.dma_start(out=x_t[:], in_=Dr["xb"][r0:r0 + 128, :]), writes=[x_t.k])

                def ev(k, p, ap, i=i):
                    S.op("act", lambda e: e.activation(xT_[:, k, i * 128:(i + 1) * 128], ap, AF.Copy), reads=[p.k], writes=[xT_.k])
                    S.op("act", lambda e: e.activation(h[:, k, i * 128:(i + 1) * 128], ap, AF.Identity,
                                                       bias=sh[:, b, k:k + 1], scale=sc[:, b, k:k + 1]),
                         reads=[p.k, sh.k, sc.k], writes=[h.k])
                transpose_tile(ctx, x_t, psT, ev)
            import os
            part = int(os.environ.get("KVQ_PART", "9"))
            if part < 2:
                continue
            k_o, q_o = ko[j % 2], qo[j % 2]
            for m in range(8):
                p1, p2 = pk[m % 2], pq[m % 2]
                for k in range(8):
                    S.op("pe", lambda e, k=k: e.matmul(p1[:], kvw[:, k, m * 128:(m + 1) * 128], xT_[:, k, :], start=(k == 0), stop=(k == 7)),
                         reads=[kvw.k, xT_.k], writes=[p1.k])
                S.op("act", lambda e: e.activation(k_o[:, m, :], p1[:], AF.Copy), reads=[p1.k], writes=[k_o.k])
                for k in range(8):
                    S.op("pe", lambda e, k=k: e.matmul(p2[:], qw[:, k, m * 128:(m + 1) * 128], h[:, k, :], start=(k == 0), stop=(k == 7)),
                         reads=[qw.k, h.k], writes=[p2.k])
                S.op("act", lambda e: e.activation(q_o[:, m, :], p2[:], AF.Copy, scale=0.125), reads=[p2.k], writes=[q_o.k])
            S.dma("sp", lambda e: e.dma_start(out=Dr["kT"][:, :, j * 512:(j + 1) * 512].rearrange("k p t -> p k t"), in_=k_o[:]), reads=[k_o.k])
            S.dma("sp", lambda e: e.dma_start(out=Dr["qT"][:, :, j * 512:(j + 1) * 512].rearrange("k p t -> p k t"), in_=q_o[:]), reads=[q_o.k])
            if part < 3:
                continue
            for i in range(4):
                r0 = j * 512 + i * 128
                v_o = vo[vi % 2]
                vi += 1
                for n in range(2):
                    p = pv[n]
                    for k in range(8):
                        S.op("pe", lambda e, k=k: e.matmul(p[:], xT_[:, k, i * 128:(i + 1) * 128], kvw[:, k, D + n * 512:D + (n + 1) * 512],
                                                          start=(k == 0), stop=(k == 7)), reads=[xT_.k, kvw.k], writes=[p.k])
                    S.op("act", lambda e: e.activation(v_o[:, n * 512:(n + 1) * 512], p[:], AF.Copy), reads=[p.k], writes=[v_o.k])
                S.dma("sp", lambda e: e.dma_start(out=Dr["v"][r0:r0 + 128, :], in_=v_o[:]), reads=[v_o.k])


def phase_attn(ctx):
    nc, S, I, Dr, G = ctx.nc, ctx.S, ctx.I, ctx.Dr, ctx.G
    with ExitStack() as st:
        kT = _sb(st, nc, "akT", [128, 8, SEQ], BF16)
        qT = _sb(st, nc, "aqT", [128, 8, SEQ], BF16)
        v = _sb(st, nc, "av", [128, 16, D], BF16)
        oT = _sb(st, nc, "aoT", [128, 8, SEQ], BF16)
        ntri = _sb(st, nc, "ntri", [128, 128], BF16)
        nones = _sb(st, nc, "nones", [128, 128], BF16)
        mtmp = _sb(st, nc, "mtmp", [128, 512], F32)
        masks = [_sb(st, nc, f"amask{r}", [128, 512], BF16) for r in range(4)]
        e_sb = [_sb(st, nc, f"ae{i}", [128, 512], F32) for i in range(2)]
        sp = [[_sb(st, nc, f"asp{s_}{i}", [128, 512], BF16) for i in range(2)] for s_ in range(2)]
        a_sb = [[_sb(st, nc, f"aa{s_}{i}", [128, 512], BF16) for i in range(2)] for s_ in range(2)]
        acc = [[_sb(st, nc, f"aacc{s_}{i}", [128, 512], BF16) for i in range(2)] for s_ in range(2)]
        pz1 = [_ps(st, nc, f"apz{s_}", [128, 512]) for s_ in range(2)]
        pz = [[pz1[s_], pz1[s_]] for s_ in range(2)]
        pdum = _ps(st, nc, "apdum", [128, 512])
        NWARM = 3

        def warm(n=NWARM):
            for _ in range(n):
                S.op("pe", lambda q: q.matmul(pdum[:], ntri[:], masks[0][:], start=True, stop=True),
                     reads=[ntri.k, masks[0].k], writes=[pdum.k])
        pw = [_ps(st, nc, f"apw{i}", [128, 512]) for i in range(2)]
        po = [_ps(st, nc, f"apo{i}", [128, 512]) for i in range(2)]
        S.op("dve", lambda e: e.tensor_scalar(ntri[:], G["iota"][:], 0.0, -1.0, ALU.is_le, ALU.mult), reads=[G["iota"].k], writes=[ntri.k])
        S.op("dve", lambda e: e.memset(nones[:], -1.0), writes=[nones.k])
        for r in range(4):
            S.op("pool", lambda e, r=r: e.iota(mtmp[:], pattern=[[1, 512]], base=-128 * r, channel_multiplier=-1,
                                              allow_small_or_imprecise_dtypes=True), writes=[mtmp.k])
            S.op("dve", lambda e, r=r: e.tensor_single_scalar(masks[r][:], mtmp[:], 0.0, ALU.is_gt), reads=[mtmp.k], writes=[masks[r].k])
        it = 0
        for b in range(2):
            t0 = b * SEQ
            S.dma("sp", lambda e: e.dma_start(out=kT[:], in_=Dr["kT"][:, :, t0:t0 + SEQ].rearrange("k p t -> p k t")), writes=[kT.k])
            S.dma("sp", lambda e: e.dma_start(out=qT[:], in_=Dr["qT"][:, :, t0:t0 + SEQ].rearrange("k p t -> p k t")), writes=[qT.k])
            S.dma("sp", lambda e: e.dma_start(out=v[:], in_=Dr["v"][t0:t0 + SEQ, :].rearrange("(s p) d -> p s d", p=128)), writes=[v.k])
            for ch in range(8):
                pbs = [0, 64]
                for c in range(4):
                    nkb = 4 * c + 4
                    kbs = list(range(nkb - 1, -1, -1))
                    q_ap = [qT[pb:pb + 64, ch, c * 512:(c + 1) * 512] for pb in pbs]

                    def k_ap(kb, s_):
                        return kT[pbs[s_]:pbs[s_] + 64, ch, kb * 128:(kb + 1) * 128]

                    def stP(idx):
                        kb = kbs[idx]
                        r = kb - 4 * c
                        i2 = idx % 2
                        for s_ in (0, 1):
                            z = pz[s_][i2]
                            S.op("pe", lambda q: q.matmul(z[:], k_ap(kb, s_), q_ap[s_], start=True, stop=True), reads=[kT.k, qT.k], writes=[z.k])
                        warm()
                        for s_ in (0, 1):
                            z = pz[s_][i2]
                            S.op("act", lambda q: q.activation(e_sb[s_][:], z[:], AF.Exp), reads=[z.k], writes=[e_sb[s_].k])
                        for s_ in (0, 1):
                            S.op("act", lambda q: q.activation(sp[s_][i2][:], e_sb[s_][:], AF.Ln, bias=G["one"][:], scale=1.0),
                                 reads=[e_sb[s_].k, G["one"].k], writes=[sp[s_][i2].k])
                        if r >= 0:
                            for s_ in (0, 1):
                                S.op("dve", lambda q: q.tensor_tensor(sp[s_][i2][:], sp[s_][i2][:], masks[r][:], ALU.mult),
                                     reads=[sp[s_][i2].k, masks[r].k], writes=[sp[s_][i2].k])

                    def stQ(idx):
                        kb = kbs[idx]
                        r = kb - 4 * c
                        i2 = idx % 2
                        for s_ in (0, 1):
                            w = pw[s_]
                            S.op("pe", lambda q: q.matmul(w[:], k_ap(kb, s_), q_ap[s_], start=True, stop=False), reads=[kT.k, qT.k], writes=[w.k])
                            S.op("pe", lambda q: q.matmul(w[:], ntri[:], sp[s_][i2][:], start=False, stop=(idx == 0)),
                                 reads=[ntri.k, sp[s_][i2].k], writes=[w.k])
                            if idx > 0:
                                ac = acc[s_][(idx - 1) % 2]
                                S.op("pe", lambda q: q.matmul(w[:], nones[:], ac[:], start=False, stop=True), reads=[nones.k, ac.k], writes=[w.k])
                        warm()
                        for s_ in (0, 1):
                            S.op("act", lambda q: q.activation(a_sb[s_][i2][:], pw[s_][:], AF.Exp), reads=[pw[s_].k], writes=[a_sb[s_][i2].k])
                        if r >= 0:
                            for s_ in (0, 1):
                                S.op("dve", lambda q: q.tensor_tensor(a_sb[s_][i2][:], a_sb[s_][i2][:], masks[r][:], ALU.mult),
                                     reads=[a_sb[s_][i2].k, masks[r].k], writes=[a_sb[s_][i2].k])
                        for s_ in (0, 1):
                            h = 2 * ch + s_
                            S.op("pe", lambda q: q.matmul(po[s_][pbs[s_]:pbs[s_] + 64, :], v[:, kb, h * 64:(h + 1) * 64], a_sb[s_][i2][:],
                                                          start=(idx == 0), stop=(idx == nkb - 1)),
                                 reads=[v.k, a_sb[s_][i2].k], writes=[po[s_].k])
                        if idx < nkb - 1:
                            for s_ in (0, 1):
                                an = acc[s_][idx % 2]
                                if idx == 0:
                                    S.op("pool", lambda q: q.tensor_copy(an[:], sp[s_][i2][:]), reads=[sp[s_][i2].k], writes=[an.k])
                                else:
                                    S.op("pool", lambda q: q.tensor_tensor(an[:], acc[s_][(idx - 1) % 2][:], sp[s_][i2][:], ALU.add),
                                         reads=[acc[s_][(idx - 1) % 2].k, sp[s_][i2].k], writes=[an.k])

                    stP(0)
                    for idx in range(nkb):
                        if idx + 1 < nkb:
                            stP(idx + 1)
                        stQ(idx)
                    for s_ in (0, 1):
                        pb = 64 * s_
                        S.op("act", lambda q: q.activation(oT[pb:pb + 64, ch, c * 512:(c + 1) * 512], po[s_][pb:pb + 64, :], AF.Copy),
                             reads=[po[s_].k], writes=[oT.k])
            S.dma("sp", lambda e: e.dma_start(out=Dr["oT"][:, :, t0:t0 + SEQ].rearrange("k p t -> p k t"), in_=oT[:]), reads=[oT.k])


def phase_oproj(ctx):
    nc, S, I, Dr, G = ctx.nc, ctx.S, ctx.I, ctx.Dr, ctx.G
    with ExitStack() as st:
        ow = _sb(st, nc, "ow", [128, 8, D], BF16)
        epi = Epi(ctx, st, 1, 0, 2)
        oTb = [_sb(st, nc, f"ooT{i}", [128, 8, 512], BF16) for i in range(2)]
        xt = [_sb(st, nc, f"ox{i}", [128, D], F32) for i in range(2)]
        py = [_ps(st, nc, f"opy{i}", [128, 512]) for i in range(4)]
        s1 = I["o_w"].rearrange("(k p) n -> p k n", p=128)
        for k in range(8):
            S.dma("pool", lambda e, k=k: e.dma_start(out=ow[:, k, :], in_=s1[:, k, :]), writes=[ow.k])
        ti = 0
        for j in range(NTOK // 512):
            b = (j * 512) // SEQ
            o_b = oTb[j % 2]
            S.dma("sp", lambda e: e.dma_start(out=o_b[:], in_=Dr["oT"][:, :, j * 512:(j + 1) * 512].rearrange("k p t -> p k t")), writes=[o_b.k])
            for i in range(4):
                r0 = j * 512 + i * 128
                x_t = xt[ti % 2]
                S.dma("sp", lambda e: e.dma_start(out=x_t[:], in_=Dr["xb"][r0:r0 + 128, :]), writes=[x_t.k])
                ys = []
                for n in range(2):
                    p = py[(ti % 2) * 2 + n]
                    for c in range(8):
                        S.op("pe", lambda e, c=c: e.matmul(p[:], o_b[:, c, i * 128:(i + 1) * 128], ow[:, c, n * 512:(n + 1) * 512],
                                                          start=(c == 0), stop=(c == 7)), reads=[o_b.k, ow.k], writes=[p.k])
                    ys.append((p, p[:]))
                ti += 1
                epi.run(ys, x_t, b, Dr["xa"][r0:r0 + 128, :])


def cast_load(S, dst_ap_fn, src_ap_fn, ncols, wkey, q="pool", step=2048):
    for c0 in range(0, ncols, step):
        c1 = min(ncols, c0 + step)
        S.dma(q, lambda e, c0=c0, c1=c1: e.dma_start(out=dst_ap_fn(c0, c1), in_=src_ap_fn(c0, c1)), writes=[wkey])


def make_in_maps(inp, cores):
    f = lambda a: np.ascontiguousarray(a, dtype=np.float32)
    x, c = inp["x"], inp["c"]
    shared = {
        "ada_w": f(inp["ada_w"]), "ada_b": f(inp["ada_b"]),
        "ln_g": f(inp["ln_g"].reshape(4, D)), "ln_b": f(inp["ln_b"].reshape(4, D)),
        "cv_w1": f(inp["cv_w1"][0]),
        "cv_b1T": f(inp["cv_b1"][0].reshape(16, 128).T),
        "cv_dwT": f(inp["cv_dw"][0].reshape(31, 8, 128).transpose(2, 1, 0)),
        "cv_vecT": f(np.stack([inp["cv_db"][0].reshape(8, 128).T, inp["cv_ln_g"][0].reshape(8, 128).T,
                               inp["cv_ln_b"][0].reshape(8, 128).T], axis=1)),
        "cv_w2": f(inp["cv_w2"][0]), "cv_b2": f(inp["cv_b2"][0].reshape(1, D)),
        "kv_w": f(inp["kv_w"]), "q_w": f(inp["q_w"][0]), "o_w": f(inp["o_w"][0]),
        "router_w": f(inp["router_w"]), "router_b": f(inp["router_b"]),
        "moe_w_in": f(inp["moe_w_in"]), "moe_b_in": f(inp["moe_b_in"]),
        "moe_w_out": f(inp["moe_w_out"]), "moe_b_out": f(inp["moe_b_out"]),
    }
    maps = []
    for ci in cores:
        m = dict(shared)
        m["x"] = f(x[2 * ci:2 * ci + 2].reshape(NTOK, D))
        m["cT"] = f(c[2 * ci:2 * ci + 2].reshape(2, 8, 128).transpose(2, 1, 0))
        maps.append(m)
    return maps


def kernel(**inputs):
    inp = {k: np.asarray(v) for k, v in inputs.items()}
    nc = build_program()
    maps = make_in_maps(inp, list(range(8)))
    res = run_bass_kernel_spmd(nc, maps, core_ids=list(range(8)))
    outs = [r["out"].reshape(2, SEQ, D) for r in res.results]
    return np.concatenate(outs, axis=0).astype(np.float32)
```

```python
import numpy as np
from contextlib import ExitStack
import concourse.bass as bass
import concourse.mybir as mybir
from concourse.bass_utils import run_bass_kernel_spmd

F32 = mybir.dt.float32
BF16 = mybir.dt.bfloat16
I32 = mybir.dt.int32
U32 = mybir.dt.uint32
AF = mybir.ActivationFunctionType
ALU = mybir.AluOpType
AX = mybir.AxisListType


class Tk:
    __slots__ = ("name", "w", "r")

    def __init__(self, name=""):
        self.name = name
        self.w = None
        self.r = {}


class Sched:
    SEM_ROLL = 30000
    NDMA = 8

    def __init__(self, nc, stack):
        self.nc = nc
        self.stack = stack
        self.engs = {"pe": nc.tensor, "dve": nc.vector, "act": nc.scalar,
                     "pool": nc.gpsimd, "sp": nc.sync}
        self.sems = []
        self.owner = []
        self.esem = {}
        self.ecnt = {}
        self.seen = {e: {} for e in self.engs}
        for e in self.engs:
            self._new_esem(e)
        self.dq = {}
        for q in ("sp", "pool", "act"):
            ids = [self._alloc_sem(f"dma_{q}_{i}", None) for i in range(self.NDMA)]
            self.dq[q] = {"ids": ids, "uses": [0] * self.NDMA, "next": 0}
        self.n_inst = 0
        self.n_wait = 0

    def _alloc_sem(self, name, owner):
        h = self.stack.enter_context(self.nc.semaphore(name))
        self.sems.append(h)
        self.owner.append(owner)
        return len(self.sems) - 1

    def _new_esem(self, e):
        sid = self._alloc_sem(f"e_{e}_{len(self.sems)}", e)
        self.esem[e] = sid
        self.ecnt[e] = 0

    def _wait(self, eng, deps):
        e = self.engs[eng]
        seen = self.seen[eng]
        for sid, val in deps.items():
            if seen.get(sid, 0) >= val:
                continue
            e.wait_ge(self.sems[sid], val)
            self.n_wait += 1
            seen[sid] = val

    def _deps(self, eng, reads, writes):
        deps = {}

        def add(tok, raw):
            sid, val = tok
            if self.owner[sid] == eng and not raw:
                return
            if deps.get(sid, 0) < val:
                deps[sid] = val
        for t in reads:
            if t.w is not None:
                add(t.w, True)
        for t in writes:
            if t.w is not None:
                add(t.w, False)
            for sid, val in t.r.items():
                add((sid, val), False)
        return deps

    def _commit(self, tok, reads, writes):
        sid, val = tok
        for t in reads:
            if t.r.get(sid, 0) < val:
                t.r[sid] = val
        for t in writes:
            t.w = tok
            t.r = {}

    def op(self, eng, fn, reads=(), writes=()):
        if self.ecnt[eng] >= self.SEM_ROLL:
            self._new_esem(eng)
        self._wait(eng, self._deps(eng, reads, writes))
        ins = fn(self.engs[eng])
        self.ecnt[eng] += 1
        ins.then_inc(self.sems[self.esem[eng]], 1)
        tok = (self.esem[eng], self.ecnt[eng])
        self._commit(tok, reads, writes)
        self.n_inst += 1
        return tok

    def dma(self, q, fn, reads=(), writes=()):
        d = self.dq[q]
        i = d["next"]
        d["next"] = (i + 1) % self.NDMA
        sid = d["ids"][i]
        deps = self._deps(q, reads, writes)
        if d["uses"][i] > 0:
            v = 16 * d["uses"][i]
            if deps.get(sid, 0) < v:
                deps[sid] = v
        self._wait(q, deps)
        ins = fn(self.engs[q])
        d["uses"][i] += 1
        ins.then_inc(self.sems[sid], 16)
        tok = (sid, 16 * d["uses"][i])
        self._commit(tok, reads, writes)
        self.n_inst += 1
        return tok

    def barrier(self):
        deps = {}
        for e in self.engs:
            if self.ecnt[e] > 0:
                deps[self.esem[e]] = self.ecnt[e]
        for q, d in self.dq.items():
            for i, sid in enumerate(d["ids"]):
                if d["uses"][i] > 0:
                    deps[sid] = 16 * d["uses"][i]
        for e in self.engs:
            dd = {s: v for s, v in deps.items() if self.owner[s] != e}
            self._wait(e, dd)

    def final_wait(self, toks, eng="sp"):
        deps = {}
        for sid, val in toks:
            if deps.get(sid, 0) < val:
                deps[sid] = val
        self._wait(eng, deps)


D = 1024
NTOK = 4096
SEQ = 2048
NT = NTOK // 128
NE = 32
CAP = 1024
NBLK = CAP // 128
ALPHA_DN = 4.0 ** 0.25
LN_EPS = 1e-5
SW_LIM = 7.0
SW_ALPHA = 1.702
BIG = 1.0e6


class B:
    def __init__(self, t):
        self.t = t
        self.k = Tk()

    def __getitem__(self, i):
        return self.t[i]


class K:
    def __init__(self, nc, S, dbg):
        self.nc = nc
        self.S = S
        self.dbg = dbg


_UID = [0]


def _sb(st, nc, name, shape, dt):
    _UID[0] += 1
    return B(st.enter_context(nc.sbuf_tensor(f"s{_UID[0]}_{name}", list(shape), dt)))


def _ps(st, nc, name, shape, dt=F32):
    _UID[0] += 1
    return B(st.enter_context(nc.psum_tensor(f"p{_UID[0]}_{name}", list(shape), dt)))


def build_program(upto=99, dbg=False, start=0, small_moe=False):
    nc = bass.Bass("TRN2", target_bir_lowering=False)
    dt_in = lambda n, s, d=F32: nc.dram_tensor(n, list(s), d, kind="ExternalInput").ap()
    I = {}
    I["x"] = dt_in("x", [NTOK, D])
    I["cT"] = dt_in("cT", [128, 8, 2])
    I["ada_w"] = dt_in("ada_w", [2, D, 6 * D] if start == 0 else [2, 8, 8])
    I["ada_b"] = dt_in("ada_b", [2, 6 * D])
    I["ln_g"] = dt_in("ln_g", [4, D])
    I["ln_b"] = dt_in("ln_b", [4, D])
    I["cv_w1"] = dt_in("cv_w1", [D, 2 * D])
    I["cv_b1T"] = dt_in("cv_b1T", [128, 16])
    I["cv_dwT"] = dt_in("cv_dwT", [128, 8, 31])
    I["cv_vecT"] = dt_in("cv_vecT", [128, 3, 8])
    I["cv_w2"] = dt_in("cv_w2", [D, D])
    I["cv_b2"] = dt_in("cv_b2", [1, D])
    I["kv_w"] = dt_in("kv_w", [D, 2 * D])
    I["q_w"] = dt_in("q_w", [D, D])
    I["o_w"] = dt_in("o_w", [D, D])
    I["router_w"] = dt_in("router_w", [2, D, NE])
    I["router_b"] = dt_in("router_b", [2, NE])
    I["moe_w_in"] = dt_in("moe_w_in", [2, NE, D, 2 * D] if not small_moe else [2, NE, 8, 16])
    I["moe_b_in"] = dt_in("moe_b_in", [2, NE, 2 * D])
    I["moe_w_out"] = dt_in("moe_w_out", [2, NE, D, D] if not small_moe else [2, NE, 8, 8])
    I["moe_b_out"] = dt_in("moe_b_out", [2, NE, D])
    out = nc.dram_tensor("out", [NTOK, D], F32, kind="ExternalOutput").ap()
    scr = lambda n, s, d: nc.dram_tensor(n, list(s), d, kind="Internal").ap()
    Dr = {}
    Dr["modrow"] = scr("modrow", [2, 2, 6 * D], F32)
    Dr["gluT"] = scr("gluT", [8, 128, NTOK], BF16)
    Dr["xa"] = scr("xa", [NTOK, D], F32)
    Dr["xb"] = scr("xb", [NTOK, D], F32)
    Dr["Xe"] = scr("Xe", [NE * CAP, D], BF16)
    Dr["Y"] = scr("Y", [NE * CAP, D], BF16)
    Dr["kT"] = scr("kT", [8, 128, NTOK], BF16)
    Dr["qT"] = scr("qT", [8, 128, NTOK], BF16)
    Dr["v"] = scr("v", [NTOK, D], BF16)
    Dr["oT"] = scr("oT", [8, 128, NTOK], BF16)
    dbg_out = {}
    if dbg:
        dbg_out["d_mod"] = nc.dram_tensor("d_mod", [2, 2, 6 * D], F32, kind="ExternalOutput").ap()
        dbg_out["d_xa"] = nc.dram_tensor("d_xa", [NTOK, D], F32, kind="ExternalOutput").ap()
        dbg_out["d_lg"] = nc.dram_tensor("d_lg", [NTOK, NE], F32, kind="ExternalOutput").ap()
        dbg_out["d_sl"] = nc.dram_tensor("d_sl", [NTOK, 4], I32, kind="ExternalOutput").ap()
        dbg_out["d_gt"] = nc.dram_tensor("d_gt", [NTOK, 4], F32, kind="ExternalOutput").ap()
        if start == 6:
            dbg_out["d_kT"] = nc.dram_tensor("d_kT", [8, 128, NTOK], BF16, kind="ExternalOutput").ap()
            dbg_out["d_v"] = nc.dram_tensor("d_v", [NTOK, D], BF16, kind="ExternalOutput").ap()

    with ExitStack() as gst:
        S = Sched(nc, gst)
        G = {}
        G["ident_f"] = _sb(gst, nc, "ident_f", [128, 128], F32)
        G["ident_b"] = _sb(gst, nc, "ident_b", [128, 128], BF16)
        G["ones_f"] = _sb(gst, nc, "ones_f", [128, 128], F32)
        G["ones_b"] = _sb(gst, nc, "ones_b", [128, 128], BF16)
        G["triU"] = _sb(gst, nc, "triU", [128, 128], F32)
        G["eps"] = _sb(gst, nc, "eps", [128, 1], F32)
        G["one"] = _sb(gst, nc, "one", [128, 1], F32)
        G["slots"] = _sb(gst, nc, "slots", [128, NT, 4], I32)
        G["gates"] = _sb(gst, nc, "gates", [128, NT, 4], F32)
        G["gmat"] = _sb(gst, nc, "gmat", [128, NT, NE], F32)
        G["modT"] = _sb(gst, nc, "modT", [128, 4, 6, 8], F32)
        G["ecap"] = _sb(gst, nc, "ecap", [128, NE], F32)
        tmp = _sb(gst, nc, "tmp_iota", [128, 128], F32)
        G["iota"] = tmp
        S.op("pool", lambda e: e.iota(tmp[:], pattern=[[1, 128]], base=0, channel_multiplier=-1,
                                      allow_small_or_imprecise_dtypes=True), writes=[tmp.k])
        S.op("dve", lambda e: e.tensor_single_scalar(G["ident_f"][:], tmp[:], 0.0, ALU.is_equal),
             reads=[tmp.k], writes=[G["ident_f"].k])
        S.op("dve", lambda e: e.tensor_single_scalar(G["ident_b"][:], tmp[:], 0.0, ALU.is_equal),
             reads=[tmp.k], writes=[G["ident_b"].k])
        S.op("dve", lambda e: e.tensor_single_scalar(G["triU"][:], tmp[:], 0.0, ALU.is_gt),
             reads=[tmp.k], writes=[G["triU"].k])
        S.op("dve", lambda e: e.memset(G["ones_f"][:], 1.0), writes=[G["ones_f"].k])
        S.op("dve", lambda e: e.memset(G["ones_b"][:], 1.0), writes=[G["ones_b"].k])
        S.op("dve", lambda e: e.memset(G["eps"][:], LN_EPS), writes=[G["eps"].k])
        S.op("dve", lambda e: e.memset(G["one"][:], 1.0), writes=[G["one"].k])
        S.op("pool", lambda e: e.iota(G["ecap"][:], pattern=[[CAP, NE]], base=0, channel_multiplier=0,
                                      allow_small_or_imprecise_dtypes=True), writes=[G["ecap"].k])
        ctx = K(nc, S, dbg)
        ctx.I, ctx.Dr, ctx.G, ctx.out, ctx.dbg_out = I, Dr, G, out, dbg_out
        ctx.upto = upto
        ctx.bc_reg = nc.gpsimd.to_reg(NE * CAP - 1)
        for nm in ('slots', 'gates', 'gmat'):
            G[nm].kt = [Tk() for _ in range(NT)]

        phases = [
            ("mod", lambda: phase_mod(ctx)),
            ("glu", lambda: phase_glu(ctx)),
            ("conv", lambda: phase_conv(ctx)),
            ("front0", lambda: phase_front(ctx, 0)),
            ("experts0", lambda: phase_experts(ctx, 0)),
            ("combine0", lambda: phase_combine(ctx, 0)),
            ("kvq", lambda: phase_kvq(ctx)),
            ("attn", lambda: phase_attn(ctx)),
            ("oproj", lambda: phase_oproj(ctx)),
            ("front1", lambda: phase_front(ctx, 1)),
            ("experts1", lambda: phase_experts(ctx, 1)),
            ("combine1", lambda: phase_combine(ctx, 1)),
        ]
        if start > 0:
            din = nc.dram_tensor("d_in_x", [NTOK, D], F32, kind="ExternalInput").ap()
            dstn = "xb" if start in (6, 7, 8) else "xa"
            S.dma("sp", lambda e: e.dma_start(out=Dr[dstn], in_=din))
            dmod = nc.dram_tensor("d_in_mod", [2, 2, 6 * D], F32, kind="ExternalInput").ap()
            S.dma("sp", lambda e: e.dma_start(out=Dr["modrow"], in_=dmod))
            S.barrier()
            load_modT(ctx)
            S.barrier()
        for i, (name, fn) in enumerate(phases):
            if i > upto:
                break
            if i < start:
                continue
            fn()
            S.barrier()
        if dbg:
            dump_dbg(ctx)
            S.barrier()
    return nc


def dump_dbg(ctx):
    nc, S = ctx.nc, ctx.S
    if "d_kT" in ctx.dbg_out:
        S.dma("sp", lambda e: e.dma_start(out=ctx.dbg_out["d_kT"], in_=ctx.Dr["kT"]))
        S.dma("sp", lambda e: e.dma_start(out=ctx.dbg_out["d_v"], in_=ctx.Dr["v"]))
    S.dma("sp", lambda e: e.dma_start(out=ctx.dbg_out["d_mod"], in_=ctx.Dr["modrow"]))
    S.dma("sp", lambda e: e.dma_start(out=ctx.dbg_out["d_xa"], in_=ctx.Dr["xa"]))
    S.dma("sp", lambda e: e.dma_start(out=ctx.dbg_out["d_sl"].rearrange("(t p) k -> p t k", p=128),
                                      in_=ctx.G["slots"][:]), reads=ctx.G["slots"].kt)
    S.dma("sp", lambda e: e.dma_start(out=ctx.dbg_out["d_gt"].rearrange("(t p) k -> p t k", p=128),
                                      in_=ctx.G["gates"][:]), reads=ctx.G["gates"].kt)


def phase_mod(ctx):
    nc, S, I, Dr, G = ctx.nc, ctx.S, ctx.I, ctx.Dr, ctx.G
    with ExitStack() as st:
        cT = _sb(st, nc, "cT", [128, 8, 2], F32)
        caT = _sb(st, nc, "caT", [128, 8, 2], F32)
        wb = [_sb(st, nc, f"adaw{i}", [128, 8, 1536], F32) for i in range(2)]
        bb = _sb(st, nc, "adab", [1, 2, 6 * D], F32)
        msb = [_sb(st, nc, f"modsb{i}", [2, 1536], F32) for i in range(2)]
        pp = [_ps(st, nc, f"pmod{i}", [128, 512]) for i in range(2)]
        S.dma("sp", lambda e: e.dma_start(out=cT[:], in_=I["cT"]), writes=[cT.k])
        S.dma("sp", lambda e: e.dma_start(out=bb[:], in_=I["ada_b"].rearrange("(o l) n -> o l n", o=1)), writes=[bb.k])
        S.op("act", lambda e: e.activation(caT[:], cT[:], AF.Silu), reads=[cT.k], writes=[caT.k])
        it = 0
        for l in range(2):
            for q4 in range(4):
                w = wb[it % 2]
                m = msb[it % 2]
                src = I["ada_w"][l, :, q4 * 1536:(q4 + 1) * 1536].rearrange("(k p) n -> p k n", p=128)
                S.dma("sp", lambda e: e.dma_start(out=w[:], in_=src), writes=[w.k])
                for n in range(3):
                    p = pp[n % 2]
                    col = q4 * 1536 + n * 512
                    for k in range(8):
                        S.op("pe", lambda e, k=k: e.matmul(p[0:2, :], caT[:, k, :], w[:, k, n * 512:(n + 1) * 512],
                                                          start=(k == 0), stop=False),
                             reads=[caT.k, w.k], writes=[p.k])
                    S.op("pe", lambda e: e.matmul(p[0:2, :], G["ones_f"][0:1, 0:2], bb[0:1, l, col:col + 512],
                                                  start=False, stop=True),
                         reads=[G["ones_f"].k, bb.k], writes=[p.k])
                    S.op("act", lambda e: e.activation(m[0:2, n * 512:(n + 1) * 512], p[0:2, :], AF.Copy),
                         reads=[p.k], writes=[m.k])
                S.dma("sp", lambda e: e.dma_start(out=Dr["modrow"][l, :, q4 * 1536:(q4 + 1) * 1536], in_=m[0:2, :]), reads=[m.k])
                it += 1
    S.barrier()
    load_modT(ctx)


def load_modT(ctx):
    nc, S, I, Dr, G = ctx.nc, ctx.S, ctx.I, ctx.Dr, ctx.G
    for l in range(2):
        for b in range(2):
            for wch in range(6):
                S.dma("sp", lambda e, l=l, b=b, wch=wch: e.dma_start(
                    out=G["modT"][:, l * 2 + b, wch, :],
                    in_=Dr["modrow"][l, b, wch * D:(wch + 1) * D].rearrange("(k p) -> p k", p=128),
                    allow_slow_non_contiguous=True), writes=[G["modT"].k])


def load_bc(ctx, st, name, src_row):
    t = _sb(st, ctx.nc, name, [128, D], F32)
    ctx.S.dma("sp", lambda e: e.dma_start(out=t[:], in_=src_row.partition_broadcast(128)), writes=[t.k])
    return t


def add_one(ctx, t):
    ctx.S.op("pool", lambda e: e.tensor_scalar_add(t[:], t[:], 1.0), reads=[t.k], writes=[t.k])


def mod_scale_bias(ctx, st, l, w_sh, w_sc, name):
    nc, S, G = ctx.nc, ctx.S, ctx.G
    sc = _sb(st, nc, name + "_sc", [128, 2, 8], F32)
    sh = _sb(st, nc, name + "_sh", [128, 2, 8], F32)
    for b in range(2):
        S.op("dve", lambda e, b=b: e.tensor_scalar_add(sc[:, b, :], G["modT"][:, l * 2 + b, w_sc, :], 1.0),
             reads=[G["modT"].k], writes=[sc.k])
        S.op("dve", lambda e, b=b: e.tensor_copy(sh[:, b, :], G["modT"][:, l * 2 + b, w_sh, :]),
             reads=[G["modT"].k], writes=[sh.k])
    return sc, sh


def run_interleaved(gens, width):
    active = []
    it = iter(gens)
    more = True
    while True:
        while more and len(active) < width:
            try:
                active.append(next(it))
            except StopIteration:
                more = False
        if not active:
            break
        for g in list(active):
            try:
                next(g)
            except StopIteration:
                active.remove(g)


class Epi:
    def __init__(self, ctx, st, l, sub, gate_which):
        nc, S, I, Dr = ctx.nc, ctx.S, ctx.I, ctx.Dr
        self.ctx = ctx
        self.lng = load_bc(ctx, st, f"lng{l}{sub}", I["ln_g"][l * 2 + sub, :])
        self.lnb = load_bc(ctx, st, f"lnb{l}{sub}", I["ln_b"][l * 2 + sub, :])
        self.gate = []
        for b in range(2):
            g = load_bc(ctx, st, f"gate{l}{sub}{b}", Dr["modrow"][l, b, gate_which * D:(gate_which + 1) * D])
            add_one(ctx, g)
            self.gate.append(g)
        self.t1 = [_sb(st, nc, f"ep_t1_{i}", [128, D], F32) for i in range(2)]
        self.r = [_sb(st, nc, f"ep_r_{i}", [128, D], F32) for i in range(2)]
        self.xo = [_sb(st, nc, f"ep_xo_{i}", [128, D], F32) for i in range(2)]
        self.st6 = [_sb(st, nc, f"ep_st_{i}", [128, 2, 6], F32) for i in range(2)]
        self.mv = [_sb(st, nc, f"ep_mv_{i}", [128, 4], F32) for i in range(2)]
        self.n = 0

    def run_g(self, ys, x_t, b, dst):
        S = self.ctx.S
        i = self.n % 2
        self.n += 1
        t1, r, xo, st6, mv = self.t1[i], self.r[i], self.xo[i], self.st6[i], self.mv[i]
        g = self.gate[b]
        for h, (yb, yap) in enumerate(ys):
            S.op("dve", lambda e, h=h, yap=yap: e.tensor_tensor(t1[:, h * 512:(h + 1) * 512], yap,
                                                              g[:, h * 512:(h + 1) * 512], ALU.mult),
                 reads=[yb.k, g.k], writes=[t1.k])
        yield
        S.op("dve", lambda e: e.scalar_tensor_tensor(r[:], x_t[:], ALPHA_DN, t1[:], ALU.mult, ALU.add),
             reads=[x_t.k, t1.k], writes=[r.k])
        yield
        for h in range(2):
            S.op("dve", lambda e, h=h: e.bn_stats(st6[:, h, :], r[:, h * 512:(h + 1) * 512]),
                 reads=[r.k], writes=[st6.k])
        S.op("dve", lambda e: e.bn_aggr(mv[:, 0:2], st6[:].rearrange("p a b -> p (a b)")), reads=[st6.k], writes=[mv.k])
        yield
        S.op("act", lambda e: e.activation(mv[:, 2:3], mv[:, 1:2], AF.Sqrt, bias=self.ctx.G["eps"][:], scale=1.0),
             reads=[mv.k, self.ctx.G["eps"].k], writes=[mv.k])
        yield
        S.op("dve", lambda e: e.reciprocal(mv[:, 2:3], mv[:, 2:3]), reads=[mv.k], writes=[mv.k])
        S.op("dve", lambda e: e.tensor_scalar(mv[:, 3:4], mv[:, 0:1], -1.0, mv[:, 2:3], ALU.mult, ALU.mult),
             reads=[mv.k], writes=[mv.k])
        S.op("act", lambda e: e.activation(t1[:], r[:], AF.Identity, bias=mv[:, 3:4], scale=mv[:, 2:3]),
             reads=[r.k, mv.k], writes=[t1.k])
        yield
        S.op("pool", lambda e: e.tensor_tensor(xo[:], t1[:], self.lng[:], ALU.mult),
             reads=[t1.k, self.lng.k], writes=[xo.k])
        S.op("pool", lambda e: e.tensor_tensor(xo[:], xo[:], self.lnb[:], ALU.add),
             reads=[xo.k, self.lnb.k], writes=[xo.k])
        S.dma("sp", lambda e: e.dma_start(out=dst, in_=xo[:]), reads=[xo.k])
        yield

    def run(self, ys, x_t, b, dst):
        for _ in self.run_g(ys, x_t, b, dst):
            pass


def transpose_tile(ctx, src, psT, evac):
    S, G = ctx.S, ctx.G
    for k in range(8):
        p = psT[k // 4]
        S.op("pe", lambda e, k=k, p=p: e.transpose(p[:, (k % 4) * 128:(k % 4 + 1) * 128], src[:, k * 128:(k + 1) * 128],
                                                 G["ident_f"][:]),
             reads=[src.k, G["ident_f"].k], writes=[p.k])
    for k in range(8):
        p = psT[k // 4]
        evac(k, p, p[:, (k % 4) * 128:(k % 4 + 1) * 128])


def phase_glu(ctx):
    nc, S, I, Dr, G = ctx.nc, ctx.S, ctx.I, ctx.Dr, ctx.G
    with ExitStack() as st:
        w1 = _sb(st, nc, "w1", [128, 8, 2 * D], BF16)
        b1T = _sb(st, nc, "b1T", [128, 16], F32)
        sc, sh = mod_scale_bias(ctx, st, 0, 0, 1, "m1")
        xt = [_sb(st, nc, f"xt{i}", [128, D], F32) for i in range(3)]
        hT = [_sb(st, nc, f"hT{i}", [128, 8, 512], BF16) for i in range(2)]
        sig = [_sb(st, nc, f"sig{i}", [128, 512], F32) for i in range(2)]
        gl = [_sb(st, nc, f"gl{i}", [128, 8, 512], BF16) for i in range(2)]
        psT = [_ps(st, nc, f"psT{i}", [128, 512]) for i in range(2)]
        pa = [_ps(st, nc, f"pa{i}", [128, 512]) for i in range(2)]
        pg = [_ps(st, nc, f"pg{i}", [128, 512]) for i in range(2)]
        src = I["cv_w1"].rearrange("(k p) n -> p k n", p=128)
        for k in range(8):
            S.dma("pool", lambda e, k=k: e.dma_start(out=w1[:, k, :], in_=src[:, k, :]), writes=[w1.k])
        S.dma("sp", lambda e: e.dma_start(out=b1T[:], in_=I["cv_b1T"]), writes=[b1T.k])
        ti = 0
        for j in range(NTOK // 512):
            b = (j * 512) // SEQ
            h = hT[j % 2]
            for i in range(4):
                x_t = xt[ti % 3]
                ti += 1
                r0 = j * 512 + i * 128
                S.dma("sp", lambda e, x_t=x_t, r0=r0: e.dma_start(out=x_t[:], in_=I["x"][r0:r0 + 128, :]), writes=[x_t.k])
                transpose_tile(ctx, x_t, psT, lambda k, p, ap, i=i: S.op(
                    "act", lambda e: e.activation(h[:, k, i * 128:(i + 1) * 128], ap, AF.Identity,
                                                  bias=sh[:, b, k:k + 1], scale=sc[:, b, k:k + 1]),
                    reads=[p.k, sh.k, sc.k], writes=[h.k]))
            g_o = gl[j % 2]
            for m in range(8):
                a_p, g_p, sg = pa[m % 2], pg[m % 2], sig[m % 2]
                for k in range(8):
                    S.op("pe", lambda e, k=k: e.matmul(a_p[:], w1[:, k, m * 128:(m + 1) * 128], h[:, k, :],
                                                      start=(k == 0), stop=(k == 7)), reads=[w1.k, h.k], writes=[a_p.k])
                for k in range(8):
                    S.op("pe", lambda e, k=k: e.matmul(g_p[:], w1[:, k, D + m * 128:D + (m + 1) * 128], h[:, k, :],
                                                      start=(k == 0), stop=(k == 7)), reads=[w1.k, h.k], writes=[g_p.k])
                S.op("act", lambda e: e.activation(sg[:], g_p[:], AF.Sigmoid, bias=b1T[:, 8 + m:9 + m], scale=1.0),
                     reads=[g_p.k, b1T.k], writes=[sg.k])
                S.op("dve", lambda e: e.scalar_tensor_tensor(g_o[:, m, :], a_p[:], b1T[:, m:m + 1], sg[:], ALU.add, ALU.mult),
                     reads=[a_p.k, b1T.k, sg.k], writes=[g_o.k])
            S.dma("sp", lambda e, j=j: e.dma_start(out=Dr["gluT"][:, :, j * 512:(j + 1) * 512].rearrange("k p t -> p k t"),
                                                  in_=g_o[:]), reads=[g_o.k])


def phase_conv(ctx):
    nc, S, I, Dr, G = ctx.nc, ctx.S, ctx.I, ctx.Dr, ctx.G
    with ExitStack() as st:
        dwT = _sb(st, nc, "dwT", [128, 8, 31], F32)
        vecT = _sb(st, nc, "vecT", [128, 3, 8], F32)
        dg = _sb(st, nc, "dg", [128, 8 * 31, 128], BF16)
        w2 = _sb(st, nc, "w2", [128, 8, D], BF16)
        b2 = _sb(st, nc, "b2", [1, D], BF16)
        epi = Epi(ctx, st, 0, 0, 2)
        glb = [_sb(st, nc, f"glb{i}", [128, 8, 544], BF16) for i in range(2)]
        vb = _sb(st, nc, "vb", [128, 8, 512], BF16)
        vsq = [_sb(st, nc, f"vsq{i}", [128, 512], BF16) for i in range(2)]
        sT = _sb(st, nc, "sT", [128, 8, 512], BF16)
        mean = _sb(st, nc, "cmean", [128, 512], F32)
        msq = _sb(st, nc, "cmsq", [128, 512], F32)
        rstd = _sb(st, nc, "crstd", [128, 512], F32)
        nmr = _sb(st, nc, "cnmr", [128, 512], F32)
        zt = [_sb(st, nc, f"czt{i}", [128, 512], F32) for i in range(2)]
        xt = [_sb(st, nc, f"cxt{i}", [128, D], F32) for i in range(2)]
        pc = [_ps(st, nc, f"pc{i}", [128, 512]) for i in range(2)]
        ps1 = _ps(st, nc, "ps1", [128, 512])
        ps2 = _ps(st, nc, "ps2", [128, 512])
        py = [_ps(st, nc, f"py{i}", [128, 512]) for i in range(4)]
        S.dma("sp", lambda e: e.dma_start(out=dwT[:], in_=I["cv_dwT"]), writes=[dwT.k])
        S.dma("sp", lambda e: e.dma_start(out=vecT[:], in_=I["cv_vecT"]), writes=[vecT.k])
        S.dma("pool", lambda e: e.dma_start(out=b2[:], in_=I["cv_b2"]), writes=[b2.k])
        src = I["cv_w2"].rearrange("(k p) n -> p k n", p=128)
        for k in range(8):
            S.dma("pool", lambda e, k=k: e.dma_start(out=w2[:, k, :], in_=src[:, k, :]), writes=[w2.k])
        for c in range(8):
            for k in range(31):
                S.op("dve", lambda e, c=c, k=k: e.tensor_scalar(dg[:, c * 31 + k, :], G["ident_b"][:], dwT[:, c, k:k + 1], None,
                                                              ALU.mult),
                     reads=[G["ident_b"].k, dwT.k], writes=[dg.k])
        vb2 = [vb, _sb(st, nc, "vb_b", [128, 8, 512], BF16)]
        mean2 = [mean, _sb(st, nc, "cmean_b", [128, 512], F32)]
        rstd2 = [rstd, _sb(st, nc, "crstd_b", [128, 512], F32)]
        nmr2 = [nmr, _sb(st, nc, "cnmr_b", [128, 512], F32)]
        tcnt = [0]

        def conv_stage(j):
            t0 = j * 512
            g_in = glb[j % 2]
            vbj, meanj, rstdj, nmrj = vb2[j % 2], mean2[j % 2], rstd2[j % 2], nmr2[j % 2]
            if t0 % SEQ == 0:
                S.op("pool", lambda e: e.memset(g_in[:, :, 0:30], 0.0), writes=[g_in.k])
                S.dma("sp", lambda e: e.dma_start(out=g_in[:, :, 30:542],
                                                  in_=Dr["gluT"][:, :, t0:t0 + 512].rearrange("k p t -> p k t")),
                      writes=[g_in.k])
            else:
                S.dma("sp", lambda e: e.dma_start(out=g_in[:, :, 0:542],
                                                  in_=Dr["gluT"][:, :, t0 - 30:t0 + 512].rearrange("k p t -> p k t")),
                      writes=[g_in.k])
            for c in range(8):
                p = pc[c % 2]
                vq = vsq[c % 2]
                for k in range(31):
                    S.op("pe", lambda e, k=k: e.matmul(p[:], dg[:, c * 31 + k, :], g_in[:, c, k:k + 512],
                                                      start=(k == 0), stop=(k == 30)),
                         reads=[dg.k, g_in.k], writes=[p.k])
                S.op("act", lambda e: e.activation(vbj[:, c, :], p[:], AF.Identity, bias=vecT[:, 0, c:c + 1], scale=1.0),
                     reads=[p.k, vecT.k], writes=[vbj.k])
                S.op("act", lambda e: e.activation(vq[:], p[:], AF.Square, bias=vecT[:, 0, c:c + 1], scale=1.0),
                     reads=[p.k, vecT.k], writes=[vq.k])
                S.op("pe", lambda e: e.matmul(ps1[:], G["ones_b"][:], vbj[:, c, :], start=(c == 0), stop=(c == 7)),
                     reads=[G["ones_b"].k, vbj.k], writes=[ps1.k])
                S.op("pe", lambda e: e.matmul(ps2[:], G["ones_b"][:], vq[:], start=(c == 0), stop=(c == 7)),
                     reads=[G["ones_b"].k, vq.k], writes=[ps2.k])
            S.op("act", lambda e: e.activation(meanj[:], ps1[:], AF.Copy, scale=1.0 / D), reads=[ps1.k], writes=[meanj.k])
            S.op("act", lambda e: e.activation(msq[:], ps1[:], AF.Square, scale=1.0 / D), reads=[ps1.k], writes=[msq.k])
            S.op("dve", lambda e: e.scalar_tensor_tensor(rstdj[:], ps2[:], 1.0 / D, msq[:], ALU.mult, ALU.subtract),
                 reads=[ps2.k, msq.k], writes=[rstdj.k])
            S.op("act", lambda e: e.activation(rstdj[:], rstdj[:], AF.Sqrt, bias=G["eps"][:], scale=1.0),
                 reads=[rstdj.k, G["eps"].k], writes=[rstdj.k])
            S.op("dve", lambda e: e.reciprocal(rstdj[:], rstdj[:]), reads=[rstdj.k], writes=[rstdj.k])
            S.op("dve", lambda e: e.scalar_tensor_tensor(nmrj[:], meanj[:], -1.0, rstdj[:], ALU.mult, ALU.mult),
                 reads=[meanj.k, rstdj.k], writes=[nmrj.k])

        def rest_stage(j):
            b = (j * 512) // SEQ
            t0 = j * 512
            vbj, rstdj, nmrj = vb2[j % 2], rstd2[j % 2], nmr2[j % 2]
            for c in range(8):
                z = zt[c % 2]
                S.op("dve", lambda e: e.tensor_tensor(z[:], vbj[:, c, :], rstdj[:], ALU.mult), reads=[vbj.k, rstdj.k], writes=[z.k])
                S.op("pool", lambda e: e.tensor_tensor(z[:], z[:], nmrj[:], ALU.add), reads=[z.k, nmrj.k], writes=[z.k])
                S.op("act", lambda e: e.activation(sT[:, c, :], z[:], AF.Silu, bias=vecT[:, 2, c:c + 1],
                                                   scale=vecT[:, 1, c:c + 1]),
                     reads=[z.k, vecT.k], writes=[sT.k])
            def tile_g(i):
                ti = j * 4 + i
                r0 = t0 + i * 128
                x_t = xt[ti % 2]
                S.dma("sp", lambda e: e.dma_start(out=x_t[:], in_=I["x"][r0:r0 + 128, :]), writes=[x_t.k])
                ys = []
                for n in range(2):
                    p = py[(ti % 2) * 2 + n]
                    for c in range(8):
                        S.op("pe", lambda e, c=c: e.matmul(p[:], sT[:, c, i * 128:(i + 1) * 128], w2[:, c, n * 512:(n + 1) * 512],
                                                          start=(c == 0), stop=False), reads=[sT.k, w2.k], writes=[p.k])
                    S.op("pe", lambda e: e.matmul(p[:], G["ones_b"][0:1, :], b2[0:1, n * 512:(n + 1) * 512], start=False, stop=True),
                         reads=[G["ones_b"].k, b2.k], writes=[p.k])
                    ys.append((p, p[:]))
                    yield
                yield from epi.run_g(ys, x_t, b, Dr["xa"][r0:r0 + 128, :])

            run_interleaved((tile_g(i) for i in range(4)), 2)

        NBK = NTOK // 512
        conv_stage(0)
        for j in range(NBK):
            if j + 1 < NBK:
                conv_stage(j + 1)
            rest_stage(j)


def phase_front(ctx, l):
    nc, S, I, Dr, G = ctx.nc, ctx.S, ctx.I, ctx.Dr, ctx.G
    with ExitStack() as st:
        S2, H2 = [], []
        for b in range(2):
            s2 = load_bc(ctx, st, f"S2_{b}", Dr["modrow"][l, b, 4 * D:5 * D])
            add_one(ctx, s2)
            S2.append(s2)
            H2.append(load_bc(ctx, st, f"H2_{b}", Dr["modrow"][l, b, 3 * D:4 * D]))
        rw = _sb(st, nc, "rw", [128, 8, NE], F32)
        rb = _sb(st, nc, "rb", [1, NE], F32)
        srun = _sb(st, nc, "srun", [128, NE], F32)
        xt = [_sb(st, nc, f"fx{i}", [128, D], F32) for i in range(4)]
        h2 = [_sb(st, nc, f"fh{i}", [128, D], F32) for i in range(4)]
        h2b = [_sb(st, nc, f"fhb{i}", [128, D], BF16) for i in range(5)]
        h2T = [_sb(st, nc, f"fhT{i}", [128, 8, 128], F32) for i in range(4)]
        sm = lambda nm, w: [_sb(st, nc, f"{nm}{i}", [128, w], F32) for i in range(4)]
        lg, top8, nv0, ex, ssum, mask, slotv, bad, oh, junk, slotf, okk, g4, gtmp = (
            sm("lg", NE), sm("top8", 8), sm("nv0", 1), sm("ex", 4), sm("ssum", 1), sm("mask", NE), sm("slotv", NE),
            sm("bad", NE), sm("oh", NE), sm("junk", NE), sm("slotf", 4), sm("okk", 4), sm("g4", 4), sm("gtmp", NE))
        cur = sm("cur", NE)
        psT = [_ps(st, nc, f"fpT{i}", [128, 512]) for i in range(2)]
        plg = [_ps(st, nc, f"fplg{i}", [128, 512]) for i in range(4)]
        S.dma("sp", lambda e: e.dma_start(out=rw[:], in_=I["router_w"][l].rearrange("(k p) e -> p k e", p=128)), writes=[rw.k])
        S.dma("sp", lambda e: e.dma_start(out=rb[:], in_=I["router_b"][l:l + 1, :]), writes=[rb.k])
        S.op("dve", lambda e: e.memset(srun[:], 0.0), writes=[srun.k])
        def tile_g(t):
            b = t // 16
            i = t % 4
            x_t, h, hb, hT = xt[i], h2[i], h2b[t % 5], h2T[i]
            S.dma("sp", lambda e: e.dma_start(out=x_t[:], in_=Dr["xa"][t * 128:(t + 1) * 128, :]), writes=[x_t.k])
            S.op("dve", lambda e: e.tensor_tensor(h[:], x_t[:], S2[b][:], ALU.mult), reads=[x_t.k, S2[b].k], writes=[h.k])
            S.op("dve", lambda e: e.tensor_tensor(h[:], h[:], H2[b][:], ALU.add), reads=[h.k, H2[b].k], writes=[h.k])
            S.op("act", lambda e: e.activation(hb[:], h[:], AF.Copy), reads=[h.k], writes=[hb.k])
            yield
            transpose_tile(ctx, h, psT, lambda k, p, ap: S.op(
                "act", lambda e: e.activation(hT[:, k, :], ap, AF.Copy), reads=[p.k], writes=[hT.k]))
            yield
            pl = plg[i]
            for k in range(8):
                S.op("pe", lambda e, k=k: e.matmul(pl[:, 0:NE], hT[:, k, :], rw[:, k, :], start=(k == 0), stop=False),
                     reads=[hT.k, rw.k], writes=[pl.k])
            S.op("pe", lambda e: e.matmul(pl[:, 0:NE], G["ones_f"][0:1, :], rb[0:1, :], start=False, stop=True),
                 reads=[G["ones_f"].k, rb.k], writes=[pl.k])
            S.op("dve", lambda e: e.tensor_copy(lg[i][:], pl[:, 0:NE]), reads=[pl.k], writes=[lg[i].k])
            yield
            S.op("dve", lambda e: e.tensor_copy(cur[i][:], lg[i][:]), reads=[lg[i].k], writes=[cur[i].k])
            yield
            for k in range(4):
                S.op("dve", lambda e, k=k: e.tensor_reduce(top8[i][:, k:k + 1], cur[i][:], AX.X, ALU.max),
                     reads=[cur[i].k], writes=[top8[i].k])
                if k < 3:
                    S.op("dve", lambda e, k=k: e.tensor_scalar(oh[i][:], cur[i][:], top8[i][:, k:k + 1], None, ALU.is_equal),
                         reads=[cur[i].k, top8[i].k], writes=[oh[i].k])
                    S.op("dve", lambda e: e.scalar_tensor_tensor(cur[i][:], oh[i][:], -BIG, cur[i][:], ALU.mult, ALU.add),
                         reads=[oh[i].k, cur[i].k], writes=[cur[i].k])
                yield
            S.op("dve", lambda e: e.tensor_scalar_mul(nv0[i][:], top8[i][:, 0:1], -1.0), reads=[top8[i].k], writes=[nv0[i].k])
            S.op("act", lambda e: e.activation(ex[i][:], top8[i][:, 0:4], AF.Exp, bias=nv0[i][:], scale=1.0),
                 reads=[top8[i].k, nv0[i].k], writes=[ex[i].k])
            S.op("dve", lambda e: e.tensor_reduce(ssum[i][:], ex[i][:], AX.X, ALU.add), reads=[ex[i].k], writes=[ssum[i].k])
            S.op("dve", lambda e: e.reciprocal(ssum[i][:], ssum[i][:]), reads=[ssum[i].k], writes=[ssum[i].k])
            S.op("dve", lambda e: e.tensor_scalar(g4[i][:], ex[i][:], ssum[i][:], None, ALU.mult),
                 reads=[ex[i].k, ssum[i].k], writes=[g4[i].k])
            yield
            S.op("dve", lambda e: e.tensor_scalar(mask[i][:], lg[i][:], top8[i][:, 3:4], None, ALU.is_ge),
                 reads=[lg[i].k, top8[i].k], writes=[mask[i].k])
            pp = plg[i]
            S.op("pe", lambda e: e.matmul(pp[:, 64:64 + NE], G["triU"][:], mask[i][:], start=True, stop=False),
                 reads=[G["triU"].k, mask[i].k], writes=[pp.k])
            S.op("pe", lambda e: e.matmul(pp[:, 64:64 + NE], G["ones_f"][:], srun[:], start=False, stop=True),
                 reads=[G["ones_f"].k, srun.k], writes=[pp.k])
            S.op("dve", lambda e: e.tensor_tensor(srun[:], srun[:], mask[i][:], ALU.add), reads=[srun.k, mask[i].k], writes=[srun.k])
            yield
            S.op("dve", lambda e: e.tensor_single_scalar(bad[i][:], pp[:, 64:64 + NE], float(CAP) - 0.5, ALU.is_ge),
                 reads=[pp.k], writes=[bad[i].k])
            S.op("dve", lambda e: e.tensor_tensor(slotv[i][:], pp[:, 64:64 + NE], G["ecap"][:], ALU.add),
                 reads=[pp.k, G["ecap"].k], writes=[slotv[i].k])
            S.op("dve", lambda e: e.scalar_tensor_tensor(slotv[i][:], bad[i][:], BIG, slotv[i][:], ALU.mult, ALU.add),
                 reads=[bad[i].k, slotv[i].k], writes=[slotv[i].k])
            yield
            for k in range(4):
                S.op("dve", lambda e, k=k: e.tensor_scalar(oh[i][:], lg[i][:], top8[i][:, k:k + 1], None, ALU.is_equal),
                     reads=[lg[i].k, top8[i].k], writes=[oh[i].k])
                S.op("dve", lambda e: e.tensor_tensor(junk[i][:], oh[i][:], slotv[i][:], ALU.mult),
                     reads=[oh[i].k, slotv[i].k], writes=[junk[i].k])
                S.op("dve", lambda e, k=k: e.tensor_reduce(slotf[i][:, k:k + 1], junk[i][:], AX.X, ALU.add),
                     reads=[junk[i].k], writes=[slotf[i].k])
                yield
            S.op("dve", lambda e: e.tensor_single_scalar(okk[i][:], slotf[i][:], 1.0e5, ALU.is_lt), reads=[slotf[i].k], writes=[okk[i].k])
            yield
            gk, sk, mk = G["gates"].kt[t], G["slots"].kt[t], G["gmat"].kt[t]
            S.op("dve", lambda e: e.tensor_tensor(G["gates"][:, t, :], g4[i][:], okk[i][:], ALU.mult),
                 reads=[g4[i].k, okk[i].k], writes=[gk])
            S.op("dve", lambda e: e.tensor_copy(G["slots"][:, t, :], slotf[i][:]), reads=[slotf[i].k], writes=[sk])
            yield
            for k in range(4):
                dstm = G["gmat"][:, t, :] if k == 0 else gtmp[i][:]
                S.op("dve", lambda e, k=k, dstm=dstm: e.tensor_scalar(dstm, lg[i][:], top8[i][:, k:k + 1], G["gates"][:, t, k:k + 1],
                                                                    ALU.is_equal, ALU.mult),
                     reads=[lg[i].k, top8[i].k, gk], writes=[mk if k == 0 else gtmp[i].k])
                if k > 0:
                    S.op("dve", lambda e: e.tensor_tensor(G["gmat"][:, t, :], G["gmat"][:, t, :], gtmp[i][:], ALU.add),
                         reads=[mk, gtmp[i].k], writes=[mk])
            for k in range(4):
                S.dma("pool", lambda e, k=k: e.indirect_dma_start(
                    out=Dr["Xe"], out_offset=bass.IndirectOffsetOnAxis(ap=G["slots"][:, t, k:k + 1], axis=0),
                    in_=hb[:], in_offset=None, bounds_check=ctx.bc_reg, oob_is_err=False), reads=[hb.k, sk])
            yield

        run_interleaved((tile_g(t) for t in range(NT)), 4)


def phase_experts(ctx, l):
    nc, S, I, Dr, G = ctx.nc, ctx.S, ctx.I, ctx.Dr, ctx.G
    with ExitStack() as st:
        win = [_sb(st, nc, f"win{i}", [128, 8, 2 * D], BF16) for i in range(2)]
        wout = [_sb(st, nc, f"wout{i}", [128, 8, D], BF16) for i in range(2)]
        bin_ = [_sb(st, nc, f"bin{i}", [128, 2 * D], F32) for i in range(2)]
        xe = [_sb(st, nc, f"xe{i}", [128, D], BF16) for i in range(2)]
        xT = [_sb(st, nc, f"xT{i}", [128, 8, 128], BF16) for i in range(2)]
        xg = [_sb(st, nc, f"xg{i}", [128, 512], F32) for i in range(2)]
        sg = [_sb(st, nc, f"sg{i}", [128, 512], F32) for i in range(2)]
        xl = [_sb(st, nc, f"xl{i}", [128, 512], F32) for i in range(2)]
        tt = [_sb(st, nc, f"tt{i}", [128, 512], F32) for i in range(2)]
        act = [_sb(st, nc, f"act{i}", [128, D], BF16) for i in range(2)]
        actT = [_sb(st, nc, f"actT{i}", [128, 8, 128], BF16) for i in range(2)]
        yb = [_sb(st, nc, f"yb{i}", [128, D], BF16) for i in range(2)]
        psT = [_ps(st, nc, f"epT{i}", [128, 1024], BF16) for i in range(2)]
        pu = [_ps(st, nc, f"epu{i}", [128, 512]) for i in range(4)]
        py = [_ps(st, nc, f"epy{i}", [128, 512]) for i in range(2)]

        def load_w(e):
            w, wo, bi = win[e % 2], wout[e % 2], bin_[e % 2]
            s1 = I["moe_w_in"][l, e].rearrange("(k p) n -> p k n", p=128)
            s2 = I["moe_w_out"][l, e].rearrange("(k p) n -> p k n", p=128)
            for k in range(8):
                S.dma("pool", lambda q, k=k: q.dma_start(out=w[:, k, :], in_=s1[:, k, :]), writes=[w.k])
            for k in range(8):
                S.dma("pool", lambda q, k=k: q.dma_start(out=wo[:, k, :], in_=s2[:, k, :]), writes=[wo.k])
            S.dma("sp", lambda q: q.dma_start(out=bi[:], in_=I["moe_b_in"][l, e, :].partition_broadcast(128)), writes=[bi.k])

        load_w(0)
        load_w(1)
        blocks = [(e_, j) for e_ in range(NE) for j in range(NBLK)]
        NB = len(blocks)
        xe4 = xe + [_sb(st, nc, f"xe{i}", [128, D], BF16) for i in range(2, 4)]

        def load_x(n):
            e_, j = blocks[n]
            r0 = e_ * CAP + j * 128
            x_e = xe4[n % 4]
            S.dma("sp", lambda q: q.dma_start(out=x_e[:], in_=Dr["Xe"][r0:r0 + 128, :]), writes=[x_e.k])

        def t_x(n):
            x_e, x_T = xe4[n % 4], xT[n % 2]
            pt = psT[0]
            for k in range(8):
                S.op("pe", lambda q, k=k: q.transpose(pt[:, k * 128:(k + 1) * 128], x_e[:, k * 128:(k + 1) * 128], G["ident_b"][:]),
                     reads=[x_e.k, G["ident_b"].k], writes=[pt.k])
            S.op("act", lambda q: q.activation(x_T[:].rearrange("p k t -> p (k t)"), pt[:], AF.Copy), reads=[pt.k], writes=[x_T.k])

        def mm1(n):
            e_, j = blocks[n]
            i = n % 2
            w, bi = win[e_ % 2], bin_[e_ % 2]
            x_T, a_ = xT[i], act[i]
            for hf in range(2):
                pg, pl = pu[hf * 2], pu[hf * 2 + 1]
                for (p, c0) in ((pg, hf * 512), (pl, D + hf * 512)):
                    for k in range(8):
                        S.op("pe", lambda q, k=k: q.matmul(p[:], x_T[:, k, :], w[:, k, c0:c0 + 512], start=(k == 0), stop=(k == 7)),
                             reads=[x_T.k, w.k], writes=[p.k])
                S.op("dve", lambda q: q.tensor_tensor(xg[hf][:], pg[:], bi[:, hf * 512:(hf + 1) * 512], ALU.add), reads=[pg.k, bi.k], writes=[xg[hf].k])
                S.op("dve", lambda q: q.tensor_scalar_min(xg[hf][:], xg[hf][:], SW_LIM), reads=[xg[hf].k], writes=[xg[hf].k])
                S.op("act", lambda q: q.activation(sg[hf][:], xg[hf][:], AF.Sigmoid, scale=SW_ALPHA), reads=[xg[hf].k], writes=[sg[hf].k])
                S.op("dve", lambda q: q.tensor_tensor(xl[hf][:], pl[:], bi[:, D + hf * 512:D + (hf + 1) * 512], ALU.add), reads=[pl.k, bi.k], writes=[xl[hf].k])
                S.op("dve", lambda q: q.tensor_scalar(xl[hf][:], xl[hf][:], SW_LIM, -SW_LIM, ALU.min, ALU.max), reads=[xl[hf].k], writes=[xl[hf].k])
                S.op("dve", lambda q: q.scalar_tensor_tensor(tt[hf][:], xl[hf][:], 1.0, xg[hf][:], ALU.add, ALU.mult),
                     reads=[xl[hf].k, xg[hf].k], writes=[tt[hf].k])
                S.op("dve", lambda q: q.tensor_tensor(a_[:, hf * 512:(hf + 1) * 512], tt[hf][:], sg[hf][:], ALU.mult),
                     reads=[tt[hf].k, sg[hf].k], writes=[a_.k])

        def t_a(n):
            i = n % 2
            a_, a_T = act[i], actT[i]
            pt2 = psT[1]
            for k in range(8):
                S.op("pe", lambda q, k=k: q.transpose(pt2[:, k * 128:(k + 1) * 128], a_[:, k * 128:(k + 1) * 128], G["ident_b"][:]),
                     reads=[a_.k, G["ident_b"].k], writes=[pt2.k])
            S.op("act", lambda q: q.activation(a_T[:].rearrange("p k t -> p (k t)"), pt2[:], AF.Copy), reads=[pt2.k], writes=[a_T.k])

        def mm2(n):
            e_, j = blocks[n]
            i = n % 2
            wo = wout[e_ % 2]
            r0 = e_ * CAP + j * 128
            a_T, y_ = actT[i], yb[i]
            for n2 in range(2):
                for k in range(8):
                    S.op("pe", lambda q, k=k: q.matmul(py[n2][:], a_T[:, k, :], wo[:, k, n2 * 512:(n2 + 1) * 512], start=(k == 0), stop=(k == 7)),
                         reads=[a_T.k, wo.k], writes=[py[n2].k])
                S.op("act", lambda q: q.activation(y_[:, n2 * 512:(n2 + 1) * 512], py[n2][:], AF.Copy), reads=[py[n2].k], writes=[y_.k])
            S.dma("act", lambda q: q.dma_start(out=Dr["Y"][r0:r0 + 128, :], in_=y_[:]), reads=[y_.k])

        for n in range(min(4, NB)):
            load_x(n)
        t_x(0)
        mm1(0)
        t_x(1)
        mm1(1)
        for m in range(NB):
            t_a(m)
            if m + 2 < NB:
                t_x(m + 2)
            mm2(m)
            if m + 4 < NB:
                load_x(m + 4)
            e_, j = blocks[m]
            if j == NBLK - 1 and e_ + 2 < NE:
                load_w(e_ + 2)
            if m + 2 < NB:
                mm1(m + 2)


def phase_combine(ctx, l):
    nc, S, I, Dr, G = ctx.nc, ctx.S, ctx.I, ctx.Dr, ctx.G
    final = (l == 1) or (ctx.upto == 5)
    with ExitStack() as st:
        epi = Epi(ctx, st, l, 1, 5)
        bo = _sb(st, nc, "bo", [NE, D], F32)
        yk = [[_sb(st, nc, f"yk{k}_{i}", [128, D], BF16) for i in range(4)] for k in range(4)]
        gmT = [_sb(st, nc, f"gmT{i}", [NE, 128], F32) for i in range(2)]
        acc = [_sb(st, nc, f"acc{i}", [128, D], F32) for i in range(2)]
        xt = [_sb(st, nc, f"cx{i}", [128, D], F32) for i in range(2)]
        pT = [_ps(st, nc, f"cpT{i}", [128, 512]) for i in range(2)]
        pb = [_ps(st, nc, f"cpb{i}", [128, 512]) for i in range(4)]
        S.dma("sp", lambda e: e.dma_start(out=bo[:], in_=I["moe_b_out"][l]), writes=[bo.k])
        for k in range(4):
            for i in range(4):
                S.op("pool", lambda e: e.memset(yk[k][i][:], 0.0), writes=[yk[k][i].k])

        def gathers(t):
            for k in range(4):
                S.dma("pool", lambda e, k=k: e.indirect_dma_start(
                    out=yk[k][t % 4][:], out_offset=None, in_=Dr["Y"],
                    in_offset=bass.IndirectOffsetOnAxis(ap=G["slots"][:, t, k:k + 1], axis=0),
                    bounds_check=ctx.bc_reg, oob_is_err=False), reads=[G["slots"].kt[t]], writes=[yk[k][t % 4].k])

        gathers(0)
        gathers(1)

        def tile_g(t):
            b = t // 16
            i = t % 2
            x_t = xt[i]
            S.dma("sp", lambda e: e.dma_start(out=x_t[:], in_=Dr["xa"][t * 128:(t + 1) * 128, :]), writes=[x_t.k])
            if t + 2 < NT:
                gathers(t + 2)
            S.op("pe", lambda e: e.transpose(pT[i][0:NE, 0:128], G["gmat"][:, t, :], G["ident_f"][:]),
                 reads=[G["gmat"].kt[t], G["ident_f"].k], writes=[pT[i].k])
            S.op("act", lambda e: e.activation(gmT[i][:], pT[i][0:NE, 0:128], AF.Copy), reads=[pT[i].k], writes=[gmT[i].k])
            yield
            a = acc[i]
            for n in range(2):
                p = pb[i * 2 + n]
                S.op("pe", lambda e: e.matmul(p[:], gmT[i][:], bo[:, n * 512:(n + 1) * 512], start=True, stop=True),
                     reads=[gmT[i].k, bo.k], writes=[p.k])
                S.op("dve", lambda e: e.scalar_tensor_tensor(a[:, n * 512:(n + 1) * 512], yk[0][t % 4][:, n * 512:(n + 1) * 512],
                                                             G["gates"][:, t, 0:1], p[:], ALU.mult, ALU.add),
                     reads=[yk[0][t % 4].k, G["gates"].kt[t], p.k], writes=[a.k])
                yield
            for k in range(1, 4):
                S.op("dve", lambda e, k=k: e.scalar_tensor_tensor(a[:], yk[k][t % 4][:], G["gates"][:, t, k:k + 1], a[:], ALU.mult, ALU.add),
                     reads=[yk[k][t % 4].k, G["gates"].kt[t], a.k], writes=[a.k])
                yield
            dst = (ctx.out if final else Dr["xb"])[t * 128:(t + 1) * 128, :]
            yield from epi.run_g([(a, a[:, 0:512]), (a, a[:, 512:1024])], x_t, b, dst)

        run_interleaved((tile_g(t) for t in range(NT)), 2)


def phase_kvq(ctx):
    nc, S, I, Dr, G = ctx.nc, ctx.S, ctx.I, ctx.Dr, ctx.G
    with ExitStack() as st:
        kvw = _sb(st, nc, "kvw", [128, 8, 2 * D], BF16)
        qw = _sb(st, nc, "qw", [128, 8, D], BF16)
        sc, sh = mod_scale_bias(ctx, st, 1, 0, 1, "m1b")
        xt = [_sb(st, nc, f"kx{i}", [128, D], F32) for i in range(3)]
        x2T = [_sb(st, nc, f"kxT{i}", [128, 8, 512], BF16) for i in range(2)]
        hT = [_sb(st, nc, f"khT{i}", [128, 8, 512], BF16) for i in range(2)]
        ko = [_sb(st, nc, f"ko{i}", [128, 8, 512], BF16) for i in range(2)]
        qo = [_sb(st, nc, f"qo{i}", [128, 8, 512], BF16) for i in range(2)]
        vo = [_sb(st, nc, f"vo{i}", [128, D], BF16) for i in range(2)]
        psT = [_ps(st, nc, f"kpT{i}", [128, 512]) for i in range(2)]
        pk = [_ps(st, nc, f"kpk{i}", [128, 512]) for i in range(2)]
        pq = [_ps(st, nc, f"kpq{i}", [128, 512]) for i in range(2)]
        pv = [_ps(st, nc, f"kpv{i}", [128, 512]) for i in range(2)]
        s1 = I["kv_w"].rearrange("(k p) n -> p k n", p=128)
        s2 = I["q_w"].rearrange("(k p) n -> p k n", p=128)
        for k in range(8):
            S.dma("pool", lambda e, k=k: e.dma_start(out=kvw[:, k, :], in_=s1[:, k, :]), writes=[kvw.k])
            S.dma("pool", lambda e, k=k: e.dma_start(out=qw[:, k, :], in_=s2[:, k, :]), writes=[qw.k])
        ti = 0
        vi = 0
        for j in range(NTOK // 512):
            b = (j * 512) // SEQ
            xT_, h = x2T[j % 2], hT[j % 2]
            for i in range(4):
                x_t = xt[ti % 3]
                ti += 1
                r0 = j * 512 + i * 128
                S.dma("sp", lambda e: e.dma_start(out=x_t[:], in_=Dr["xb"][r0:r0 + 128, :]), writes=[x_t.k])

                def ev(k, p, ap, i=i):
                    S.op("act", lambda e: e.activation(xT_[:, k, i * 128:(i + 1) * 128], ap, AF.Copy), reads=[p.k], writes=[xT_.k])
                    S.op("act", lambda e: e.activation(h[:, k, i * 128:(i + 1) * 128], ap, AF.Identity,
                                                       bias=sh[:, b, k:k + 1], scale=sc[:, b, k:k + 1]),
                         reads=[p.k, sh.k, sc.k], writes=[h.k])
                transpose_tile(ctx, x_t, psT, ev)
            import os
            part = int(os.environ.get("KVQ_PART", "9"))
            if part < 2:
                continue
            k_o, q_o = ko[j % 2], qo[j % 2]
            for m in range(8):
                p1, p2 = pk[m % 2], pq[m % 2]
                for k in range(8):
                    S.op("pe", lambda e, k=k: e.matmul(p1[:], kvw[:, k, m * 128:(m + 1) * 128], xT_[:, k, :], start=(k == 0), stop=(k == 7)),
                         reads=[kvw.k, xT_.k], writes=[p1.k])
                S.op("act", lambda e: e.activation(k_o[:, m, :], p1[:], AF.Copy), reads=[p1.k], writes=[k_o.k])
                for k in range(8):
                    S.op("pe", lambda e, k=k: e.matmul(p2[:], qw[:, k, m * 128:(m + 1) * 128], h[:, k, :], start=(k == 0), stop=(k == 7)),
                         reads=[qw.k, h.k], writes=[p2.k])
                S.op("act", lambda e: e.activation(q_o[:, m, :], p2[:], AF.Copy, scale=0.125), reads=[p2.k], writes=[q_o.k])
            S.dma("sp", lambda e: e.dma_start(out=Dr["kT"][:, :, j * 512:(j + 1) * 512].rearrange("k p t -> p k t"), in_=k_o[:]), reads=[k_o.k])
            S.dma("sp", lambda e: e.dma_start(out=Dr["qT"][:, :, j * 512:(j + 1) * 512].rearrange("k p t -> p k t"), in_=q_o[:]), reads=[q_o.k])
            if part < 3:
                continue
            for i in range(4):
                r0 = j * 512 + i * 128
                v_o = vo[vi % 2]
                vi += 1
                for n in range(2):
                    p = pv[n]
                    for k in range(8):
                        S.op("pe", lambda e, k=k: e.matmul(p[:], xT_[:, k, i * 128:(i + 1) * 128], kvw[:, k, D + n * 512:D + (n + 1) * 512],
                                                          start=(k == 0), stop=(k == 7)), reads=[xT_.k, kvw.k], writes=[p.k])
                    S.op("act", lambda e: e.activation(v_o[:, n * 512:(n + 1) * 512], p[:], AF.Copy), reads=[p.k], writes=[v_o.k])
                S.dma("sp", lambda e: e.dma_start(out=Dr["v"][r0:r0 + 128, :], in_=v_o[:]), reads=[v_o.k])


def phase_attn(ctx):
    nc, S, I, Dr, G = ctx.nc, ctx.S, ctx.I, ctx.Dr, ctx.G
    with ExitStack() as st:
        kT = _sb(st, nc, "akT", [128, 8, SEQ], BF16)
        qT = _sb(st, nc, "aqT", [128, 8, SEQ], BF16)
        v = _sb(st, nc, "av", [128, 16, D], BF16)
        oT = _sb(st, nc, "aoT", [128, 8, SEQ], BF16)
        ntri = _sb(st, nc, "ntri", [128, 128], BF16)
        nones = _sb(st, nc, "nones", [128, 128], BF16)
        mtmp = _sb(st, nc, "mtmp", [128, 512], F32)
        masks = [_sb(st, nc, f"amask{r}", [128, 512], BF16) for r in range(4)]
        e_sb = [_sb(st, nc, f"ae{i}", [128, 512], F32) for i in range(2)]
        sp = [[_sb(st, nc, f"asp{s_}{i}", [128, 512], BF16) for i in range(2)] for s_ in range(2)]
        a_sb = [[_sb(st, nc, f"aa{s_}{i}", [128, 512], BF16) for i in range(2)] for s_ in range(2)]
        acc = [[_sb(st, nc, f"aacc{s_}{i}", [128, 512], BF16) for i in range(2)] for s_ in range(2)]
        pz1 = [_ps(st, nc, f"apz{s_}", [128, 512]) for s_ in range(2)]
        pz = [[pz1[s_], pz1[s_]] for s_ in range(2)]
        pdum = _ps(st, nc, "apdum", [128, 512])
        NWARM = 3

        def warm(n=NWARM):
            for _ in range(n):
                S.op("pe", lambda q: q.matmul(pdum[:], ntri[:], masks[0][:], start=True, stop=True),
                     reads=[ntri.k, masks[0].k], writes=[pdum.k])
        pw = [_ps(st, nc, f"apw{i}", [128, 512]) for i in range(2)]
        po = [_ps(st, nc, f"apo{i}", [128, 512]) for i in range(2)]
        S.op("dve", lambda e: e.tensor_scalar(ntri[:], G["iota"][:], 0.0, -1.0, ALU.is_le, ALU.mult), reads=[G["iota"].k], writes=[ntri.k])
        S.op("dve", lambda e: e.memset(nones[:], -1.0), writes=[nones.k])
        for r in range(4):
            S.op("pool", lambda e, r=r: e.iota(mtmp[:], pattern=[[1, 512]], base=-128 * r, channel_multiplier=-1,
                                              allow_small_or_imprecise_dtypes=True), writes=[mtmp.k])
            S.op("dve", lambda e, r=r: e.tensor_single_scalar(masks[r][:], mtmp[:], 0.0, ALU.is_gt), reads=[mtmp.k], writes=[masks[r].k])
        it = 0
        for b in range(2):
            t0 = b * SEQ
            S.dma("sp", lambda e: e.dma_start(out=kT[:], in_=Dr["kT"][:, :, t0:t0 + SEQ].rearrange("k p t -> p k t")), writes=[kT.k])
            S.dma("sp", lambda e: e.dma_start(out=qT[:], in_=Dr["qT"][:, :, t0:t0 + SEQ].rearrange("k p t -> p k t")), writes=[qT.k])
            S.dma("sp", lambda e: e.dma_start(out=v[:], in_=Dr["v"][t0:t0 + SEQ, :].rearrange("(s p) d -> p s d", p=128)), writes=[v.k])
            for ch in range(8):
                pbs = [0, 64]
                for c in range(4):
                    nkb = 4 * c + 4
                    kbs = list(range(nkb - 1, -1, -1))
                    q_ap = [qT[pb:pb + 64, ch, c * 512:(c + 1) * 512] for pb in pbs]

                    def k_ap(kb, s_):
                        return kT[pbs[s_]:pbs[s_] + 64, ch, kb * 128:(kb + 1) * 128]

                    def stP(idx):
                        kb = kbs[idx]
                        r = kb - 4 * c
                        i2 = idx % 2
                        for s_ in (0, 1):
                            z = pz[s_][i2]
                            S.op("pe", lambda q: q.matmul(z[:], k_ap(kb, s_), q_ap[s_], start=True, stop=True), reads=[kT.k, qT.k], writes=[z.k])
                        warm()
                        for s_ in (0, 1):
                            z = pz[s_][i2]
                            S.op("act", lambda q: q.activation(e_sb[s_][:], z[:], AF.Exp), reads=[z.k], writes=[e_sb[s_].k])
                        for s_ in (0, 1):
                            S.op("act", lambda q: q.activation(sp[s_][i2][:], e_sb[s_][:], AF.Ln, bias=G["one"][:], scale=1.0),
                                 reads=[e_sb[s_].k, G["one"].k], writes=[sp[s_][i2].k])
                        if r >= 0:
                            for s_ in (0, 1):
                                S.op("dve", lambda q: q.tensor_tensor(sp[s_][i2][:], sp[s_][i2][:], masks[r][:], ALU.mult),
                                     reads=[sp[s_][i2].k, masks[r].k], writes=[sp[s_][i2].k])

                    def stQ(idx):
                        kb = kbs[idx]
                        r = kb - 4 * c
                        i2 = idx % 2
                        for s_ in (0, 1):
                            w = pw[s_]
                            S.op("pe", lambda q: q.matmul(w[:], k_ap(kb, s_), q_ap[s_], start=True, stop=False), reads=[kT.k, qT.k], writes=[w.k])
                            S.op("pe", lambda q: q.matmul(w[:], ntri[:], sp[s_][i2][:], start=False, stop=(idx == 0)),
                                 reads=[ntri.k, sp[s_][i2].k], writes=[w.k])
                            if idx > 0:
                                ac = acc[s_][(idx - 1) % 2]
                                S.op("pe", lambda q: q.matmul(w[:], nones[:], ac[:], start=False, stop=True), reads=[nones.k, ac.k], writes=[w.k])
                        warm()
                        for s_ in (0, 1):
                            S.op("act", lambda q: q.activation(a_sb[s_][i2][:], pw[s_][:], AF.Exp), reads=[pw[s_].k], writes=[a_sb[s_][i2].k])
                        if r >= 0:
                            for s_ in (0, 1):
                                S.op("dve", lambda q: q.tensor_tensor(a_sb[s_][i2][:], a_sb[s_][i2][:], masks[r][:], ALU.mult),
                                     reads=[a_sb[s_][i2].k, masks[r].k], writes=[a_sb[s_][i2].k])
                        for s_ in (0, 1):
                            h = 2 * ch + s_
                            S.op("pe", lambda q: q.matmul(po[s_][pbs[s_]:pbs[s_] + 64, :], v[:, kb, h * 64:(h + 1) * 64], a_sb[s_][i2][:],
                                                          start=(idx == 0), stop=(idx == nkb - 1)),
                                 reads=[v.k, a_sb[s_][i2].k], writes=[po[s_].k])
                        if idx < nkb - 1:
                            for s_ in (0, 1):
                                an = acc[s_][idx % 2]
                                if idx == 0:
                                    S.op("pool", lambda q: q.tensor_copy(an[:], sp[s_][i2][:]), reads=[sp[s_][i2].k], writes=[an.k])
                                else:
                                    S.op("pool", lambda q: q.tensor_tensor(an[:], acc[s_][(idx - 1) % 2][:], sp[s_][i2][:], ALU.add),
                                         reads=[acc[s_][(idx - 1) % 2].k, sp[s_][i2].k], writes=[an.k])

                    stP(0)
                    for idx in range(nkb):
                        if idx + 1 < nkb:
                            stP(idx + 1)
                        stQ(idx)
                    for s_ in (0, 1):
                        pb = 64 * s_
                        S.op("act", lambda q: q.activation(oT[pb:pb + 64, ch, c * 512:(c + 1) * 512], po[s_][pb:pb + 64, :], AF.Copy),
                             reads=[po[s_].k], writes=[oT.k])
            S.dma("sp", lambda e: e.dma_start(out=Dr["oT"][:, :, t0:t0 + SEQ].rearrange("k p t -> p k t"), in_=oT[:]), reads=[oT.k])


def phase_oproj(ctx):
    nc, S, I, Dr, G = ctx.nc, ctx.S, ctx.I, ctx.Dr, ctx.G
    with ExitStack() as st:
        ow = _sb(st, nc, "ow", [128, 8, D], BF16)
        epi = Epi(ctx, st, 1, 0, 2)
        oTb = [_sb(st, nc, f"ooT{i}", [128, 8, 512], BF16) for i in range(2)]
        xt = [_sb(st, nc, f"ox{i}", [128, D], F32) for i in range(2)]
        py = [_ps(st, nc, f"opy{i}", [128, 512]) for i in range(4)]
        s1 = I["o_w"].rearrange("(k p) n -> p k n", p=128)
        for k in range(8):
            S.dma("pool", lambda e, k=k: e.dma_start(out=ow[:, k, :], in_=s1[:, k, :]), writes=[ow.k])
        def tile_g(ti):
            j, i = ti // 4, ti % 4
            b = (j * 512) // SEQ
            o_b = oTb[j % 2]
            if i == 0:
                S.dma("sp", lambda e: e.dma_start(out=o_b[:], in_=Dr["oT"][:, :, j * 512:(j + 1) * 512].rearrange("k p t -> p k t")), writes=[o_b.k])
            r0 = j * 512 + i * 128
            x_t = xt[ti % 2]
            S.dma("sp", lambda e: e.dma_start(out=x_t[:], in_=Dr["xb"][r0:r0 + 128, :]), writes=[x_t.k])
            ys = []
            for n in range(2):
                p = py[(ti % 2) * 2 + n]
                for c in range(8):
                    S.op("pe", lambda e, c=c: e.matmul(p[:], o_b[:, c, i * 128:(i + 1) * 128], ow[:, c, n * 512:(n + 1) * 512],
                                                      start=(c == 0), stop=(c == 7)), reads=[o_b.k, ow.k], writes=[p.k])
                ys.append((p, p[:]))
                yield
            yield from epi.run_g(ys, x_t, b, Dr["xa"][r0:r0 + 128, :])

        run_interleaved((tile_g(ti) for ti in range(NT)), 2)


def cast_load(S, dst_ap_fn, src_ap_fn, ncols, wkey, q="pool", step=2048):
    for c0 in range(0, ncols, step):
        c1 = min(ncols, c0 + step)
        S.dma(q, lambda e, c0=c0, c1=c1: e.dma_start(out=dst_ap_fn(c0, c1), in_=src_ap_fn(c0, c1)), writes=[wkey])


def make_in_maps(inp, cores):
    f = lambda a: np.ascontiguousarray(a, dtype=np.float32)
    x, c = inp["x"], inp["c"]
    shared = {
        "ada_w": f(inp["ada_w"]), "ada_b": f(inp["ada_b"]),
        "ln_g": f(inp["ln_g"].reshape(4, D)), "ln_b": f(inp["ln_b"].reshape(4, D)),
        "cv_w1": f(inp["cv_w1"][0]),
        "cv_b1T": f(inp["cv_b1"][0].reshape(16, 128).T),
        "cv_dwT": f(inp["cv_dw"][0].reshape(31, 8, 128).transpose(2, 1, 0)),
        "cv_vecT": f(np.stack([inp["cv_db"][0].reshape(8, 128).T, inp["cv_ln_g"][0].reshape(8, 128).T,
                               inp["cv_ln_b"][0].reshape(8, 128).T], axis=1)),
        "cv_w2": f(inp["cv_w2"][0]), "cv_b2": f(inp["cv_b2"][0].reshape(1, D)),
        "kv_w": f(inp["kv_w"]), "q_w": f(inp["q_w"][0]), "o_w": f(inp["o_w"][0]),
        "router_w": f(inp["router_w"]), "router_b": f(inp["router_b"]),
        "moe_w_in": f(inp["moe_w_in"]), "moe_b_in": f(inp["moe_b_in"]),
        "moe_w_out": f(inp["moe_w_out"]), "moe_b_out": f(inp["moe_b_out"]),
    }
    maps = []
    for ci in cores:
        m = dict(shared)
        m["x"] = f(x[2 * ci:2 * ci + 2].reshape(NTOK, D))
        m["cT"] = f(c[2 * ci:2 * ci + 2].reshape(2, 8, 128).transpose(2, 1, 0))
        maps.append(m)
    return maps


def kernel(**inputs):
    inp = {k: np.asarray(v) for k, v in inputs.items()}
    nc = build_program()
    maps = make_in_maps(inp, list(range(8)))
    res = run_bass_kernel_spmd(nc, maps, core_ids=list(range(8)))
    outs = [r["out"].reshape(2, SEQ, D) for r in res.results]
    return np.concatenate(outs, axis=0).astype(np.float32)
```
